# Optimizing a Trainium2 kernel written in Bass

```python
import math
import jax, jax.numpy as jnp
from jax import lax
import numpy as np

D_MODEL = 1024
BATCH = 4
SEQ = 8192
DEPTH = 1

SB_WIDTH = D_MODEL // 2
SB_HEADS = 8
SB_HEAD_DIM = SB_WIDTH // SB_HEADS
ML_WIDTH = D_MODEL - SB_WIDTH
ML_HEADS = 4
ML_HEAD_DIM = ML_WIDTH // ML_HEADS
MIX_WIDTH = SB_WIDTH + ML_WIDTH
Q_BLOCK = 128
ML_CHUNK = 64
CONV_K = 4
PROJ_COLS = 3 * SB_WIDTH + 4 * ML_WIDTH + 2 * ML_HEADS
N_EXPERTS = 256
TOP_K = 8
N_GROUPS = 8
TOPK_GROUPS = 4
EXPERT_FF = D_MODEL // 4
SHARED_FF = D_MODEL // 4
ROUTED_SCALE = 2.5
DISPATCH_BLOCK = 128
DN_ALPHA = (2 * DEPTH) ** 0.25
DN_BETA = (8 * DEPTH) ** -0.25
LN_EPS = 1e-5

kernel_name = 'hymba_sb_mlstm_moe_deepnorm_adaln'


def layer_norm(x, g=None, b=None):
    xf = x.astype(jnp.float32)
    mu = xf.mean(-1, keepdims=True)
    var = jnp.square(xf - mu).mean(-1, keepdims=True)
    y = (xf - mu) * lax.rsqrt(var + LN_EPS)
    if g is not None:
        y = y * g.astype(jnp.float32) + b.astype(jnp.float32)
    return y.astype(x.dtype)


def causal_depthwise_conv(x, w, b):
    C = x.shape[-1]
    y = lax.conv_general_dilated(x, w[:, None, :].astype(x.dtype), window_strides=(1,),
                                 padding=[(CONV_K - 1, 0)],
                                 dimension_numbers=('NWC', 'WIO', 'NWC'),
                                 feature_group_count=C)
    return y + b


def stick_breaking_attention(q, k, v):
    B, H, S, dh = q.shape
    nq = S // Q_BLOCK
    scale = dh ** -0.5
    kf = k.astype(jnp.float32)
    vf = v.astype(jnp.float32)
    qb = jnp.moveaxis(q.astype(jnp.float32).reshape(B, H, nq, Q_BLOCK, dh), 2, 0)
    key_pos = jnp.arange(S)

    def block(args):
        q_blk, q0 = args
        z = jnp.einsum('bhqd,bhkd->bhqk', q_blk, kf) * scale
        q_pos = q0 + jnp.arange(Q_BLOCK)
        mask = key_pos[None, :] < q_pos[:, None]
        log_beta = jax.nn.log_sigmoid(z)
        log_1m = jnp.where(mask, log_beta - z, 0.0)
        after = lax.cumsum(log_1m, axis=3, reverse=True) - log_1m
        a = jnp.where(mask, jnp.exp(log_beta + after), 0.0)
        return jnp.einsum('bhqk,bhkd->bhqd', a, vf)

    out = lax.map(block, (qb, jnp.arange(nq) * Q_BLOCK))
    return jnp.moveaxis(out, 0, 2).reshape(B, H, S, dh)


def mlstm_chunkwise(q, k, v, i_pre, logf):
    B, H, S, dk = q.shape
    dv = v.shape[-1]
    L = ML_CHUNK
    nc = S // L
    k = k * dk ** -0.5

    def chunks(t):
        return jnp.moveaxis(t.reshape((B, H, nc, L) + t.shape[3:]), 2, 0)

    bcum = lax.cumsum(logf.reshape(B, H, nc, L), axis=3)
    causal = jnp.tril(jnp.ones((L, L), dtype=bool))

    def step(carry, xs):
        C, n, m = carry
        qc, kc, vc, ic, bc = xs
        log_d = jnp.where(causal, bc[..., :, None] - bc[..., None, :] + ic[..., None, :], -jnp.inf)
        inter = bc + m[..., None]
        m_t = jnp.maximum(inter, log_d.max(-1))
        w = jnp.einsum('bhtd,bhsd->bhts', qc, kc) * jnp.exp(log_d - m_t[..., None])
        s_inter = jnp.exp(inter - m_t)
        num = (s_inter[..., None] * jnp.einsum('bhtk,bhvk->bhtv', qc, C)
               + jnp.einsum('bhts,bhsv->bhtv', w, vc))
        den = s_inter * jnp.einsum('bhtk,bhk->bht', qc, n) + w.sum(-1)
        h = num / jnp.maximum(jnp.abs(den), jnp.exp(-m_t))[..., None]
        b_last = bc[..., -1]
        log_w = b_last[..., None] - bc + ic
        m_new = jnp.maximum(b_last + m, log_w.max(-1))
        wk = jnp.exp(log_w - m_new[..., None])
        decay = jnp.exp(b_last + m - m_new)
        C = decay[..., None, None] * C + jnp.einsum('bhs,bhsv,bhsk->bhvk', wk, vc, kc)
        n = decay[..., None] * n + jnp.einsum('bhs,bhsk->bhk', wk, kc)
        return (C, n, m_new), h

    init = (jnp.zeros((B, H, dv, dk), jnp.float32), jnp.zeros((B, H, dk), jnp.float32),
            jnp.zeros((B, H), jnp.float32))
    _, h = lax.scan(step, init, (chunks(q), chunks(k), chunks(v), chunks(i_pre), jnp.moveaxis(bcum, 2, 0)))
    return jnp.moveaxis(h, 0, 2).reshape(B, H, S, dv)


def hybrid_mixer(u, w_in, ml_conv_w, ml_conv_b, ml_gate_b, ml_norm_g, w_out):
    B, S, _ = u.shape
    proj = u @ w_in
    cuts = [SB_WIDTH, 2 * SB_WIDTH, 3 * SB_WIDTH, 3 * SB_WIDTH + ML_WIDTH,
            3 * SB_WIDTH + 2 * ML_WIDTH, 3 * SB_WIDTH + 3 * ML_WIDTH, 3 * SB_WIDTH + 4 * ML_WIDTH]
    sb_q, sb_k, sb_v, ml_q, ml_k, ml_v, ml_o, ml_if = jnp.split(proj, cuts, axis=-1)

    def heads(t, h):
        return t.reshape(B, S, h, -1).transpose(0, 2, 1, 3)

    sb = stick_breaking_attention(heads(sb_q, SB_HEADS), heads(sb_k, SB_HEADS), heads(sb_v, SB_HEADS))
    sb = sb.transpose(0, 2, 1, 3).reshape(B, S, SB_WIDTH).astype(u.dtype)

    qk = jax.nn.silu(causal_depthwise_conv(jnp.concatenate([ml_q, ml_k], -1), ml_conv_w, ml_conv_b))
    ml_q, ml_k = jnp.split(qk, 2, axis=-1)
    gates = ml_if.astype(jnp.float32) + ml_gate_b.astype(jnp.float32)
    i_pre = gates[..., :ML_HEADS].transpose(0, 2, 1)
    logf = jax.nn.log_sigmoid(gates[..., ML_HEADS:]).transpose(0, 2, 1)
    h = mlstm_chunkwise(heads(ml_q, ML_HEADS).astype(jnp.float32), heads(ml_k, ML_HEADS).astype(jnp.float32),
                        heads(ml_v, ML_HEADS).astype(jnp.float32), i_pre, logf)
    h = layer_norm(h).transpose(0, 2, 1, 3).reshape(B, S, ML_WIDTH) * ml_norm_g.astype(jnp.float32)
    ml = jax.nn.sigmoid(ml_o) * h.astype(u.dtype)

    return jnp.concatenate([sb, ml], axis=-1) @ w_out


def moe_ffn(u, w_router, router_bias, moe_w1, moe_w3, moe_w2, sh_w1, sh_w3, sh_w2):
    B, S, D = u.shape
    T = B * S
    xt = u.reshape(T, D)
    scores = jax.nn.sigmoid((xt @ w_router).astype(jnp.float32))
    sel = scores + router_bias.astype(jnp.float32)
    grp_score = lax.top_k(sel.reshape(T, N_GROUPS, -1), 2)[0].sum(-1)
    _, top_g = lax.top_k(grp_score, TOPK_GROUPS)
    gmask = jnp.any(top_g[..., None] == jnp.arange(N_GROUPS), axis=1)
    sel = jnp.where(jnp.repeat(gmask, N_EXPERTS // N_GROUPS, axis=1), sel, -jnp.inf)
    _, top_e = lax.top_k(sel, TOP_K)
    g = jnp.take_along_axis(scores, top_e, axis=1)
    g = g / g.sum(-1, keepdims=True) * ROUTED_SCALE

    TK = T * TOP_K
    e_flat = top_e.reshape(-1)
    tok_flat = jnp.repeat(jnp.arange(T, dtype=jnp.int32), TOP_K)
    order = jnp.argsort(e_flat)
    se, stok, sw = e_flat[order], tok_flat[order], g.reshape(-1)[order]
    counts = jax.ops.segment_sum(jnp.ones((TK,), jnp.int32), e_flat, num_segments=N_EXPERTS)
    offsets = jnp.cumsum(counts) - counts
    pcounts = (counts + DISPATCH_BLOCK - 1) // DISPATCH_BLOCK * DISPATCH_BLOCK
    pend = jnp.cumsum(pcounts)
    poffsets = pend - pcounts
    dest = poffsets[se] + (jnp.arange(TK, dtype=jnp.int32) - offsets[se])
    R = -(-TK // DISPATCH_BLOCK) * DISPATCH_BLOCK + N_EXPERTS * DISPATCH_BLOCK
    nb = R // DISPATCH_BLOCK
    buf_tok = jnp.full((R,), T, jnp.int32).at[dest].set(stok)
    buf_w = jnp.zeros((R,), xt.dtype).at[dest].set(sw.astype(xt.dtype))
    block_e = jnp.minimum(jnp.searchsorted(pend, jnp.arange(nb) * DISPATCH_BLOCK, side='right'),
                          N_EXPERTS - 1)
    x_pad = jnp.concatenate([xt, jnp.zeros((1, D), xt.dtype)], axis=0)

    def expert_block(y, blk):
        tok_b, w_b, e_b = blk
        xb = x_pad[tok_b]
        h = jax.nn.silu(xb @ moe_w1[e_b]) * (xb @ moe_w3[e_b])
        return y.at[tok_b].add((h @ moe_w2[e_b]) * w_b[:, None]), None

    y, _ = lax.scan(expert_block, jnp.zeros((T + 1, D), xt.dtype),
                    (buf_tok.reshape(nb, DISPATCH_BLOCK), buf_w.reshape(nb, DISPATCH_BLOCK), block_e))
    shared = (jax.nn.silu(xt @ sh_w1) * (xt @ sh_w3)) @ sh_w2
    return (y[:T] + shared).reshape(B, S, D)


def setup_inputs(seed: int = 0) -> dict:
    key = jax.random.key(seed)
    ks = jax.random.split(key, 24)
    f32 = jnp.float32
    D, L = D_MODEL, DEPTH

    def nrm(k, shape, scale):
        return jax.random.normal(k, shape, f32) * scale

    x = nrm(ks[0], (BATCH, SEQ, D), 1.0)
    c = nrm(ks[1], (BATCH, D), 1.0)
    w_ada = nrm(ks[2], (L, D, 6 * D), 0.5 * D ** -0.5)
    b_ada = nrm(ks[3], (L, 6 * D), 0.02)
    s_in = D ** -0.5
    w_in = jnp.concatenate([
        nrm(ks[4], (L, D, 2 * SB_WIDTH), s_in),
        nrm(ks[5], (L, D, SB_WIDTH), s_in * DN_BETA),
        nrm(ks[6], (L, D, 2 * ML_WIDTH), s_in),
        nrm(ks[7], (L, D, ML_WIDTH), s_in * DN_BETA),
        nrm(ks[8], (L, D, ML_WIDTH + 2 * ML_HEADS), s_in),
    ], axis=-1)
    ml_conv_w = nrm(ks[9], (L, CONV_K, 2 * ML_WIDTH), CONV_K ** -0.5)
    ml_conv_b = nrm(ks[10], (L, 2 * ML_WIDTH), 0.02)
    f_bias = jnp.linspace(3.0, 6.0, ML_HEADS, dtype=f32)
    ml_gate_b = jnp.concatenate([nrm(ks[11], (L, ML_HEADS), 0.1),
                                 f_bias[None, :] + nrm(ks[12], (L, ML_HEADS), 0.01)], axis=-1)
    ml_norm_g = 1.0 + nrm(ks[13], (L, ML_WIDTH), 0.02)
    w_out = nrm(ks[14], (L, MIX_WIDTH, D), MIX_WIDTH ** -0.5 * DN_BETA)
    ln1_g = 1.0 + nrm(ks[15], (L, D), 0.02)
    ln1_b = nrm(ks[16], (L, D), 0.02)
    w_router = nrm(ks[17], (L, D, N_EXPERTS), D ** -0.5)
    router_bias = nrm(ks[18], (L, N_EXPERTS), 0.01)
    kk = jax.random.split(ks[19], 6)
    moe_w1 = nrm(kk[0], (L, N_EXPERTS, D, EXPERT_FF), D ** -0.5)
    moe_w3 = nrm(kk[1], (L, N_EXPERTS, D, EXPERT_FF), D ** -0.5)
    moe_w2 = nrm(kk[2], (L, N_EXPERTS, EXPERT_FF, D), EXPERT_FF ** -0.5 * DN_BETA)
    sh_w1 = nrm(kk[3], (L, D, SHARED_FF), D ** -0.5)
    sh_w3 = nrm(kk[4], (L, D, SHARED_FF), D ** -0.5)
    sh_w2 = nrm(kk[5], (L, SHARED_FF, D), SHARED_FF ** -0.5 * DN_BETA)
    ln2_g = 1.0 + nrm(ks[20], (L, D), 0.02)
    ln2_b = nrm(ks[21], (L, D), 0.02)
    return {'x': x, 'c': c, 'w_ada': w_ada, 'b_ada': b_ada, 'w_in': w_in,
            'ml_conv_w': ml_conv_w, 'ml_conv_b': ml_conv_b, 'ml_gate_b': ml_gate_b,
            'ml_norm_g': ml_norm_g, 'w_out': w_out, 'ln1_g': ln1_g, 'ln1_b': ln1_b,
            'w_router': w_router, 'router_bias': router_bias, 'moe_w1': moe_w1,
            'moe_w3': moe_w3, 'moe_w2': moe_w2, 'sh_w1': sh_w1, 'sh_w3': sh_w3, 'sh_w2': sh_w2,
            'ln2_g': ln2_g, 'ln2_b': ln2_b}


def reference(x, c, w_ada, b_ada, w_in, ml_conv_w, ml_conv_b, ml_gate_b, ml_norm_g, w_out,
              ln1_g, ln1_b, w_router, router_bias, moe_w1, moe_w3, moe_w2, sh_w1, sh_w3, sh_w2,
              ln2_g, ln2_b):
    for l in range(DEPTH):
        mod = jax.nn.silu(c) @ w_ada[l] + b_ada[l]
        sh1, sc1, g1, sh2, sc2, g2 = [m[:, None, :] for m in jnp.split(mod, 6, axis=-1)]
        u = layer_norm(x) * (1.0 + sc1) + sh1
        mix = hybrid_mixer(u, w_in[l], ml_conv_w[l], ml_conv_b[l], ml_gate_b[l], ml_norm_g[l], w_out[l])
        x = layer_norm(DN_ALPHA * x + g1 * mix, ln1_g[l], ln1_b[l])
        u = layer_norm(x) * (1.0 + sc2) + sh2
        ffn = moe_ffn(u, w_router[l], router_bias[l], moe_w1[l], moe_w3[l], moe_w2[l],
                      sh_w1[l], sh_w3[l], sh_w2[l])
        x = layer_norm(DN_ALPHA * x + g2 * ffn, ln2_g[l], ln2_b[l])
    return x
```

```python
import os
import numpy as np
from contextlib import ExitStack
import concourse.bass as bass
import concourse.mybir as mybir
from concourse.bass_utils import run_bass_kernel_spmd

F32 = mybir.dt.float32
BF16 = mybir.dt.bfloat16
U32 = mybir.dt.uint32
I32 = mybir.dt.int32
NBLK = 512
AF = mybir.ActivationFunctionType
ALU = mybir.AluOpType

D = 1024
S_LEN = 8192
NOWN = 4096
PC = 3592
NE = 256
ALPHA = 2.0 ** 0.25
EPS = 1e-5
NEGV = -30000.0


class Res:
    def __init__(self, name, t=None):
        self.name = name
        self.t = t
        self.w = None
        self.r = []
        self.sem = None
        self.cnt = 0


class Sched:
    ENG = ("tensor", "vector", "scalar", "gpsimd", "sync")

    def __init__(self, nc, es):
        self.nc = nc
        self.es = es
        self.thunks = {e: [] for e in self.ENG}
        self.esem = {e: es.enter_context(nc.semaphore("s_" + e)) for e in self.ENG}
        self.ecnt = {e: 0 for e in self.ENG}
        self.known = {e: {} for e in self.ENG}
        self.semobj = {id(s): s for s in self.esem.values()}
        self.pool = []
        self.live = []
        self.nsem = 0
        self.nblocks = 0

    def _need(self, need, ev):
        if ev is None:
            return
        k, v = ev
        if need.get(k, 0) < v:
            need[k] = v

    def _deps(self, eng, reads, writes):
        need = {}
        for r in reads:
            self._need(need, r.w)
        for w in writes:
            self._need(need, w.w)
            for ev in w.r:
                self._need(need, ev)
        waits = []
        kn = self.known[eng]
        own = id(self.esem[eng])
        for k, v in need.items():
            if eng == "tensor" and k == own:
                continue
            if kn.get(k, 0) < v:
                kn[k] = v
                waits.append((self.semobj[k], v))
        return waits

    def _commit(self, ev, reads, writes):
        for r in reads:
            r.r.append(ev)
            if len(r.r) > 24:
                m = {}
                for k, v in r.r:
                    if m.get(k, 0) < v:
                        m[k] = v
                r.r = list(m.items())
        for w in writes:
            w.w = ev
            w.r = []

    def op(self, eng, fn, reads=(), writes=(), inc=True):
        waits = self._deps(eng, reads, writes)
        sem = self.esem[eng]
        if inc:
            self.ecnt[eng] += 1
            ev = (id(sem), self.ecnt[eng])
        else:
            ev = (id(sem), self.ecnt[eng] + 1)
        self._commit(ev, reads, writes)

        def thunk(e, waits=waits, fn=fn, inc=inc, sem=sem):
            for s, v in waits:
                e.wait_ge(s, v)
            ins = fn(e)
            if inc:
                ins.then_inc(sem, 1)
        self.thunks[eng].append(thunk)
        return ev

    def dma(self, eng, fn, sb, reads=(), writes=()):
        if sb.sem is None:
            if self.pool:
                sb.sem, sb.cnt = self.pool.pop()
            else:
                sb.sem = self.es.enter_context(self.nc.semaphore("d%d" % self.nsem))
                self.nsem += 1
                sb.cnt = 0
                self.semobj[id(sb.sem)] = sb.sem
            self.live.append(sb)
        waits = self._deps(eng, reads, writes)
        sb.cnt += 16
        ev = (id(sb.sem), sb.cnt)
        self._commit(ev, reads, writes)
        sem = sb.sem

        def thunk(e, waits=waits, fn=fn, sem=sem):
            for s, v in waits:
                e.wait_ge(s, v)
            fn(e).then_inc(sem, 16)
        self.thunks[eng].append(thunk)
        return ev

    def flush(self):
        nc = self.nc
        targets = [(self.esem[x], self.ecnt[x]) for x in self.ENG if self.ecnt[x] > 0]
        targets += [(r.sem, r.cnt) for r in self.live]
        for e in self.ENG:
            def thunk(eng, targets=targets):
                for s, v in targets:
                    eng.wait_ge(s, v)
            self.thunks[e].append(thunk)
            kn = self.known[e]
            for s, v in targets:
                kn[id(s)] = v
        th = self.thunks
        with nc.Block() as block:
            @block.sync
            def _(e):
                for t in th["sync"]:
                    t(e)

            @block.tensor
            def _(e):
                for t in th["tensor"]:
                    t(e)

            @block.vector
            def _(e):
                for t in th["vector"]:
                    t(e)

            @block.scalar
            def _(e):
                for t in th["scalar"]:
                    t(e)

            @block.gpsimd
            def _(e):
                for t in th["gpsimd"]:
                    t(e)
        self.thunks = {e: [] for e in self.ENG}
        self.nblocks += 1
        for r in self.live:
            self.pool.append((r.sem, r.cnt))
            r.sem = None
        self.live = []


def build_nc(stop_after=None, n_exp=NE + 1, dbg=False, n_decl=NE + 1, sparse=True):
    nc = bass.Bass("TRN2", target_bir_lowering=False)
    din = lambda n, sh, dt=F32: nc.dram_tensor(n, sh, dt, kind="ExternalInput").ap()
    dbgs = dbg if dbg else ()
    dsc = lambda n, sh, dt: nc.dram_tensor(n, sh, dt, kind=("ExternalOutput" if n in dbgs else "Internal")).ap()
    x_b = din("x_b", [S_LEN, D])
    x_own = din("x_own", [NOWN, D])
    cT = din("cT", [128, 8])
    flag_in = din("flag", [128, 2])
    w_ada = din("w_ada", [D, 6 * D])
    b_ada = din("b_ada", [1, 6 * D])
    w_in = din("w_in", [D, PC])
    conv_wT = din("conv_wT", [D, 4])
    conv_bT = din("conv_bT", [128, 8])
    gate_b_rep = din("gate_b_rep", [128, 512])
    ml_norm_g = din("ml_norm_g", [1, 512])
    w_out = din("w_out", [D, D])
    ln1_g = din("ln1_g", [1, D]); ln1_b = din("ln1_b", [1, D])
    w_router = din("w_router", [D, NE])
    router_bias = din("router_bias", [1, NE])
    if sparse:
        moe_w1 = din("moe_w1", [n_decl, 128, 8, 256])
        moe_w3 = din("moe_w3", [n_decl, 128, 8, 256])
        moe_w2 = din("moe_w2", [n_decl, 128, 2, D])
    else:
        moe_w1 = din("moe_w1", [n_decl, D, 256])
        moe_w3 = din("moe_w3", [n_decl, D, 256])
        moe_w2 = din("moe_w2", [n_decl, 256, D])
    ln2_g = din("ln2_g", [1, D]); ln2_b = din("ln2_b", [1, D])
    out = nc.dram_tensor("out", [NOWN, D], F32, kind="ExternalOutput").ap()

    mod_s = dsc("mod_s", [1, 6 * D], F32)
    sbqT = dsc("sbqT", [512, S_LEN], BF16); sbkT = dsc("sbkT", [512, S_LEN], BF16)
    sbv = dsc("sbv", [S_LEN, 512], BF16)
    mlqT = dsc("mlqT", [512, S_LEN], BF16); mlkT = dsc("mlkT", [512, S_LEN], BF16)
    mlv = dsc("mlv", [S_LEN, 512], BF16)
    mlo = dsc("mlo", [S_LEN, 512], F32)
    mlif = dsc("mlif", [S_LEN, 8], F32)
    sbo = dsc("sbo", [512, NOWN], BF16)
    mlout = dsc("mlout", [S_LEN, 512], BF16)
    x1s = dsc("x1s", [NOWN, D], F32)
    u2Ts = dsc("u2Ts", [128, 8, NOWN], BF16)
    gates_s = dsc("gates_s", [128, 32, NE + 1], F32)
    u2s = dsc("u2s", [NOWN, D], BF16)
    Xs = dsc("Xs", [NBLK * 128, D], BF16)
    Ys = dsc("Ys", [NBLK * 128, D], F32)

    with ExitStack() as eg:
        S = Sched(nc, eg)

        def mk(es, name, shape, dt, psum=False):
            t = es.enter_context(nc.psum_tensor(name, shape, dt) if psum else nc.sbuf_tensor(name, shape, dt))
            return Res(name, t)

        V = lambda fn, r=(), w=(), **k: S.op("vector", fn, r, w, **k)
        A = lambda fn, r=(), w=(), **k: S.op("scalar", fn, r, w, **k)
        G = lambda fn, r=(), w=(), **k: S.op("gpsimd", fn, r, w, **k)
        T = lambda fn, r=(), w=(), **k: S.op("tensor", fn, r, w, **k)
        DM = lambda fn, sb, r=(), w=(), eng="sync": S.dma(eng, fn, sb, r, w)

        identb = mk(eg, "identb", [128, 128], BF16)
        identf = mk(eg, "identf", [128, 128], F32)
        onesf = mk(eg, "onesf", [128, 128], F32)
        onesb = mk(eg, "onesb", [128, 128], BF16)
        epsc = mk(eg, "epsc", [128, 1], F32)
        onec = mk(eg, "onec", [128, 1], F32)
        flag = mk(eg, "flagt", [128, 2], F32)
        G(lambda e: e.memset(onesf.t[:], 1.0), w=[onesf])
        G(lambda e: e.memset(identf.t[:], 1.0), w=[identf])
        G(lambda e: e.affine_select(out=identf.t[:], in_=identf.t[:], pattern=[[-1, 128]], base=0,
                                     channel_multiplier=1, compare_op=ALU.is_equal, fill=0.0), r=[identf], w=[identf])
        V(lambda e: e.tensor_copy(out=identb.t[:], in_=identf.t[:]), r=[identf], w=[identb])
        V(lambda e: e.tensor_copy(out=onesb.t[:], in_=onesf.t[:]), r=[onesf], w=[onesb])
        V(lambda e: e.memset(epsc.t[:], EPS), w=[epsc])
        V(lambda e: e.memset(onec.t[:], 1.0), w=[onec])
        DM(lambda e: e.dma_start(out=flag.t[:], in_=flag_in[:, :]), flag, w=[flag])
        Mall = mk(eg, "Mall", [128, 32, NE], BF16)

        def layer_norm_stats(src, mv_out, scr6, n):
            nch = (n + 511) // 512
            for ci in range(nch):
                lo = ci * 512
                hi = min(n, lo + 512)
                V(lambda e, ci=ci, lo=lo, hi=hi: e.bn_stats(out=scr6.t[:, ci * 6:(ci + 1) * 6], in_=src.t[:, lo:hi]),
                  r=[src], w=[scr6])
            V(lambda e: e.bn_aggr(out=mv_out.t[:, 0:2], in_=scr6.t[:, 0:6 * nch]), r=[scr6], w=[mv_out])
            A(lambda e: e.activation(out=mv_out.t[:, 2:3], in_=mv_out.t[:, 1:2], func=AF.Ln, bias=epsc.t[:, 0:1], scale=1.0),
              r=[mv_out, epsc], w=[mv_out])
            A(lambda e: e.activation(out=mv_out.t[:, 1:2], in_=mv_out.t[:, 2:3], func=AF.Exp, scale=-0.5),
              r=[mv_out], w=[mv_out])

        with ExitStack() as es:
            ct = mk(es, "ct", [128, 8], F32)
            st = mk(es, "st", [128, 8], F32)
            rep = mk(es, "rep", [128, 8, 128], F32)
            wa = [mk(es, "wa%d" % i, [128, 8, 512], F32) for i in range(2)]
            bad = mk(es, "bad", [1, 6 * D], F32)
            modr = mk(es, "modr", [1, 6 * D], F32)
            pm = [mk(es, "pm%d" % i, [128, 512], F32, psum=True) for i in range(2)]
            DM(lambda e: e.dma_start(out=ct.t[:], in_=cT[:, :]), ct, w=[ct])
            DM(lambda e: e.dma_start(out=bad.t[:], in_=b_ada[:, :]), bad, w=[bad])
            A(lambda e: e.activation(out=st.t[:], in_=ct.t[:], func=AF.Silu), r=[ct], w=[st])
            for kt in range(8):
                V(lambda e, kt=kt: e.tensor_copy(out=rep.t[:, kt, :], in_=st.t[:, kt:kt + 1].to_broadcast([128, 128])),
                  r=[st], w=[rep])
            wav = w_ada.rearrange("(kt k) c -> k kt c", k=128)
            for ch in range(12):
                wb = wa[ch % 2]
                for kh in range(2):
                    DM(lambda e, wb=wb, ch=ch, kh=kh: e.dma_start(out=wb.t[:, kh * 4:(kh + 1) * 4, :],
                                                                  in_=wav[:, kh * 4:(kh + 1) * 4, ch * 512:(ch + 1) * 512]),
                       wb, w=[wb], eng=("sync" if kh == 0 else "gpsimd"))
                p = pm[ch % 2]
                for kt in range(8):
                    T(lambda e, p=p, wb=wb, kt=kt: e.matmul(p.t[:], lhsT=rep.t[:, kt, :], rhs=wb.t[:, kt, :],
                                                            start=(kt == 0), stop=(kt == 7)),
                      r=[rep, wb], w=[p], inc=(kt == 7))
                V(lambda e, p=p, ch=ch: e.tensor_tensor(out=modr.t[0:1, ch * 512:(ch + 1) * 512], in0=p.t[0:1, :],
                                                        in1=bad.t[0:1, ch * 512:(ch + 1) * 512], op=ALU.add),
                  r=[p, bad], w=[modr])
            for cidx in (1, 4):
                V(lambda e, cidx=cidx: e.tensor_scalar_add(out=modr.t[0:1, cidx * D:(cidx + 1) * D],
                                                           in0=modr.t[0:1, cidx * D:(cidx + 1) * D], scalar1=1.0),
                  r=[modr], w=[modr])
            DM(lambda e: e.dma_start(out=mod_s[:, :], in_=modr.t[:]), modr, r=[modr])
            S.flush()
        if stop_after == 0:
            return nc

        def load_bc(es, name, src_ap, n, eng="sync"):
            t = mk(es, name, [128, n], F32)
            DM(lambda e: e.dma_start(out=t.t[:], in_=src_ap.partition_broadcast(128)), t, w=[t], eng=eng)
            return t

        with ExitStack() as es:
            winb = mk(es, "winb", [128, 8, PC], BF16)
            sc1p = load_bc(es, "sc1p", mod_s[0:1, D:2 * D], D)
            sh1 = load_bc(es, "sh1", mod_s[0:1, 0:D], D, eng="gpsimd")
            cw = mk(es, "cw", [128, 8, 4], F32)
            cb = mk(es, "cb", [128, 8], F32)
            DM(lambda e: e.dma_start(out=cw.t[:], in_=conv_wT.rearrange("(c f) k -> f c k", f=128)), cw, w=[cw])
            DM(lambda e: e.dma_start(out=cb.t[:], in_=conv_bT[:, :]), cb, w=[cb])
            with ExitStack() as es2:
                wst = [mk(es2, "wst%d" % i, [128, PC], F32) for i in range(2)]
                wiv = w_in.rearrange("(kt k) c -> k kt c", k=128)
                for kt in range(8):
                    w = wst[kt % 2]
                    DM(lambda e, w=w, kt=kt: e.dma_start(out=w.t[:], in_=wiv[:, kt, :]), w, w=[w],
                       eng=("sync" if kt % 2 == 0 else "gpsimd"))
                    if kt % 2 == 0:
                        V(lambda e, w=w, kt=kt: e.tensor_copy(out=winb.t[:, kt, :], in_=w.t[:]), r=[w], w=[winb])
                    else:
                        G(lambda e, w=w, kt=kt: e.tensor_copy(out=winb.t[:, kt, :], in_=w.t[:]), r=[w], w=[winb])
                S.flush()
            xt = [mk(es, "xt%d" % i, [128, D], F32) for i in range(4)]
            ub = [mk(es, "ub%d" % i, [128, D], BF16) for i in range(4)]
            uT = [mk(es, "uT%d" % i, [128, 8, 512], BF16) for i in range(2)]
            scr4 = [mk(es, "scr6_%d" % i, [128, 12], F32) for i in range(4)]
            mv = [mk(es, "mv%d" % i, [128, 4], F32) for i in range(4)]
            ptp = [mk(es, "ptp%d" % i, [128, 8, 128], BF16, psum=True) for i in range(2)]
            ptk = [mk(es, "ptk%d" % i, [128, 512], F32, psum=True) for i in range(3)]
            pfm = [mk(es, "pfm%d" % i, [128, 512], F32, psum=True) for i in range(2)]
            pif = mk(es, "pif", [128, 8], F32, psum=True)
            evb = [mk(es, "evb%d" % i, [128, 512], BF16) for i in range(4)]
            evo = [mk(es, "evo%d" % i, [128, 512], F32) for i in range(4)]
            evi = [mk(es, "evi%d" % i, [128, 8], F32) for i in range(4)]
            fo = [mk(es, "fo%d" % i, [128, 512], BF16) for i in range(3)]
            cin = [mk(es, "cin%d" % i, [128, 515], F32) for i in range(8)]
            cacc = [mk(es, "cacc%d" % i, [128, 512], F32) for i in range(2)]
            csl = [mk(es, "csl%d" % i, [128, 512], F32) for i in range(2)]
            for i in range(8):
                G(lambda e, i=i: e.memset(cin[i].t[:, 0:3], 0.0), w=[cin[i]])
            nev = 0
            nfo = 0
            for stl in range(S_LEN // 512):
                u = uT[stl % 2]
                for j in range(4):
                    ti = stl * 4 + j
                    x = xt[j]
                    DM(lambda e, x=x, ti=ti: e.dma_start(out=x.t[:], in_=x_b[ti * 128:(ti + 1) * 128, :]), x, w=[x],
                       eng=("sync" if j % 2 == 0 else "gpsimd"))
                for j in range(4):
                    x = xt[j]; m = mv[j]; sc6 = scr4[j]
                    for ci in range(2):
                        V(lambda e, x=x, sc6=sc6, ci=ci: e.bn_stats(out=sc6.t[:, ci * 6:(ci + 1) * 6], in_=x.t[:, ci * 512:(ci + 1) * 512]),
                          r=[x], w=[sc6])
                    V(lambda e, m=m, sc6=sc6: e.bn_aggr(out=m.t[:, 0:2], in_=sc6.t[:, 0:12]), r=[sc6], w=[m])
                for j in range(4):
                    m = mv[j]
                    A(lambda e, m=m: e.activation(out=m.t[:, 2:3], in_=m.t[:, 1:2], func=AF.Ln, bias=epsc.t[:, 0:1], scale=1.0),
                      r=[m, epsc], w=[m])
                for j in range(4):
                    m = mv[j]
                    A(lambda e, m=m: e.activation(out=m.t[:, 1:2], in_=m.t[:, 2:3], func=AF.Exp, scale=-0.5), r=[m], w=[m])
                for j in range(4):
                    x = xt[j]; m = mv[j]
                    V(lambda e, x=x, m=m: e.tensor_scalar(out=x.t[:], in0=x.t[:], scalar1=m.t[:, 0:1], scalar2=m.t[:, 1:2],
                                                          op0=ALU.subtract, op1=ALU.mult), r=[x, m], w=[x])
                for j in range(4):
                    x = xt[j]
                    G(lambda e, x=x: e.tensor_tensor(out=x.t[:], in0=x.t[:], in1=sc1p.t[:], op=ALU.mult), r=[x, sc1p], w=[x])
                for j in range(4):
                    x = xt[j]; ubf = ub[j]
                    V(lambda e, x=x, ubf=ubf: e.tensor_tensor(out=ubf.t[:], in0=x.t[:], in1=sh1.t[:], op=ALU.add),
                      r=[x, sh1], w=[ubf])
                for j in range(4):
                    ti = stl * 4 + j
                    ubf = ub[j]
                    pt = ptp[ti % 2]
                    for kt in range(8):
                        T(lambda e, pt=pt, ubf=ubf, kt=kt: e.transpose(out=pt.t[:, kt, :], in_=ubf.t[:, kt * 128:(kt + 1) * 128],
                                                                      identity=identb.t[:]),
                          r=[ubf, identb], w=[pt], inc=(kt == 7))
                    A(lambda e, pt=pt, u=u, j=j: e.activation(out=u.t[:, :, j * 128:(j + 1) * 128], in_=pt.t[:], func=AF.Copy),
                      r=[pt], w=[u])
                for j in range(4):
                    ti = stl * 4 + j
                    for ci, (c0, dst) in enumerate(((1024, sbv), (2560, mlv), (3072, mlo))):
                        p = ptk[ci]
                        for kt in range(8):
                            T(lambda e, p=p, u=u, kt=kt, j=j, c0=c0: e.matmul(p.t[:], lhsT=u.t[:, kt, j * 128:(j + 1) * 128],
                                                                              rhs=winb.t[:, kt, c0:c0 + 512],
                                                                              start=(kt == 0), stop=(kt == 7)),
                              r=[u, winb], w=[p], inc=(kt == 7))
                        if ci < 2:
                            ev = evb[nev % 4]; nev += 1
                            A(lambda e, p=p, ev=ev: e.activation(out=ev.t[:], in_=p.t[:], func=AF.Copy), r=[p], w=[ev])
                        else:
                            ev = evo[j]
                            A(lambda e, p=p, ev=ev: e.activation(out=ev.t[:], in_=p.t[:], func=AF.Sigmoid), r=[p], w=[ev])
                        DM(lambda e, ev=ev, dst=dst, ti=ti: e.dma_start(out=dst[ti * 128:(ti + 1) * 128, :], in_=ev.t[:]),
                           ev, r=[ev], eng="gpsimd")
                    for kt in range(8):
                        T(lambda e, u=u, kt=kt, j=j: e.matmul(pif.t[:], lhsT=u.t[:, kt, j * 128:(j + 1) * 128],
                                                              rhs=winb.t[:, kt, 3584:3592], start=(kt == 0), stop=(kt == 7)),
                          r=[u, winb], w=[pif], inc=(kt == 7))
                    ev = evi[j]
                    V(lambda e, ev=ev: e.tensor_copy(out=ev.t[:], in_=pif.t[:]), r=[pif], w=[ev])
                    DM(lambda e, ev=ev, ti=ti: e.dma_start(out=mlif[ti * 128:(ti + 1) * 128, :], in_=ev.t[:]), ev, r=[ev],
                       eng="gpsimd")
                for ct_ in range(16):
                    c0 = (ct_ * 128) if ct_ < 8 else (1536 + (ct_ - 8) * 128)
                    p = pfm[ct_ % 2]
                    for kt in range(8):
                        T(lambda e, p=p, u=u, kt=kt, c0=c0: e.matmul(p.t[:], lhsT=winb.t[:, kt, c0:c0 + 128], rhs=u.t[:, kt, :],
                                                                     start=(kt == 0), stop=(kt == 7)),
                          r=[u, winb], w=[p], inc=(kt == 7))
                    f = fo[nfo % 3]; nfo += 1
                    if ct_ < 8:
                        dst = sbqT if ct_ < 4 else sbkT
                        A(lambda e, p=p, f=f: e.activation(out=f.t[:], in_=p.t[:], func=AF.Copy), r=[p], w=[f])
                    else:
                        c8 = ct_ - 8
                        dst = mlqT if c8 < 4 else mlkT
                        ci_ = cin[c8]; ac = cacc[c8 % 2]; sl = csl[c8 % 2]
                        V(lambda e, p=p, ci_=ci_: e.tensor_copy(out=ci_.t[:, 3:515], in_=p.t[:]), r=[p], w=[ci_])
                        V(lambda e, ci_=ci_, ac=ac, c8=c8: e.tensor_scalar(out=ac.t[:], in0=ci_.t[:, 3:515],
                                                                           scalar1=cw.t[:, c8, 3:4], scalar2=cb.t[:, c8:c8 + 1],
                                                                           op0=ALU.mult, op1=ALU.add), r=[ci_, cw, cb], w=[ac])
                        for k in range(3):
                            V(lambda e, ci_=ci_, ac=ac, c8=c8, k=k: e.scalar_tensor_tensor(
                                out=ac.t[:], in0=ci_.t[:, k:k + 512], scalar=cw.t[:, c8, k:k + 1], in1=ac.t[:],
                                op0=ALU.mult, op1=ALU.add), r=[ci_, cw, ac], w=[ac])
                        G(lambda e, ci_=ci_: e.tensor_copy(out=ci_.t[:, 0:3], in_=ci_.t[:, 512:515]), r=[ci_], w=[ci_])
                        if c8 < 4:
                            A(lambda e, ac=ac, f=f: e.activation(out=f.t[:], in_=ac.t[:], func=AF.Silu), r=[ac], w=[f])
                        else:
                            A(lambda e, ac=ac, sl=sl: e.activation(out=sl.t[:], in_=ac.t[:], func=AF.Silu), r=[ac], w=[sl])
                            G(lambda e, sl=sl, f=f: e.tensor_scalar(out=f.t[:], in0=sl.t[:], scalar1=128.0 ** -0.5, scalar2=None,
                                                                    op0=ALU.mult), r=[sl], w=[f])
                    r0 = (ct_ % 4) * 128
                    DM(lambda e, f=f, dst=dst, r0=r0, stl=stl: e.dma_start(out=dst[r0:r0 + 128, stl * 512:(stl + 1) * 512],
                                                                           in_=f.t[:]), f, r=[f])
            S.flush()
        if stop_after == 1:
            return nc

        with ExitStack() as es:
            tri = mk(es, "tri", [128, 128], BF16)
            trif = mk(es, "trif", [128, 128], F32)
            G(lambda e: e.memset(trif.t[:], 1.0), w=[trif])
            G(lambda e: e.affine_select(out=trif.t[:], in_=trif.t[:], pattern=[[-1, 128]], base=0, channel_multiplier=1,
                                         compare_op=ALU.is_ge, fill=0.0), r=[trif], w=[trif])
            V(lambda e: e.tensor_copy(out=tri.t[:], in_=trif.t[:]), r=[trif], w=[tri])
            msk = []
            for j in range(4):
                mj = mk(es, "msk%d" % j, [128, 512], F32)
                G(lambda e, mj=mj: e.memset(mj.t[:], 1.0), w=[mj])
                G(lambda e, mj=mj, j=j: e.affine_select(out=mj.t[:], in_=mj.t[:], pattern=[[1, 512]], base=-128 * j - 1,
                                                        channel_multiplier=-1, compare_op=ALU.is_ge, fill=0.0), r=[mj], w=[mj])
                msk.append(mj)
            meff = []
            for pp_ in range(8):
                me = mk(es, "meff%d" % pp_, [128, 512], BF16)
                if pp_ < 4:
                    V(lambda e, me=me, pp_=pp_: e.tensor_scalar(out=me.t[:], in0=msk[pp_].t[:], scalar1=flag.t[:, 1:2],
                                                                scalar2=flag.t[:, 0:1], op0=ALU.mult, op1=ALU.add),
                      r=[msk[pp_], flag], w=[me])
                else:
                    V(lambda e, me=me, pp_=pp_: e.tensor_scalar(out=me.t[:], in0=msk[pp_ - 4].t[:], scalar1=flag.t[:, 0:1],
                                                                scalar2=None, op0=ALU.mult), r=[msk[pp_ - 4], flag], w=[me])
                meff.append(me)
            kT2 = [mk(es, "kT2_%d" % i, [128, S_LEN], BF16) for i in range(2)]
            qT2 = [mk(es, "qT2_%d" % i, [128, S_LEN], BF16) for i in range(2)]
            v2 = [mk(es, "v2_%d" % i, [128, 64, 128], BF16) for i in range(2)]
            q_own = mk(es, "q_own", [128, NOWN], BF16)
            NS = 2
            E32 = [[mk(es, "E32_%d_%d" % (st_, i), [128, 512], F32) for i in range(3)] for st_ in range(NS)]
            G32 = [[mk(es, "G32_%d_%d" % (st_, i), [128, 512], F32) for i in range(2)] for st_ in range(NS)]
            Lb = [[mk(es, "Lb%d_%d" % (st_, i), [128, 512], BF16) for i in range(3)] for st_ in range(NS)]
            Ab = [[mk(es, "Ab%d_%d" % (st_, i), [128, 512], BF16) for i in range(3)] for st_ in range(NS)]
            Acc = [mk(es, "Acc%d" % st_, [128, 512], F32) for st_ in range(NS)]
            Accb = [[mk(es, "Accb%d_%d" % (st_, i), [128, 512], BF16) for i in range(3)] for st_ in range(NS)]
            ob = [[mk(es, "ob%d_%d" % (st_, i), [64, 512], BF16) for i in range(2)] for st_ in range(NS)]
            pZ = [mk(es, "pZ%d" % st_, [128, 512], F32, psum=True) for st_ in range(NS)]
            pC = [mk(es, "pC%d" % st_, [128, 512], F32, psum=True) for st_ in range(NS)]
            pO = [[mk(es, "pO%d_%d" % (st_, i), [64, 512], F32, psum=True) for i in range(1)] for st_ in range(NS)]
            pA = [mk(es, "pA%d" % st_, [128, 512], F32, psum=True) for st_ in range(NS)]
            sbv_v = sbv.rearrange("(b p) d -> p b d", p=128)
            n = 0
            nq = 0
            for hp in range(int(os.environ.get('KB_HEADS', '8')) // 2):
                kT = kT2[hp % 2]; qT = qT2[hp % 2]; v = v2[hp % 2]
                DM(lambda e, kT=kT, hp=hp: e.dma_start(out=kT.t[:], in_=sbkT[hp * 128:(hp + 1) * 128, :]), kT, w=[kT])
                DM(lambda e, qT=qT, hp=hp: e.dma_start(out=qT.t[:], in_=sbqT[hp * 128:(hp + 1) * 128, :]), qT, w=[qT], eng="gpsimd")
                for q4 in range(4):
                    DM(lambda e, v=v, hp=hp, q4=q4: e.dma_start(out=v.t[:, q4 * 16:(q4 + 1) * 16, :],
                                                                in_=sbv_v[:, q4 * 16:(q4 + 1) * 16, hp * 128:(hp + 1) * 128]),
                       v, w=[v])
                qv = qT.t[:].rearrange("p (j two t) -> p j two t", two=2, t=512)
                qo = q_own.t[:].rearrange("p (j t) -> p j t", t=512)
                G(lambda e, qv=qv, qT=qT: e.tensor_scalar(out=qv[:, :, 1, :], in0=qv[:, :, 1, :], scalar1=flag.t[:, 0:1], scalar2=None,
                                                          op0=ALU.mult), r=[qT, flag], w=[qT])
                V(lambda e, qv=qv, qo=qo, qT=qT: e.scalar_tensor_tensor(out=qo, in0=qv[:, :, 0, :], scalar=flag.t[:, 1:2],
                                                                       in1=qv[:, :, 1, :], op0=ALU.mult, op1=ALU.add),
                  r=[qT, flag], w=[q_own])
                for qt in range(int(os.environ.get('KB_QT', str(NOWN // 512)))):
                    blocks = list(reversed(range(8 * qt + 8)))
                    nkb = len(blocks)
                    pos = [pO[st_][0] for st_ in range(NS)]
                    obs = [ob[st_][nq % 2] for st_ in range(NS)]
                    nq += 1

                    def stage1(st_, idx, kb, n_, qt=qt, kT=kT):
                        dj = kb - 8 * qt
                        pz = pZ[st_]; Et = E32[st_][n_ % 3]; Lt = Lb[st_][n_ % 3]
                        p0 = st_ * 64
                        T(lambda e: e.matmul(pz.t[:], lhsT=kT.t[p0:p0 + 64, kb * 128:(kb + 1) * 128],
                                             rhs=q_own.t[p0:p0 + 64, qt * 512:(qt + 1) * 512], start=True, stop=True),
                          r=[kT, q_own], w=[pz])
                        A(lambda e: e.activation(out=Et.t[:], in_=pz.t[:], func=AF.Exp, scale=0.125), r=[pz], w=[Et])
                        if dj >= 0:
                            V(lambda e: e.tensor_tensor(out=Et.t[:], in0=Et.t[:], in1=meff[dj].t[:], op=ALU.mult),
                              r=[Et, meff[dj]], w=[Et])
                        return (idx, kb, Et, Lt, n_)

                    def stage1b(st_, item, nkb=nkb):
                        (idx, kb, Et, Lt, n_) = item
                        ac = Acc[st_]
                        A(lambda e: e.activation(out=Lt.t[:], in_=Et.t[:], func=AF.Ln, bias=onec.t[:, 0:1], scale=1.0),
                          r=[Et, onec], w=[Lt])

                    def stage1c(st_, item):
                        (idx, kb, Et, Lt, n_) = item
                        pa = pA[st_]
                        T(lambda e: e.matmul(pa.t[:], lhsT=identb.t[:], rhs=Lt.t[:], start=(idx == 0), stop=True),
                          r=[identb, Lt], w=[pa])
                        ab = Accb[st_][idx % 3]
                        V(lambda e: e.tensor_copy(out=ab.t[:], in_=pa.t[:]), r=[pa], w=[ab])

                    def stage2(st_, item):
                        (idx, kb, Et, Lt, n_) = item
                        pc = pC[st_]; g = G32[st_][n_ % 2]; a = Ab[st_][n_ % 3]
                        first = (idx == 0)
                        T(lambda e: e.matmul(pc.t[:], lhsT=tri.t[:], rhs=Lt.t[:], start=True, stop=first),
                          r=[tri, Lt], w=[pc], inc=first)
                        if not first:
                            ab = Accb[st_][(idx - 1) % 3]
                            T(lambda e: e.matmul(pc.t[:], lhsT=onesb.t[:], rhs=ab.t[:], start=False, stop=True),
                              r=[onesb, ab], w=[pc])
                        A(lambda e: e.activation(out=g.t[:], in_=pc.t[:], func=AF.Exp, scale=-1.0), r=[pc], w=[g])
                        V(lambda e: e.tensor_tensor(out=a.t[:], in0=Et.t[:], in1=g.t[:], op=ALU.mult), r=[Et, g], w=[a])
                        return (idx, kb, a)

                    def stage3(st_, item, v=v, nkb=nkb, pos=pos):
                        (idx, kb, a) = item
                        po = pos[st_]
                        last = (idx == nkb - 1)
                        T(lambda e: e.matmul(po.t[:], lhsT=v.t[:, kb, st_ * 64:(st_ + 1) * 64], rhs=a.t[:],
                                             start=(idx == 0), stop=last), r=[v, a], w=[po], inc=True)

                    s2q = [[] for _ in range(NS)]
                    s3q = [[] for _ in range(NS)]
                    pend1c = [None] * NS
                    for idx, blk in enumerate(blocks):
                        its = [stage1(st_, idx, blk, n) for st_ in range(NS)]
                        for st_ in range(NS):
                            if pend1c[st_] is not None:
                                stage1c(st_, pend1c[st_])
                                pend1c[st_] = None
                        for st_ in range(NS):
                            if s3q[st_]:
                                stage3(st_, s3q[st_].pop(0))
                        for st_ in range(NS):
                            if s2q[st_]:
                                s3q[st_].append(stage2(st_, s2q[st_].pop(0)))
                        for st_ in range(NS):
                            stage1b(st_, its[st_])
                            s2q[st_].append(its[st_])
                            if idx < nkb - 1:
                                pend1c[st_] = its[st_]
                        n += 1
                    while any(s2q) or any(s3q):
                        for st_ in range(NS):
                            if s3q[st_]:
                                stage3(st_, s3q[st_].pop(0))
                        for st_ in range(NS):
                            if s2q[st_]:
                                s3q[st_].append(stage2(st_, s2q[st_].pop(0)))
                    for st_ in range(NS):
                        po = pos[st_]; o = obs[st_]; h = hp * 2 + st_
                        V(lambda e, po=po, o=o: e.tensor_copy(out=o.t[:], in_=po.t[:]), r=[po], w=[o])
                        DM(lambda e, o=o, h=h, qt=qt: e.dma_start(out=sbo[h * 64:(h + 1) * 64, qt * 512:(qt + 1) * 512], in_=o.t[:]),
                           o, r=[o], eng="gpsimd")
                if hp % 2 == 1:
                    S.flush()
            S.flush()
        if stop_after == 2:
            return nc

        with ExitStack() as es:
            NT = S_LEN // 128
            triLE = mk(es, "triLE", [128, 128], F32)
            negm = mk(es, "negm", [128, 128], F32)
            G(lambda e: e.memset(triLE.t[:], 1.0), w=[triLE])
            G(lambda e: e.affine_select(out=triLE.t[:], in_=triLE.t[:], pattern=[[1, 128]], base=0, channel_multiplier=-1,
                                         compare_op=ALU.is_ge, fill=0.0), r=[triLE], w=[triLE])
            G(lambda e: e.memset(negm.t[:], 0.0), w=[negm])
            G(lambda e: e.affine_select(out=negm.t[:], in_=negm.t[:], pattern=[[1, 128]], base=0, channel_multiplier=-1,
                                         compare_op=ALU.is_ge, fill=NEGV), r=[negm], w=[negm])
            gbr = mk(es, "gbr", [128, 512], F32)
            DM(lambda e: e.dma_start(out=gbr.t[:], in_=gate_b_rep[:, :]), gbr, w=[gbr])
            gnb = load_bc(es, "gnb", ml_norm_g[0:1, :], 512, eng="gpsimd")
            gi = mk(es, "gi", [128, NT, 8], F32)
            for q4 in range(4):
                DM(lambda e, q4=q4: e.dma_start(out=gi.t[:, q4 * 16:(q4 + 1) * 16, :],
                                                in_=mlif.rearrange("(c p) g -> p c g", p=128)[:, q4 * 16:(q4 + 1) * 16, :]),
                   gi, w=[gi])
            V(lambda e: e.tensor_tensor(out=gi.t[:], in0=gi.t[:], in1=gbr.t[:].rearrange("p (c g) -> p c g", g=8), op=ALU.add),
              r=[gi, gbr], w=[gi])
            if os.environ.get('KC_STOP') == '1':
                S.flush(); return nc
            lf = mk(es, "lf", [128, NT, 4], F32)
            ipre = mk(es, "ipre", [128, NT, 4], F32)
            V(lambda e: e.tensor_copy(out=ipre.t[:], in_=gi.t[:, :, 0:4]), r=[gi], w=[ipre])
            A(lambda e: e.activation(out=lf.t[:], in_=gi.t[:, :, 4:8], func=AF.Exp, scale=-1.0), r=[gi], w=[lf])
            A(lambda e: e.activation(out=lf.t[:], in_=lf.t[:], func=AF.Ln, bias=onec.t[:, 0:1], scale=1.0), r=[lf, onec], w=[lf])
            V(lambda e: e.tensor_scalar(out=lf.t[:], in0=lf.t[:], scalar1=-1.0, scalar2=None, op0=ALU.mult), r=[lf], w=[lf])
            if os.environ.get('KC_STOP') == '2':
                S.flush(); return nc
            esg = ExitStack()
            pg = [mk(esg, "pg%d" % i, [128, NT * 4], F32, psum=True) for i in range(2)]
            lf2 = lf.t[:].rearrange("p c h -> p (c h)")
            T(lambda e: e.matmul(pg[0].t[:], lhsT=triLE.t[:], rhs=lf2, start=True, stop=True), r=[triLE, lf], w=[pg[0]])
            T(lambda e: e.matmul(pg[1].t[:], lhsT=onesf.t[:], rhs=lf2, start=True, stop=True), r=[onesf, lf], w=[pg[1]])
            av = mk(es, "av", [128, NT * 4], F32)
            civ = mk(es, "civ", [128, NT * 4], F32)
            wkv = mk(es, "wkv", [128, NT * 4], F32)
            dec = mk(es, "dec", [128, NT * 4], F32)
            ip2 = ipre.t[:].rearrange("p c h -> p (c h)")
            if os.environ.get('KC_STOP') == '3':
                S.flush(); return nc
            bsb = mk(es, "bsb", [128, NT * 4], F32)
            blsb = mk(es, "blsb", [128, NT * 4], F32)
            A(lambda e: e.activation(out=bsb.t[:], in_=pg[0].t[:], func=AF.Copy), r=[pg[0]], w=[bsb])
            A(lambda e: e.activation(out=blsb.t[:], in_=pg[1].t[:], func=AF.Copy), r=[pg[1]], w=[blsb])
            A(lambda e: e.activation(out=av.t[:], in_=bsb.t[:], func=AF.Exp), r=[bsb], w=[av])
            A(lambda e: e.activation(out=dec.t[:], in_=blsb.t[:], func=AF.Exp), r=[blsb], w=[dec])
            V(lambda e: e.tensor_tensor(out=civ.t[:], in0=ip2, in1=bsb.t[:], op=ALU.subtract), r=[ipre, bsb], w=[civ])
            V(lambda e: e.tensor_tensor(out=wkv.t[:], in0=civ.t[:], in1=blsb.t[:], op=ALU.add), r=[civ, blsb], w=[wkv])
            A(lambda e: e.activation(out=wkv.t[:], in_=wkv.t[:], func=AF.Exp), r=[wkv], w=[wkv])
            S.flush()
            esg.close()
            if os.environ.get('KC_STOP') == '4':
                return nc

            qTt = [mk(es, "qTt%d" % i, [128, 4, 512], BF16) for i in range(2)]
            kTt = [mk(es, "kTt%d" % i, [128, 4, 512], BF16) for i in range(2)]
            vp = [mk(es, "vp%d" % i, [128, 4, 130], BF16) for i in range(2)]
            sgo = [mk(es, "sgo%d" % i, [128, 512], F32) for i in range(2)]
            mot = [mk(es, "mot%d" % i, [128, 512], BF16) for i in range(2)]
            for i in range(2):
                G(lambda e, i=i: e.memset(vp[i].t[:], 1.0), w=[vp[i]])
            trl = [mk(es, "trl%d" % i, [128, 128], F32) for i in range(2)]
            Dt = [mk(es, "Dt%d" % i, [128, 128], F32) for i in range(2)]
            Wt = [mk(es, "Wt%d" % i, [128, 128], BF16) for i in range(4)]
            ktok = [mk(es, "ktok%d" % i, [128, 128], BF16) for i in range(4)]
            vw = [mk(es, "vw%d" % i, [128, 130], BF16) for i in range(4)]
            Hs = [mk(es, "Hs%d" % i, [128, 130], F32) for i in range(2)]
            num = [mk(es, "num%d" % i, [128, 130], F32) for i in range(2)]
            rr = [mk(es, "rr%d" % i, [128, 2], F32) for i in range(2)]
            hsc = [mk(es, "hsc%d" % i, [128, 128], F32) for i in range(2)]
            hmv = [mk(es, "hmv%d" % i, [128, 4], F32) for i in range(2)]
            hscr = mk(es, "hscr", [128, 6], F32)
            S32 = [mk(es, "S32_%d" % i, [128, 130], F32) for i in range(4)]
            Sbf = [mk(es, "Sbf%d" % i, [128, 130], BF16) for i in range(4)]
            pST = [mk(es, "pST%d" % i, [128, 128], F32, psum=True) for i in range(2)]
            pB = [mk(es, "pB%d" % i, [128, 128], F32, psum=True) for i in range(2)]
            pK = mk(es, "pK", [128, 128], BF16, psum=True)
            pHa = mk(es, "pHa", [128, 130], F32, psum=True)
            pHb = mk(es, "pHb", [128, 130], F32, psum=True)
            pU = mk(es, "pU", [128, 130], F32, psum=True)
            mlqv = mlqT.rearrange("(h k) t -> k h t", k=128)
            mlkv = mlkT.rearrange("(h k) t -> k h t", k=128)
            u_ = 0
            for c in range(int(os.environ.get('KC_NT', str(NT)))):
                g4 = c // 4
                qq = qTt[g4 % 2]; kk = kTt[g4 % 2]
                if c % 4 == 0:
                    DM(lambda e, qq=qq, g4=g4: e.dma_start(out=qq.t[:], in_=mlqv[:, :, g4 * 512:(g4 + 1) * 512]), qq, w=[qq])
                    DM(lambda e, kk=kk, g4=g4: e.dma_start(out=kk.t[:], in_=mlkv[:, :, g4 * 512:(g4 + 1) * 512]), kk, w=[kk])
                vv = vp[c % 2]; so = sgo[c % 2]; mo = mot[c % 2]
                DM(lambda e, vv=vv, c=c: e.dma_start(out=vv.t[:, :, 0:128],
                                                     in_=mlv[c * 128:(c + 1) * 128, :].rearrange("p (h d) -> p h d", d=128)),
                   vv, w=[vv], eng="gpsimd")
                DM(lambda e, so=so, c=c: e.dma_start(out=so.t[:], in_=mlo[c * 128:(c + 1) * 128, :]), so, w=[so], eng="gpsimd")
                t0 = (c % 4) * 128
                for hh in range(8):
                    h = hh % 4
                    back = hh >= 4
                    b2 = h % 2
                    col = c * 4 + h
                    kslc = lambda kk=kk, h=h, t0=t0: kk.t[:, h, t0:t0 + 128]
                    qslc = lambda qq=qq, h=h, t0=t0: qq.t[:, h, t0:t0 + 128]
                    ps = pST[b2]; pb = pB[b2]; tl = trl[b2]; dt_ = Dt[b2]; wt = Wt[h]; kt_ = ktok[h]; vw_ = vw[h]
                    hs = Hs[b2]; nm = num[b2]; r_ = rr[b2]; hc = hsc[b2]; hm = hmv[b2]
                    if not back:
                        phase_c_front(T, V, A, G, ps, pb, tl, dt_, wt, kt_, vw_, kslc, qslc, kk, qq, vv, h, col, triLE, lf, lf2, onesf,
                                      identf, identb, negm, civ, wkv, pK)
                        continue
                    T(lambda e, wt=wt, vv=vv, h=h: e.matmul(pHa.t[:, 0:129], lhsT=wt.t[:], rhs=vv.t[:, h, 0:129],
                                                            start=True, stop=True), r=[wt, vv], w=[pHa])
                    if c > 0:
                        T(lambda e, qslc=qslc, h=h: e.matmul(pHb.t[:, 0:129], lhsT=qslc(), rhs=Sbf[h].t[:, 0:129],
                                                             start=True, stop=True), r=[qq, Sbf[h]], w=[pHb])
                        A(lambda e, hs=hs, col=col: e.activation(out=hs.t[:, 0:129], in_=pHb.t[:, 0:129], func=AF.Identity,
                                                                 scale=av.t[:, col:col + 1]), r=[pHb, av], w=[hs])
                        V(lambda e, nm=nm, hs=hs: e.tensor_tensor(out=nm.t[:, 0:129], in0=pHa.t[:, 0:129], in1=hs.t[:, 0:129],
                                                                  op=ALU.add), r=[hs, pHa], w=[nm])
                    else:
                        V(lambda e, nm=nm: e.tensor_copy(out=nm.t[:, 0:129], in_=pHa.t[:, 0:129]), r=[pHa], w=[nm])
                    T(lambda e, kt_=kt_, vw_=vw_: e.matmul(pU.t[:, 0:129], lhsT=kt_.t[:], rhs=vw_.t[:, 0:129],
                                                           start=True, stop=True), r=[kt_, vw_], w=[pU])
                    V(lambda e, nm=nm, r_=r_: e.tensor_scalar(out=r_.t[:, 0:1], in0=nm.t[:, 128:129], scalar1=-1.0,
                                                              scalar2=nm.t[:, 128:129], op0=ALU.mult, op1=ALU.max),
                      r=[nm], w=[r_])
                    V(lambda e, r_=r_: e.tensor_scalar_max(out=r_.t[:, 0:1], in0=r_.t[:, 0:1], scalar1=1.0), r=[r_], w=[r_])
                    V(lambda e, r_=r_: e.reciprocal(out=r_.t[:, 1:2], in_=r_.t[:, 0:1]), r=[r_], w=[r_])
                    V(lambda e, hc=hc, nm=nm, r_=r_: e.tensor_scalar(out=hc.t[:], in0=nm.t[:, 0:128], scalar1=r_.t[:, 1:2],
                                                                     scalar2=None, op0=ALU.mult), r=[nm, r_], w=[hc])
                    layer_norm_stats(hc, hm, hscr, 128)
                    V(lambda e, hc=hc, hm=hm: e.tensor_scalar(out=hc.t[:], in0=hc.t[:], scalar1=hm.t[:, 0:1], scalar2=hm.t[:, 1:2],
                                                              op0=ALU.subtract, op1=ALU.mult), r=[hc, hm], w=[hc])
                    G(lambda e, hc=hc, h=h: e.tensor_tensor(out=hc.t[:], in0=hc.t[:], in1=gnb.t[:, h * 128:(h + 1) * 128],
                                                            op=ALU.mult), r=[hc, gnb], w=[hc])
                    G(lambda e, hc=hc, h=h, so=so, mo=mo: e.tensor_tensor(out=mo.t[:, h * 128:(h + 1) * 128], in0=hc.t[:],
                                                                          in1=so.t[:, h * 128:(h + 1) * 128], op=ALU.mult),
                      r=[hc, so], w=[mo])
                    if c == 0:
                        V(lambda e, h=h: e.tensor_copy(out=S32[h].t[:, 0:129], in_=pU.t[:, 0:129]), r=[pU], w=[S32[h]])
                    else:
                        V(lambda e, h=h, col=col: e.scalar_tensor_tensor(out=S32[h].t[:, 0:129], in0=S32[h].t[:, 0:129],
                                                                         scalar=dec.t[:, col:col + 1], in1=pU.t[:, 0:129],
                                                                         op0=ALU.mult, op1=ALU.add),
                          r=[S32[h], dec, pU], w=[S32[h]])
                    A(lambda e, h=h: e.activation(out=Sbf[h].t[:, 0:129], in_=S32[h].t[:, 0:129], func=AF.Copy),
                      r=[S32[h]], w=[Sbf[h]])
                DM(lambda e, mo=mo, c=c: e.dma_start(out=mlout[c * 128:(c + 1) * 128, :], in_=mo.t[:]), mo, r=[mo])
            S.flush()
        if stop_after == 3:
            return nc

        with ExitStack() as es:
            woutb = mk(es, "woutb", [128, 8, D], BF16)
            wrt = mk(es, "wrt", [128, 8, NE], F32)
            DM(lambda e: e.dma_start(out=wrt.t[:], in_=w_router.rearrange("(kt k) c -> k kt c", k=128)), wrt, w=[wrt])
            with ExitStack() as es2:
                wst = [mk(es2, "wost%d" % i, [128, D], F32) for i in range(2)]
                wov = w_out.rearrange("(kt k) c -> k kt c", k=128)
                for kt in range(8):
                    w = wst[kt % 2]
                    DM(lambda e, w=w, kt=kt: e.dma_start(out=w.t[:], in_=wov[:, kt, :]), w, w=[w])
                    V(lambda e, w=w, kt=kt: e.tensor_copy(out=woutb.t[:, kt, :], in_=w.t[:]), r=[w], w=[woutb])
                S.flush()
            g1b = load_bc(es, "g1b", mod_s[0:1, 2 * D:3 * D], D)
            sh2 = load_bc(es, "sh2", mod_s[0:1, 3 * D:4 * D], D, eng="gpsimd")
            sc2p = load_bc(es, "sc2p", mod_s[0:1, 4 * D:5 * D], D)
            l1g = load_bc(es, "l1g", ln1_g[0:1, :], D, eng="gpsimd")
            l1b = load_bc(es, "l1b", ln1_b[0:1, :], D)
            rbb = load_bc(es, "rbb", router_bias[0:1, :], NE, eng="gpsimd")
            sA = [mk(es, "sA%d" % i, [128, 4, 128], BF16) for i in range(2)]
            sB = [mk(es, "sB%d" % i, [128, 4, 128], BF16) for i in range(2)]
            mA = [mk(es, "mA%d" % i, [128, 512], BF16) for i in range(2)]
            mB = [mk(es, "mB%d" % i, [128, 512], BF16) for i in range(2)]
            ssel = [mk(es, "ssel%d" % i, [128, 4, 128], BF16) for i in range(2)]
            msel = [mk(es, "msel%d" % i, [128, 512], BF16) for i in range(2)]
            mlT = [mk(es, "mlT%d" % i, [128, 4, 128], BF16) for i in range(2)]
            xo = [mk(es, "xo%d" % i, [128, D], F32) for i in range(2)]
            tt = [mk(es, "tt%d" % i, [128, D], F32) for i in range(2)]
            x1 = [mk(es, "x1_%d" % i, [128, D], F32) for i in range(2)]
            u2 = [mk(es, "u2_%d" % i, [128, D], F32) for i in range(2)]
            u2b = [mk(es, "u2b%d" % i, [128, D], BF16) for i in range(2)]
            u2T = [mk(es, "u2T%d" % i, [128, 8, 128], BF16) for i in range(2)]
            u2Tf = [mk(es, "u2Tf%d" % i, [128, 8, 128], F32) for i in range(2)]
            scr = mk(es, "scrD", [128, 12], F32)
            mvd = [mk(es, "mvd%d" % i, [128, 4], F32) for i in range(4)]
            scs = [mk(es, "scs%d" % i, [128, NE], F32) for i in range(2)]
            sel = [mk(es, "sel%d" % i, [128, NE], F32) for i in range(2)]
            selm = [mk(es, "selm%d" % i, [128, NE], F32) for i in range(2)]
            m8 = [mk(es, "m8_%d" % i, [128, 8, 8], F32) for i in range(2)]
            grp = [mk(es, "grp%d" % i, [128, 8], F32) for i in range(2)]
            gs8 = [mk(es, "gs8_%d" % i, [128, 8], F32) for i in range(2)]
            gmk = [mk(es, "gmk%d" % i, [128, 8], F32) for i in range(2)]
            t8 = [mk(es, "t8_%d" % i, [128, 8], F32) for i in range(2)]
            dn = [mk(es, "dn%d" % i, [128, 2], F32) for i in range(2)]
            gt = [mk(es, "gt%d" % i, [128, NE + 1], F32) for i in range(2)]
            for i in range(2):
                G(lambda e, i=i: e.memset(gt[i].t[:, NE:NE + 1], 1.0), w=[gt[i]])
            pM = [mk(es, "pM%d" % i, [128, 512], F32, psum=True) for i in range(2)]
            pT2 = mk(es, "pT2", [128, 8, 128], BF16, psum=True)
            pTf = [mk(es, "pTf%d" % i, [128, 4, 128], F32, psum=True) for i in range(2)]
            pT = mk(es, "pTd", [128, 4, 128], BF16, psum=True)
            pR = mk(es, "pR", [128, NE], F32, psum=True)
            sbov = sbo.rearrange("(ft f) t -> f ft t", f=128)
            for i in range(NOWN // 128):
                b2 = i % 2
                a_ = sA[b2]; b_ = sB[b2]; ma = mA[b2]; mb = mB[b2]; ss = ssel[b2]; ms = msel[b2]; mt = mlT[b2]
                x = xo[b2]; t_ = tt[b2]; x1_ = x1[b2]; u_2 = u2[b2]; ub_ = u2b[b2]; uT_ = u2T[b2]; uTf = u2Tf[b2]
                DM(lambda e, a_=a_, i=i: e.dma_start(out=a_.t[:], in_=sbov[:, :, i * 128:(i + 1) * 128]), a_, w=[a_])
                rA = (8 * (i // 4) + (i % 4)) * 128
                rB = rA + 512
                DM(lambda e, ma=ma, rA=rA: e.dma_start(out=ma.t[:], in_=mlout[rA:rA + 128, :]), ma, w=[ma], eng="gpsimd")
                DM(lambda e, mb=mb, rB=rB: e.dma_start(out=mb.t[:], in_=mlout[rB:rB + 128, :]), mb, w=[mb], eng="gpsimd")
                DM(lambda e, x=x, i=i: e.dma_start(out=x.t[:], in_=x_own[i * 128:(i + 1) * 128, :]), x, w=[x])
                a2 = a_.t[:].rearrange("p a b -> p (a b)"); b2_ = b_.t[:].rearrange("p a b -> p (a b)")
                s2 = ss.t[:].rearrange("p a b -> p (a b)")
                ss = a_
                G(lambda e, mb=mb: e.tensor_scalar(out=mb.t[:], in0=mb.t[:], scalar1=flag.t[:, 0:1], scalar2=None, op0=ALU.mult),
                  r=[mb, flag], w=[mb])
                V(lambda e, ms=ms, ma=ma, mb=mb: e.scalar_tensor_tensor(out=ms.t[:], in0=ma.t[:], scalar=flag.t[:, 1:2], in1=mb.t[:],
                                                                       op0=ALU.mult, op1=ALU.add), r=[ma, mb, flag], w=[ms])
                for k in range(4):
                    T(lambda e, ms=ms, k=k: e.transpose(out=pT.t[:, k, :], in_=ms.t[:, k * 128:(k + 1) * 128], identity=identb.t[:]),
                      r=[ms, identb], w=[pT], inc=(k == 3))
                A(lambda e, mt=mt: e.activation(out=mt.t[:], in_=pT.t[:], func=AF.Copy), r=[pT], w=[mt])
                for ch in range(2):
                    p = pM[ch]
                    for ft in range(8):
                        src = ss if ft < 4 else mt
                        T(lambda e, p=p, src=src, ft=ft, ch=ch: e.matmul(p.t[:], lhsT=src.t[:, ft % 4, :],
                                                                         rhs=woutb.t[:, ft, ch * 512:(ch + 1) * 512],
                                                                         start=(ft == 0), stop=(ft == 7)),
                          r=[src, woutb], w=[p], inc=(ft == 7))
                    V(lambda e, p=p, t_=t_, ch=ch: e.tensor_tensor(out=t_.t[:, ch * 512:(ch + 1) * 512], in0=p.t[:],
                                                                   in1=g1b.t[:, ch * 512:(ch + 1) * 512], op=ALU.mult),
                      r=[p, g1b], w=[t_])
                V(lambda e, t_=t_, x=x: e.scalar_tensor_tensor(out=t_.t[:], in0=x.t[:], scalar=ALPHA, in1=t_.t[:],
                                                               op0=ALU.mult, op1=ALU.add), r=[x, t_], w=[t_])
                m1 = mvd[(2 * i) % 4]; m2 = mvd[(2 * i + 1) % 4]
                layer_norm_stats(t_, m1, scr, D)
                V(lambda e, t_=t_, m1=m1: e.tensor_scalar(out=t_.t[:], in0=t_.t[:], scalar1=m1.t[:, 0:1], scalar2=m1.t[:, 1:2],
                                                          op0=ALU.subtract, op1=ALU.mult), r=[t_, m1], w=[t_])
                G(lambda e, t_=t_: e.tensor_tensor(out=t_.t[:], in0=t_.t[:], in1=l1g.t[:], op=ALU.mult), r=[t_, l1g], w=[t_])
                V(lambda e, t_=t_, x1_=x1_: e.tensor_tensor(out=x1_.t[:], in0=t_.t[:], in1=l1b.t[:], op=ALU.add),
                  r=[t_, l1b], w=[x1_])
                DM(lambda e, x1_=x1_, i=i: e.dma_start(out=x1s[i * 128:(i + 1) * 128, :], in_=x1_.t[:]), x1_, r=[x1_], eng="scalar")
                layer_norm_stats(x1_, m2, scr, D)
                V(lambda e, u_2=u_2, x1_=x1_, m2=m2: e.tensor_scalar(out=u_2.t[:], in0=x1_.t[:], scalar1=m2.t[:, 0:1],
                                                                     scalar2=m2.t[:, 1:2], op0=ALU.subtract, op1=ALU.mult),
                  r=[x1_, m2], w=[u_2])
                G(lambda e, u_2=u_2: e.tensor_tensor(out=u_2.t[:], in0=u_2.t[:], in1=sc2p.t[:], op=ALU.mult), r=[u_2, sc2p], w=[u_2])
                V(lambda e, u_2=u_2: e.tensor_tensor(out=u_2.t[:], in0=u_2.t[:], in1=sh2.t[:], op=ALU.add), r=[u_2, sh2], w=[u_2])
                G(lambda e, u_2=u_2, ub_=ub_: e.tensor_copy(out=ub_.t[:], in_=u_2.t[:]), r=[u_2], w=[ub_])
                DM(lambda e, ub_=ub_, i=i: e.dma_start(out=u2s[i * 128:(i + 1) * 128, :], in_=ub_.t[:]), ub_, r=[ub_], eng="scalar")
                for kt in range(8):
                    T(lambda e, ub_=ub_, kt=kt: e.transpose(out=pT2.t[:, kt, :], in_=ub_.t[:, kt * 128:(kt + 1) * 128],
                                                            identity=identb.t[:]), r=[ub_, identb], w=[pT2], inc=(kt == 7))
                A(lambda e, uT_=uT_: e.activation(out=uT_.t[:], in_=pT2.t[:], func=AF.Copy), r=[pT2], w=[uT_])
                DM(lambda e, uT_=uT_, i=i: e.dma_start(out=u2Ts[:, :, i * 128:(i + 1) * 128], in_=uT_.t[:]), uT_, r=[uT_],
                   eng="gpsimd")
                for hf in range(2):
                    pf = pTf[hf]
                    for k in range(4):
                        kt = hf * 4 + k
                        T(lambda e, pf=pf, u_2=u_2, k=k, kt=kt: e.transpose(out=pf.t[:, k, :], in_=u_2.t[:, kt * 128:(kt + 1) * 128],
                                                                           identity=identf.t[:]),
                          r=[u_2, identf], w=[pf], inc=(k == 3))
                    A(lambda e, pf=pf, uTf=uTf, hf=hf: e.activation(out=uTf.t[:, hf * 4:(hf + 1) * 4, :], in_=pf.t[:], func=AF.Copy),
                      r=[pf], w=[uTf])
                for kt in range(8):
                    T(lambda e, uTf=uTf, kt=kt: e.matmul(pR.t[:], lhsT=uTf.t[:, kt, :], rhs=wrt.t[:, kt, :],
                                                         start=(kt == 0), stop=(kt == 7)), r=[uTf, wrt], w=[pR], inc=(kt == 7))
                sc_ = scs[b2]; sl_ = sel[b2]; sm_ = selm[b2]; m8_ = m8[b2]; gr_ = grp[b2]; g8_ = gs8[b2]; gm_ = gmk[b2]
                t8_ = t8[b2]; dn_ = dn[b2]; gt_ = gt[b2]
                A(lambda e, sc_=sc_: e.activation(out=sc_.t[:], in_=pR.t[:], func=AF.Sigmoid), r=[pR], w=[sc_])
                V(lambda e, sc_=sc_, sl_=sl_: e.tensor_tensor(out=sl_.t[:], in0=sc_.t[:], in1=rbb.t[:], op=ALU.add),
                  r=[sc_, rbb], w=[sl_])
                for g in range(8):
                    V(lambda e, sl_=sl_, m8_=m8_, g=g: e.max(out=m8_.t[:, g, :], in_=sl_.t[:, g * 32:(g + 1) * 32]), r=[sl_], w=[m8_])
                V(lambda e, m8_=m8_, gr_=gr_: e.tensor_tensor(out=gr_.t[:], in0=m8_.t[:, :, 0], in1=m8_.t[:, :, 1], op=ALU.add),
                  r=[m8_], w=[gr_])
                V(lambda e, gr_=gr_, g8_=g8_: e.max(out=g8_.t[:], in_=gr_.t[:]), r=[gr_], w=[g8_])
                V(lambda e, gr_=gr_, g8_=g8_, gm_=gm_: e.tensor_scalar(out=gm_.t[:], in0=gr_.t[:], scalar1=g8_.t[:, 3:4], scalar2=None,
                                                                       op0=ALU.is_ge), r=[gr_, g8_], w=[gm_])
                for g in range(8):
                    V(lambda e, sl_=sl_, sm_=sm_, gm_=gm_, g=g: e.tensor_scalar(out=sm_.t[:, g * 32:(g + 1) * 32],
                                                                               in0=sl_.t[:, g * 32:(g + 1) * 32], scalar1=2.0,
                                                                               scalar2=gm_.t[:, g:g + 1], op0=ALU.add, op1=ALU.mult),
                      r=[sl_, gm_], w=[sm_])
                V(lambda e, sm_=sm_, t8_=t8_: e.max(out=t8_.t[:], in_=sm_.t[:]), r=[sm_], w=[t8_])
                V(lambda e, sm_=sm_, t8_=t8_: e.tensor_scalar(out=sm_.t[:], in0=sm_.t[:], scalar1=t8_.t[:, 7:8], scalar2=None,
                                                              op0=ALU.is_ge), r=[sm_, t8_], w=[sm_])
                G(lambda e, sm_=sm_, i=i: e.tensor_copy(out=Mall.t[:, i, :], in_=sm_.t[:]), r=[sm_], w=[Mall])
                V(lambda e, sm_=sm_, sc_=sc_: e.tensor_tensor(out=sm_.t[:], in0=sm_.t[:], in1=sc_.t[:], op=ALU.mult),
                  r=[sm_, sc_], w=[sm_])
                V(lambda e, sm_=sm_, dn_=dn_: e.reduce_sum(out=dn_.t[:, 0:1], in_=sm_.t[:], axis=mybir.AxisListType.X),
                  r=[sm_], w=[dn_])
                V(lambda e, dn_=dn_: e.reciprocal(out=dn_.t[:, 1:2], in_=dn_.t[:, 0:1]), r=[dn_], w=[dn_])
                V(lambda e, sm_=sm_, dn_=dn_, gt_=gt_: e.tensor_scalar(out=gt_.t[:, 0:NE], in0=sm_.t[:], scalar1=dn_.t[:, 1:2],
                                                                       scalar2=2.5, op0=ALU.mult, op1=ALU.mult),
                  r=[sm_, dn_], w=[gt_])
                DM(lambda e, gt_=gt_, i=i: e.dma_start(out=gates_s[:, i, :], in_=gt_.t[:]), gt_, r=[gt_], eng="scalar")
            S.flush()
        if stop_after == 4:
            return nc

        if sparse:
            phase_e_sparse(nc, S, mk, V, A, G, T, DM, load_bc, layer_norm_stats, Mall, identb, identf, onesf, onesb,
                           dict(mod_s=mod_s, ln2_g=ln2_g, ln2_b=ln2_b, gates_s=gates_s, u2s=u2s, u2Ts=u2Ts, x1s=x1s, Xs=Xs, Ys=Ys,
                                moe_w1=moe_w1, moe_w3=moe_w3, moe_w2=moe_w2, out=out), n_decl)
            return nc
        with ExitStack() as es:
            NP_TOK = 1024
            g2b = load_bc(es, "g2b", mod_s[0:1, 5 * D:6 * D], D)
            l2g = load_bc(es, "l2g", ln2_g[0:1, :], D, eng="gpsimd")
            l2b = load_bc(es, "l2b", ln2_b[0:1, :], D)
            uTp = mk(es, "uTp", [128, 8, NP_TOK], BF16)
            gtp = mk(es, "gtp", [128, 8, NE + 1], F32)
            yacc = mk(es, "yacc", [128, 8, D], F32)
            st1 = mk(es, "st1", [128, 8, 256], F32)
            st3 = mk(es, "st3", [128, 8, 256], F32)
            st2 = mk(es, "st2", [128, 2, D], F32)
            w1b = [mk(es, "w1b%d" % i, [128, 8, 256], BF16) for i in range(2)]
            w3b = [mk(es, "w3b%d" % i, [128, 8, 256], BF16) for i in range(2)]
            w2b = [mk(es, "w2b%d" % i, [128, 2, D], BF16) for i in range(2)]
            s1 = [mk(es, "s1_%d" % i, [128, 512], F32) for i in range(4)]
            hT = [mk(es, "hT%d" % i, [128, 512], BF16) for i in range(4)]
            ph1 = [mk(es, "ph1_%d" % i, [128, 512], F32, psum=True) for i in range(2)]
            ph3 = [mk(es, "ph3_%d" % i, [128, 512], F32, psum=True) for i in range(2)]
            py = [[mk(es, "py%d_%d" % (i, j), [128, 512], F32, psum=True) for j in range(2)] for i in range(2)]
            xl = [mk(es, "xl%d" % i, [128, D], F32) for i in range(2)]
            ot = [mk(es, "ot%d" % i, [128, D], F32) for i in range(2)]
            mve = [mk(es, "mve%d" % i, [128, 4], F32) for i in range(2)]
            scr = mk(es, "scrE", [128, 12], F32)
            w1v = moe_w1.rearrange("e (kt k) f -> e k kt f", k=128)
            w3v = moe_w3.rearrange("e (kt k) f -> e k kt f", k=128)
            w2v = moe_w2.rearrange("e (ft f) d -> e f ft d", f=128)
            nh = 0
            nyy = 0
            for ps_ in range(NOWN // NP_TOK):
                tok0 = ps_ * NP_TOK
                DM(lambda e, tok0=tok0: e.dma_start(out=uTp.t[:], in_=u2Ts[:, :, tok0:tok0 + NP_TOK]), uTp, w=[uTp])
                DM(lambda e, ps_=ps_: e.dma_start(out=gtp.t[:], in_=gates_s[:, ps_ * 8:(ps_ + 1) * 8, :]), gtp, w=[gtp], eng="gpsimd")
                for ei in range(n_exp):
                    ex = ei if ei < n_exp - 1 else NE
                    exw = ei if ei < n_exp - 1 else n_decl - 1
                    eb = ei % 2
                    a1 = w1b[eb]; a3 = w3b[eb]; a2 = w2b[eb]
                    DM(lambda e, exw=exw: e.dma_start(out=st1.t[:], in_=w1v[exw]), st1, w=[st1])
                    DM(lambda e, exw=exw: e.dma_start(out=st3.t[:], in_=w3v[exw]), st3, w=[st3], eng="gpsimd")
                    DM(lambda e, exw=exw: e.dma_start(out=st2.t[:], in_=w2v[exw]), st2, w=[st2])
                    G(lambda e, a1=a1: e.tensor_copy(out=a1.t[:], in_=st1.t[:]), r=[st1], w=[a1])
                    G(lambda e, a3=a3: e.tensor_copy(out=a3.t[:], in_=st3.t[:]), r=[st3], w=[a3])
                    G(lambda e, a2=a2: e.tensor_copy(out=a2.t[:], in_=st2.t[:]), r=[st2], w=[a2])
                    for tb in range(NP_TOK // 512):
                        hts = []
                        for ft in range(2):
                            p1 = ph1[ft]; p3 = ph3[ft]
                            for kt in range(8):
                                T(lambda e, p1=p1, a1=a1, kt=kt, ft=ft, tb=tb: e.matmul(
                                    p1.t[:], lhsT=a1.t[:, kt, ft * 128:(ft + 1) * 128], rhs=uTp.t[:, kt, tb * 512:(tb + 1) * 512],
                                    start=(kt == 0), stop=(kt == 7)), r=[a1, uTp], w=[p1], inc=(kt == 7))
                            for kt in range(8):
                                T(lambda e, p3=p3, a3=a3, kt=kt, ft=ft, tb=tb: e.matmul(
                                    p3.t[:], lhsT=a3.t[:, kt, ft * 128:(ft + 1) * 128], rhs=uTp.t[:, kt, tb * 512:(tb + 1) * 512],
                                    start=(kt == 0), stop=(kt == 7)), r=[a3, uTp], w=[p3], inc=(kt == 7))
                            s_ = s1[nh % 4]; h_ = hT[nh % 4]; nh += 1
                            A(lambda e, p1=p1, s_=s_: e.activation(out=s_.t[:], in_=p1.t[:], func=AF.Silu), r=[p1], w=[s_])
                            V(lambda e, s_=s_, p3=p3, h_=h_: e.tensor_tensor(out=h_.t[:], in0=p3.t[:], in1=s_.t[:], op=ALU.mult),
                              r=[s_, p3], w=[h_])
                            hts.append(h_)
                        for tq in range(4):
                            tl = tb * 4 + tq
                            pp = py[nyy % 2]; nyy += 1
                            for dc in range(2):
                                for ft in range(2):
                                    T(lambda e, pp=pp, dc=dc, ft=ft, tq=tq, hts=hts, a2=a2: e.matmul(
                                        pp[dc].t[:], lhsT=hts[ft].t[:, tq * 128:(tq + 1) * 128],
                                        rhs=a2.t[:, ft, dc * 512:(dc + 1) * 512], start=(ft == 0), stop=(ft == 1)),
                                      r=[hts[ft], a2], w=[pp[dc]], inc=(ft == 1))
                                if ei == 0:
                                    V(lambda e, pp=pp, dc=dc, tl=tl, ex=ex: e.tensor_scalar(
                                        out=yacc.t[:, tl, dc * 512:(dc + 1) * 512], in0=pp[dc].t[:],
                                        scalar1=gtp.t[:, tl, ex:ex + 1], scalar2=None, op0=ALU.mult),
                                      r=[pp[dc], gtp], w=[yacc])
                                else:
                                    V(lambda e, pp=pp, dc=dc, tl=tl, ex=ex: e.scalar_tensor_tensor(
                                        out=yacc.t[:, tl, dc * 512:(dc + 1) * 512], in0=pp[dc].t[:],
                                        scalar=gtp.t[:, tl, ex:ex + 1], in1=yacc.t[:, tl, dc * 512:(dc + 1) * 512],
                                        op0=ALU.mult, op1=ALU.add), r=[pp[dc], gtp, yacc], w=[yacc])
                    if ei % 32 == 31:
                        S.flush()
                for tl in range(8):
                    r0 = tok0 + tl * 128
                    xl_ = xl[tl % 2]; o_ = ot[tl % 2]; m_ = mve[tl % 2]
                    DM(lambda e, xl_=xl_, r0=r0: e.dma_start(out=xl_.t[:], in_=x1s[r0:r0 + 128, :]), xl_, w=[xl_])
                    G(lambda e, tl=tl: e.tensor_tensor(out=yacc.t[:, tl, :], in0=yacc.t[:, tl, :], in1=g2b.t[:], op=ALU.mult),
                      r=[yacc, g2b], w=[yacc])
                    V(lambda e, tl=tl, xl_=xl_: e.scalar_tensor_tensor(out=xl_.t[:], in0=xl_.t[:], scalar=ALPHA, in1=yacc.t[:, tl, :],
                                                                       op0=ALU.mult, op1=ALU.add), r=[xl_, yacc], w=[xl_])
                    layer_norm_stats(xl_, m_, scr, D)
                    V(lambda e, xl_=xl_, m_=m_: e.tensor_scalar(out=xl_.t[:], in0=xl_.t[:], scalar1=m_.t[:, 0:1], scalar2=m_.t[:, 1:2],
                                                                op0=ALU.subtract, op1=ALU.mult), r=[xl_, m_], w=[xl_])
                    G(lambda e, xl_=xl_: e.tensor_tensor(out=xl_.t[:], in0=xl_.t[:], in1=l2g.t[:], op=ALU.mult), r=[xl_, l2g], w=[xl_])
                    V(lambda e, xl_=xl_, o_=o_: e.tensor_tensor(out=o_.t[:], in0=xl_.t[:], in1=l2b.t[:], op=ALU.add),
                      r=[xl_, l2b], w=[o_])
                    DM(lambda e, o_=o_, r0=r0: e.dma_start(out=out[r0:r0 + 128, :], in_=o_.t[:]), o_, r=[o_])
            S.flush()
    return nc


def phase_c_front(T, V, A, G, ps, pb, tl, dt_, wt, kt_, vw_, kslc, qslc, kk, qq, vv, h, col, triLE, lf, lf2, onesf, identf, identb,
                  negm, civ, wkv, pK):
    T(lambda e, ps=ps, kslc=kslc, qslc=qslc: e.matmul(ps.t[:], lhsT=kslc(), rhs=qslc(), start=True, stop=True),
      r=[kk, qq], w=[ps])
    V(lambda e, tl=tl, col=col: e.tensor_scalar(out=tl.t[:], in0=triLE.t[:], scalar1=lf2[:, col:col + 1],
                                                scalar2=None, op0=ALU.mult), r=[triLE, lf], w=[tl])
    T(lambda e, pb=pb, tl=tl: e.matmul(pb.t[:], lhsT=onesf.t[:], rhs=tl.t[:], start=True, stop=False),
      r=[onesf, tl], w=[pb], inc=False)
    T(lambda e, pb=pb: e.matmul(pb.t[:], lhsT=identf.t[:], rhs=negm.t[:], start=False, stop=True),
      r=[identf, negm], w=[pb])
    A(lambda e, pb=pb, dt_=dt_, col=col: e.activation(out=dt_.t[:], in_=pb.t[:], func=AF.Exp,
                                                     bias=civ.t[:, col:col + 1], scale=1.0),
      r=[pb, civ], w=[dt_])
    V(lambda e, wt=wt, ps=ps, dt_=dt_: e.tensor_tensor(out=wt.t[:], in0=ps.t[:], in1=dt_.t[:], op=ALU.mult),
      r=[ps, dt_], w=[wt])
    T(lambda e, kslc=kslc: e.transpose(out=pK.t[:], in_=kslc(), identity=identb.t[:]), r=[kk, identb], w=[pK])
    A(lambda e, kt_=kt_: e.activation(out=kt_.t[:], in_=pK.t[:], func=AF.Copy), r=[pK], w=[kt_])
    G(lambda e, vw_=vw_, vv=vv, h=h, col=col: e.tensor_scalar(out=vw_.t[:, 0:129], in0=vv.t[:, h, 0:129],
                                                             scalar1=wkv.t[:, col:col + 1], scalar2=None,
                                                             op0=ALU.mult), r=[vv, wkv], w=[vw_])


def phase_e_sparse(nc, S, mk, V, A, G, T, DM, load_bc, layer_norm_stats, Mall, identb, identf, onesf, onesb, dr, n_decl):
    mod_s = dr["mod_s"]; gates_s = dr["gates_s"]; u2s = dr["u2s"]; u2Ts = dr["u2Ts"]; x1s = dr["x1s"]
    Xs = dr["Xs"]; Ys = dr["Ys"]; out = dr["out"]
    w1r = dr["moe_w1"].rearrange("e k kt f -> (e k) (kt f)")
    w3r = dr["moe_w3"].rearrange("e k kt f -> (e k) (kt f)")
    w2r = dr["moe_w2"].rearrange("e f ft d -> (e f) (ft d)")
    IOA = bass.IndirectOffsetOnAxis
    NT = NOWN // 128
    _breg = []

    def breg(e):
        if not _breg or _breg[0][0] != S.nblocks:
            _breg[:] = [(S.nblocks, e.to_reg(NE * 128 - 1))]
        return _breg[0][1]
    with ExitStack() as es:
        stri = mk(es, "stri", [128, 128], BF16)
        strif = mk(es, "strif", [128, 128], F32)
        G(lambda e: e.memset(strif.t[:], 1.0), w=[strif])
        G(lambda e: e.affine_select(out=strif.t[:], in_=strif.t[:], pattern=[[1, 128]], base=-1, channel_multiplier=-1,
                                     compare_op=ALU.is_ge, fill=0.0), r=[strif], w=[strif])
        V(lambda e: e.tensor_copy(out=stri.t[:], in_=strif.t[:]), r=[strif], w=[stri])
        ut = [mk(es, "ut%d" % h, [128, NE], F32) for h in range(2)]
        for h in range(2):
            G(lambda e, h=h: e.memset(ut[h].t[:], 1.0), w=[ut[h]])
            G(lambda e, h=h: e.affine_select(out=ut[h].t[:], in_=ut[h].t[:], pattern=[[1, NE]], base=-1 - 128 * h,
                                             channel_multiplier=-1, compare_op=ALU.is_ge, fill=0.0), r=[ut[h]], w=[ut[h]])
        iota_e = mk(es, "iota_e", [128, NE], F32)
        G(lambda e: e.iota(iota_e.t[:], pattern=[[1, NE]], base=0, channel_multiplier=0, allow_small_or_imprecise_dtypes=True),
          w=[iota_e])
        iota_p = mk(es, "iota_p", [128, 1], F32)
        G(lambda e: e.iota(iota_p.t[:], pattern=[[0, 1]], base=0, channel_multiplier=1, allow_small_or_imprecise_dtypes=True),
          w=[iota_p])
        bglob = mk(es, "bglob", [128, 4], F32)
        G(lambda e: e.iota(bglob.t[:], pattern=[[128, 4]], base=0, channel_multiplier=1, allow_small_or_imprecise_dtypes=True),
          w=[bglob])
        Pall = mk(es, "Pall", [128, NT, NE], F32)
        cnt = mk(es, "cnt", [128, NE], F32)
        esc = ExitStack()
        pP = [mk(esc, "pP%d" % i, [128, NE], F32, psum=True) for i in range(2)]
        for i in range(NT):
            p = pP[i % 2]
            for j in range(i):
                T(lambda e, p=p, j=j: e.matmul(p.t[:], lhsT=onesb.t[:], rhs=Mall.t[:, j, :], start=(j == 0), stop=False),
                  r=[onesb, Mall], w=[p], inc=False)
            T(lambda e, p=p, i=i: e.matmul(p.t[:], lhsT=stri.t[:], rhs=Mall.t[:, i, :], start=(i == 0), stop=True),
              r=[stri, Mall], w=[p])
            A(lambda e, p=p, i=i: e.activation(out=Pall.t[:, i, :], in_=p.t[:], func=AF.Copy), r=[p], w=[Pall])
        p = pP[0]
        for j in range(NT):
            T(lambda e, p=p, j=j: e.matmul(p.t[:], lhsT=onesb.t[:], rhs=Mall.t[:, j, :], start=(j == 0), stop=(j == NT - 1)),
              r=[onesb, Mall], w=[p], inc=(j == NT - 1))
        V(lambda e, p=p: e.tensor_scalar_add(out=cnt.t[:], in0=p.t[:], scalar1=127.0), r=[p], w=[cnt])
        cnti = mk(es, "cnti", [128, NE], I32)
        nblk = mk(es, "nblk", [128, NE], F32)
        V(lambda e: e.tensor_copy(out=cnti.t[:], in_=cnt.t[:]), r=[cnt], w=[cnti])
        V(lambda e: e.tensor_scalar(out=cnti.t[:], in0=cnti.t[:], scalar1=7, scalar2=None, op0=ALU.arith_shift_right),
          r=[cnti], w=[cnti])
        V(lambda e: e.tensor_copy(out=nblk.t[:], in_=cnti.t[:]), r=[cnti], w=[nblk])
        pTn = mk(esc, "pTn", [128, 2, 128], F32, psum=True)
        for h in range(2):
            T(lambda e, h=h: e.transpose(out=pTn.t[:, h, :], in_=nblk.t[:, h * 128:(h + 1) * 128], identity=identf.t[:]),
              r=[nblk, identf], w=[pTn], inc=(h == 1))
        ncol = mk(es, "ncol", [128, 2], F32)
        V(lambda e: e.tensor_copy(out=ncol.t[:], in_=pTn.t[:, :, 0]), r=[pTn], w=[ncol])
        utn = [mk(es, "utn%d" % h, [128, NE], F32) for h in range(2)]
        for h in range(2):
            V(lambda e, h=h: e.tensor_scalar(out=utn[h].t[:], in0=ut[h].t[:], scalar1=ncol.t[:, h:h + 1], scalar2=None, op0=ALU.mult),
              r=[ut[h], ncol], w=[utn[h]])
        pBo = mk(esc, "pBo", [128, NE], F32, psum=True)
        for h in range(2):
            T(lambda e, h=h: e.matmul(pBo.t[:], lhsT=onesf.t[:], rhs=utn[h].t[:], start=(h == 0), stop=(h == 1)),
              r=[onesf, utn[h]], w=[pBo], inc=(h == 1))
        boff128 = mk(es, "boff128", [128, NE], F32)
        bend = mk(es, "bend", [128, NE], F32)
        V(lambda e: e.tensor_scalar(out=boff128.t[:], in0=pBo.t[:], scalar1=128.0, scalar2=1.0, op0=ALU.mult, op1=ALU.add),
          r=[pBo], w=[boff128])
        V(lambda e: e.tensor_tensor(out=bend.t[:], in0=pBo.t[:], in1=nblk.t[:], op=ALU.add), r=[pBo, nblk], w=[bend])
        becol = mk(es, "becol", [128, 4], F32)
        junk = mk(es, "junkE", [128, NE], F32)
        for bt in range(4):
            V(lambda e, bt=bt: e.tensor_scalar(out=junk.t[:], in0=bend.t[:], scalar1=bglob.t[:, bt:bt + 1], scalar2=None,
                                               op0=ALU.is_le), r=[bend, bglob], w=[junk])
            V(lambda e, bt=bt: e.reduce_sum(out=becol.t[:, bt:bt + 1], in_=junk.t[:], axis=mybir.AxisListType.X), r=[junk], w=[becol])
        dg = mk(es, "dg", [128, 4, 128], F32)
        for bt in range(4):
            V(lambda e, bt=bt: e.tensor_scalar(out=dg.t[:, bt, :], in0=identf.t[:], scalar1=becol.t[:, bt:bt + 1], scalar2=None,
                                               op0=ALU.mult), r=[identf, becol], w=[dg])
        pBe = mk(esc, "pBe", [128, 4, 128], F32, psum=True)
        for bt in range(4):
            T(lambda e, bt=bt: e.matmul(pBe.t[:, bt, :], lhsT=onesf.t[:], rhs=dg.t[:, bt, :], start=True, stop=True),
              r=[onesf, dg], w=[pBe], inc=(bt == 3))
        widf = mk(es, "widf", [128, NBLK], F32)
        widx = mk(es, "widx", [128, NBLK], U32)
        V(lambda e: e.tensor_scalar(out=widf.t[:], in0=pBe.t[:].rearrange("p a b -> p (a b)"), scalar1=128.0,
                                    scalar2=iota_p.t[:, 0:1], op0=ALU.mult, op1=ALU.add), r=[pBe, iota_p], w=[widf])
        bef = mk(es, "bef", [128, NBLK], F32)
        same = mk(es, "same", [128, NBLK], F32)
        V(lambda e: e.tensor_copy(out=bef.t[:], in_=pBe.t[:].rearrange("p a b -> p (a b)")), r=[pBe], w=[bef])
        V(lambda e: e.memset(same.t[:, 0:1], 0.0), w=[same])
        V(lambda e: e.tensor_tensor(out=same.t[:, 1:NBLK], in0=bef.t[:, 1:NBLK], in1=bef.t[:, 0:NBLK - 1], op=ALU.is_equal),
          r=[bef], w=[same])
        V(lambda e: e.scalar_tensor_tensor(out=widf.t[:], in0=same.t[:], scalar=1.0e6, in1=widf.t[:], op0=ALU.mult, op1=ALU.add),
          r=[same, widf], w=[widf])
        V(lambda e: e.tensor_copy(out=widx.t[:], in_=widf.t[:]), r=[widf], w=[widx])
        destU = mk(es, "destU", [128, NT, 8], U32)
        gk = mk(es, "gk", [128, NT, 8], F32)
        Dm = [mk(es, "Dm%d" % i, [128, NE], F32) for i in range(2)]
        dv = [mk(es, "dv%d" % i, [128, 8], F32) for i in range(2)]
        ixu = [mk(es, "ixu%d" % i, [128, 8], U32) for i in range(2)]
        ixf = [mk(es, "ixf%d" % i, [128, 8], F32) for i in range(2)]
        gtl = [mk(es, "gtl%d" % i, [128, NE + 1], F32) for i in range(2)]
        ubt = [mk(es, "ubt%d" % i, [128, D], BF16) for i in range(2)]
        for i in range(NT):
            dm = Dm[i % 2]; dv_ = dv[i % 2]; xu = ixu[i % 2]; xf = ixf[i % 2]; gl = gtl[i % 2]; ub = ubt[i % 2]
            DM(lambda e, gl=gl, i=i: e.dma_start(out=gl.t[:], in_=gates_s[:, i, :]), gl, w=[gl])
            DM(lambda e, ub=ub, i=i: e.dma_start(out=ub.t[:], in_=u2s[i * 128:(i + 1) * 128, :]), ub, w=[ub])
            V(lambda e, dm=dm, i=i: e.tensor_tensor(out=dm.t[:], in0=Pall.t[:, i, :], in1=boff128.t[:], op=ALU.add),
              r=[Pall, boff128], w=[dm])
            V(lambda e, dm=dm, i=i: e.tensor_tensor(out=dm.t[:], in0=dm.t[:], in1=Mall.t[:, i, :], op=ALU.mult), r=[dm, Mall], w=[dm])
            V(lambda e, dm=dm, dv_=dv_: e.max(out=dv_.t[:], in_=dm.t[:]), r=[dm], w=[dv_])
            V(lambda e, dm=dm, dv_=dv_, xu=xu: e.max_index(out=xu.t[:], in_max=dv_.t[:], in_values=dm.t[:]), r=[dm, dv_], w=[xu])
            V(lambda e, xu=xu, xf=xf: e.tensor_copy(out=xf.t[:], in_=xu.t[:]), r=[xu], w=[xf])
            V(lambda e, dv_=dv_: e.tensor_scalar_add(out=dv_.t[:], in0=dv_.t[:], scalar1=-1.0), r=[dv_], w=[dv_])
            V(lambda e, dv_=dv_, i=i: e.tensor_copy(out=destU.t[:, i, :], in_=dv_.t[:]), r=[dv_], w=[destU])
            for k in range(8):
                V(lambda e, dm=dm, xf=xf, gl=gl, i=i, k=k: e.scalar_tensor_tensor(
                    out=dm.t[:], in0=iota_e.t[:], scalar=xf.t[:, k:k + 1], in1=gl.t[:, 0:NE], op0=ALU.is_equal, op1=ALU.mult,
                    accum_out=gk.t[:, i, k:k + 1]), r=[iota_e, xf, gl, dm], w=[dm, gk])
            for k in range(8):
                DM(lambda e, ub=ub, i=i, k=k: e.indirect_dma_start(out=Xs[:, :], out_offset=IOA(destU.t[:, i, k:k + 1], 0),
                                                                  in_=ub.t[:], in_offset=None),
                   ub, r=[ub, destU], eng="gpsimd")
        S.flush()
        esc.close()
        es_all = es
        es = ExitStack()
        xs = [mk(es, "xs%d" % i, [128, D], BF16) for i in range(2)]
        xT = [mk(es, "xTb%d" % i, [128, 8, 128], BF16) for i in range(2)]
        w1g = [mk(es, "w1g%d" % i, [128, 8, 256], F32) for i in range(1)]
        w3g = [mk(es, "w3g%d" % i, [128, 8, 256], F32) for i in range(1)]
        w2g = [mk(es, "w2g%d" % i, [128, 2, D], F32) for i in range(1)]
        w1b = [mk(es, "w1bs%d" % i, [128, 8, 256], BF16) for i in range(2)]
        w3b = [mk(es, "w3bs%d" % i, [128, 8, 256], BF16) for i in range(2)]
        w2b = [mk(es, "w2bs%d" % i, [128, 2, D], BF16) for i in range(3)]
        s1 = [mk(es, "s1s%d" % i, [128, 2, 128], F32) for i in range(2)]
        hT = [mk(es, "hTs%d" % i, [128, 2, 128], BF16) for i in range(2)]
        yb = [mk(es, "yb%d" % i, [128, D], F32) for i in range(2)]
        pX = mk(es, "pX", [128, 8, 128], BF16, psum=True)
        ph1 = [mk(es, "ph1s%d" % i, [128, 2, 128], F32, psum=True) for i in range(1)]
        ph3 = [mk(es, "ph3s%d" % i, [128, 2, 128], F32, psum=True) for i in range(1)]
        pY = [[mk(es, "pYs%d_%d" % (i, j), [128, 512], F32, psum=True) for j in range(2)] for i in range(2)]
        def blk_bufs(b):
            bb = b % 2
            return dict(x_=xs[bb], xt_=xT[bb], g1_=w1g[0], g3_=w3g[0], g2_=w2g[0], a1=w1b[bb], a3=w3b[bb], a2=w2b[b % 3],
                        s_=s1[bb], h_=hT[bb], y_=yb[bb], p1=ph1[0], p3=ph3[0], pp=pY[bb])

        def stA(b):
            d_ = blk_bufs(b)
            x_, xt_, g1_, g3_, g2_, a1, a3, a2 = (d_[k] for k in ("x_", "xt_", "g1_", "g3_", "g2_", "a1", "a3", "a2"))
            DM(lambda e: e.dma_start(out=x_.t[:], in_=Xs[b * 128:(b + 1) * 128, :]), x_, w=[x_])
            DM(lambda e: e.indirect_dma_start(out=g1_.t[:].rearrange("p a b -> p (a b)"), out_offset=None,
                                              in_=w1r, in_offset=IOA(widx.t[:, b:b + 1], 0),
                                              bounds_check=breg(e), oob_is_err=False), g1_, r=[widx], w=[g1_], eng="gpsimd")
            DM(lambda e: e.indirect_dma_start(out=g3_.t[:].rearrange("p a b -> p (a b)"), out_offset=None,
                                              in_=w3r, in_offset=IOA(widx.t[:, b:b + 1], 0),
                                              bounds_check=breg(e), oob_is_err=False), g3_, r=[widx], w=[g3_], eng="gpsimd")
            DM(lambda e: e.indirect_dma_start(out=g2_.t[:].rearrange("p a b -> p (a b)"), out_offset=None,
                                              in_=w2r, in_offset=IOA(widx.t[:, b:b + 1], 0),
                                              bounds_check=breg(e), oob_is_err=False), g2_, r=[widx], w=[g2_], eng="gpsimd")
            A(lambda e: e.activation(out=a1.t[:], in_=g1_.t[:], func=AF.Copy), r=[g1_], w=[a1])
            V(lambda e: e.tensor_copy(out=a3.t[:], in_=g3_.t[:]), r=[g3_], w=[a3])
            A(lambda e: e.activation(out=a2.t[:], in_=g2_.t[:], func=AF.Copy), r=[g2_], w=[a2])
            for kt in range(8):
                T(lambda e, kt=kt: e.transpose(out=pX.t[:, kt, :], in_=x_.t[:, kt * 128:(kt + 1) * 128], identity=identb.t[:]),
                  r=[x_, identb], w=[pX], inc=(kt == 7))
            V(lambda e: e.tensor_copy(out=xt_.t[:], in_=pX.t[:]), r=[pX], w=[xt_])

        def stB(b):
            d_ = blk_bufs(b)
            xt_, a1, a3, s_, h_, p1, p3 = (d_[k] for k in ("xt_", "a1", "a3", "s_", "h_", "p1", "p3"))
            for ft in range(2):
                for kt in range(8):
                    T(lambda e, kt=kt, ft=ft: e.matmul(p1.t[:, ft, :], lhsT=a1.t[:, kt, ft * 128:(ft + 1) * 128],
                                                       rhs=xt_.t[:, kt, :], start=(kt == 0), stop=(kt == 7)),
                      r=[a1, xt_], w=[p1], inc=(kt == 7 and ft == 1))
            for ft in range(2):
                for kt in range(8):
                    T(lambda e, kt=kt, ft=ft: e.matmul(p3.t[:, ft, :], lhsT=a3.t[:, kt, ft * 128:(ft + 1) * 128],
                                                       rhs=xt_.t[:, kt, :], start=(kt == 0), stop=(kt == 7)),
                      r=[a3, xt_], w=[p3], inc=(kt == 7 and ft == 1))
            A(lambda e: e.activation(out=s_.t[:], in_=p1.t[:], func=AF.Silu), r=[p1], w=[s_])
            V(lambda e: e.tensor_tensor(out=h_.t[:], in0=p3.t[:], in1=s_.t[:], op=ALU.mult), r=[p3, s_], w=[h_])

        def stC(b):
            d_ = blk_bufs(b)
            h_, a2, y_, pp = (d_[k] for k in ("h_", "a2", "y_", "pp"))
            for dc in range(2):
                for ft in range(2):
                    T(lambda e, dc=dc, ft=ft: e.matmul(pp[dc].t[:], lhsT=h_.t[:, ft, :], rhs=a2.t[:, ft, dc * 512:(dc + 1) * 512],
                                                       start=(ft == 0), stop=(ft == 1)),
                      r=[h_, a2], w=[pp[dc]], inc=(ft == 1))
            A(lambda e: e.activation(out=y_.t[:, 0:512], in_=pp[0].t[:], func=AF.Copy), r=[pp[0]], w=[y_])
            V(lambda e: e.tensor_copy(out=y_.t[:, 512:1024], in_=pp[1].t[:]), r=[pp[1]], w=[y_])
            DM(lambda e: e.dma_start(out=Ys[b * 128:(b + 1) * 128, :], in_=y_.t[:]), y_, r=[y_], eng="scalar")

        stA(0)
        for b in range(NBLK):
            if b + 1 < NBLK:
                stA(b + 1)
            stB(b)
            if b >= 1:
                stC(b - 1)
            if b % 128 == 127:
                S.flush()
        stC(NBLK - 1)
        S.flush()
        es.close()
        es = es_all
        g2b = load_bc(es, "g2b", mod_s[0:1, 5 * D:6 * D], D)
        l2g = load_bc(es, "l2g", dr["ln2_g"][0:1, :], D, eng="gpsimd")
        l2b = load_bc(es, "l2b", dr["ln2_b"][0:1, :], D)
        sw1 = mk(es, "sw1", [128, 8, 256], BF16); sw3 = mk(es, "sw3", [128, 8, 256], BF16); sw2 = mk(es, "sw2", [128, 2, D], BF16)
        with ExitStack() as es3:
            f1 = mk(es3, "sf1", [128, 8, 256], F32); f3 = mk(es3, "sf3", [128, 8, 256], F32); f2 = mk(es3, "sf2", [128, 2, D], F32)
            DM(lambda e: e.dma_start(out=f1.t[:], in_=dr["moe_w1"][n_decl - 1]), f1, w=[f1])
            DM(lambda e: e.dma_start(out=f3.t[:], in_=dr["moe_w3"][n_decl - 1]), f3, w=[f3])
            DM(lambda e: e.dma_start(out=f2.t[:], in_=dr["moe_w2"][n_decl - 1]), f2, w=[f2])
            V(lambda e: e.tensor_copy(out=sw1.t[:], in_=f1.t[:]), r=[f1], w=[sw1])
            V(lambda e: e.tensor_copy(out=sw3.t[:], in_=f3.t[:]), r=[f3], w=[sw3])
            V(lambda e: e.tensor_copy(out=sw2.t[:], in_=f2.t[:]), r=[f2], w=[sw2])
            S.flush()
        uTl = [mk(es, "uTl%d" % i, [128, 8, 128], BF16) for i in range(2)]
        s1c = [mk(es, "s1c%d" % i, [128, 2, 128], F32) for i in range(2)]
        hTc = [mk(es, "hTc%d" % i, [128, 2, 128], BF16) for i in range(2)]
        acc = [mk(es, "acc%d" % i, [128, D], F32) for i in range(2)]
        yk = [mk(es, "yk%d" % i, [128, D], F32) for i in range(3)]
        xl = [mk(es, "xlc%d" % i, [128, D], F32) for i in range(2)]
        ot = [mk(es, "otc%d" % i, [128, D], F32) for i in range(2)]
        mve = [mk(es, "mvc%d" % i, [128, 4], F32) for i in range(2)]
        scr = mk(es, "scrC", [128, 12], F32)
        qh1 = mk(es, "qh1", [128, 2, 128], F32, psum=True)
        qh3 = mk(es, "qh3", [128, 2, 128], F32, psum=True)
        qY = [[mk(es, "qY%d_%d" % (i, j), [128, 512], F32, psum=True) for j in range(2)] for i in range(2)]
        nyk = 0
        for i in range(NT):
            ib = i % 2
            u_ = uTl[ib]; s_ = s1c[ib]; h_ = hTc[ib]; ac = acc[ib]; xl_ = xl[ib]; o_ = ot[ib]; m_ = mve[ib]; pp = qY[ib]
            DM(lambda e, u_=u_, i=i: e.dma_start(out=u_.t[:], in_=u2Ts[:, :, i * 128:(i + 1) * 128]), u_, w=[u_])
            DM(lambda e, xl_=xl_, i=i: e.dma_start(out=xl_.t[:], in_=x1s[i * 128:(i + 1) * 128, :]), xl_, w=[xl_])
            for ft in range(2):
                for kt in range(8):
                    T(lambda e, u_=u_, kt=kt, ft=ft: e.matmul(qh1.t[:, ft, :], lhsT=sw1.t[:, kt, ft * 128:(ft + 1) * 128],
                                                              rhs=u_.t[:, kt, :], start=(kt == 0), stop=(kt == 7)),
                      r=[sw1, u_], w=[qh1], inc=(kt == 7 and ft == 1))
            for ft in range(2):
                for kt in range(8):
                    T(lambda e, u_=u_, kt=kt, ft=ft: e.matmul(qh3.t[:, ft, :], lhsT=sw3.t[:, kt, ft * 128:(ft + 1) * 128],
                                                              rhs=u_.t[:, kt, :], start=(kt == 0), stop=(kt == 7)),
                      r=[sw3, u_], w=[qh3], inc=(kt == 7 and ft == 1))
            A(lambda e, s_=s_: e.activation(out=s_.t[:], in_=qh1.t[:], func=AF.Silu), r=[qh1], w=[s_])
            V(lambda e, s_=s_, h_=h_: e.tensor_tensor(out=h_.t[:], in0=qh3.t[:], in1=s_.t[:], op=ALU.mult), r=[qh3, s_], w=[h_])
            for dc in range(2):
                for ft in range(2):
                    T(lambda e, pp=pp, dc=dc, ft=ft, h_=h_: e.matmul(pp[dc].t[:], lhsT=h_.t[:, ft, :],
                                                                    rhs=sw2.t[:, ft, dc * 512:(dc + 1) * 512],
                                                                    start=(ft == 0), stop=(ft == 1)),
                      r=[h_, sw2], w=[pp[dc]], inc=(ft == 1))
            A(lambda e, pp=pp, ac=ac: e.activation(out=ac.t[:, 0:512], in_=pp[0].t[:], func=AF.Copy), r=[pp[0]], w=[ac])
            A(lambda e, pp=pp, ac=ac: e.activation(out=ac.t[:, 512:1024], in_=pp[1].t[:], func=AF.Copy), r=[pp[1]], w=[ac])
            for k in range(8):
                y_ = yk[nyk % 3]; nyk += 1
                DM(lambda e, y_=y_, i=i, k=k: e.indirect_dma_start(out=y_.t[:], out_offset=None, in_=Ys[:, :],
                                                                  in_offset=IOA(destU.t[:, i, k:k + 1], 0)),
                   y_, r=[destU], w=[y_], eng="gpsimd")
                V(lambda e, y_=y_, ac=ac, i=i, k=k: e.scalar_tensor_tensor(out=ac.t[:], in0=y_.t[:], scalar=gk.t[:, i, k:k + 1],
                                                                           in1=ac.t[:], op0=ALU.mult, op1=ALU.add),
                  r=[y_, gk, ac], w=[ac])
            V(lambda e, ac=ac: e.tensor_tensor(out=ac.t[:], in0=ac.t[:], in1=g2b.t[:], op=ALU.mult), r=[ac, g2b], w=[ac])
            V(lambda e, xl_=xl_, ac=ac: e.scalar_tensor_tensor(out=xl_.t[:], in0=xl_.t[:], scalar=ALPHA, in1=ac.t[:],
                                                               op0=ALU.mult, op1=ALU.add), r=[xl_, ac], w=[xl_])
            layer_norm_stats(xl_, m_, scr, D)
            V(lambda e, xl_=xl_, m_=m_: e.tensor_scalar(out=xl_.t[:], in0=xl_.t[:], scalar1=m_.t[:, 0:1], scalar2=m_.t[:, 1:2],
                                                        op0=ALU.subtract, op1=ALU.mult), r=[xl_, m_], w=[xl_])
            V(lambda e, xl_=xl_: e.tensor_tensor(out=xl_.t[:], in0=xl_.t[:], in1=l2g.t[:], op=ALU.mult), r=[xl_, l2g], w=[xl_])
            V(lambda e, xl_=xl_, o_=o_: e.tensor_tensor(out=o_.t[:], in0=xl_.t[:], in1=l2b.t[:], op=ALU.add), r=[xl_, l2b], w=[o_])
            DM(lambda e, o_=o_, i=i: e.dma_start(out=out[i * 128:(i + 1) * 128, :], in_=o_.t[:]), o_, r=[o_], eng="scalar")
        S.flush()
    return


def make_in_maps(inputs, sparse=True):
    f = lambda a: np.ascontiguousarray(np.asarray(a, dtype=np.float32))
    x = f(inputs["x"]); c = f(inputs["c"])
    w1 = np.concatenate([f(inputs["moe_w1"])[0], f(inputs["sh_w1"])], axis=0)
    w3 = np.concatenate([f(inputs["moe_w3"])[0], f(inputs["sh_w3"])], axis=0)
    w2 = np.concatenate([f(inputs["moe_w2"])[0], f(inputs["sh_w2"])], axis=0)
    if sparse:
        ne = w1.shape[0]
        w1 = np.ascontiguousarray(w1.reshape(ne, 8, 128, 256).transpose(0, 2, 1, 3))
        w3 = np.ascontiguousarray(w3.reshape(ne, 8, 128, 256).transpose(0, 2, 1, 3))
        w2 = np.ascontiguousarray(w2.reshape(ne, 2, 128, D).transpose(0, 2, 1, 3))
    shared = {
        "w_ada": f(inputs["w_ada"])[0], "b_ada": f(inputs["b_ada"]), "w_in": f(inputs["w_in"])[0],
        "conv_wT": np.ascontiguousarray(f(inputs["ml_conv_w"])[0].T),
        "conv_bT": np.ascontiguousarray(f(inputs["ml_conv_b"])[0].reshape(8, 128).T),
        "gate_b_rep": np.ascontiguousarray(np.broadcast_to(np.tile(f(inputs["ml_gate_b"])[0], 64)[None, :], (128, 512))),
        "ml_norm_g": f(inputs["ml_norm_g"]), "w_out": f(inputs["w_out"])[0],
        "ln1_g": f(inputs["ln1_g"]), "ln1_b": f(inputs["ln1_b"]),
        "w_router": f(inputs["w_router"])[0], "router_bias": f(inputs["router_bias"]),
        "moe_w1": w1, "moe_w3": w3, "moe_w2": w2,
        "ln2_g": f(inputs["ln2_g"]), "ln2_b": f(inputs["ln2_b"]),
    }
    maps = []
    for core in range(8):
        b, half = core // 2, core % 2
        m = dict(shared)
        m["x_b"] = x[b]
        m["x_own"] = np.ascontiguousarray(x[b].reshape(8, 2, 512, D)[:, half].reshape(NOWN, D))
        m["cT"] = np.ascontiguousarray(c[b].reshape(8, 128).T)
        fl = np.zeros((128, 2), np.float32); fl[:, 0] = half; fl[:, 1] = 1 - half
        m["flag"] = fl
        maps.append(m)
    return maps


_NC = None


def kernel(**inputs):
    global _NC
    if _NC is None:
        _NC = build_nc()
    maps = make_in_maps(inputs)
    res = run_bass_kernel_spmd(_NC, maps, core_ids=list(range(8)))
    outp = np.empty((4, S_LEN, D), np.float32)
    for core in range(8):
        b, half = core // 2, core % 2
        outp[b].reshape(8, 2, 512, D)[:, half] = np.asarray(res.results[core]["out"]).reshape(8, 512, D)
    return outp
```

```python
import os
import numpy as np
from contextlib import ExitStack
import concourse.bass as bass
import concourse.mybir as mybir
from concourse.bass_utils import run_bass_kernel_spmd

F32 = mybir.dt.float32
BF16 = mybir.dt.bfloat16
U32 = mybir.dt.uint32
I32 = mybir.dt.int32
NBLK = 512
AF = mybir.ActivationFunctionType
ALU = mybir.AluOpType

D = 1024
S_LEN = 8192
NOWN = 4096
PC = 3592
NE = 256
ALPHA = 2.0 ** 0.25
EPS = 1e-5
NEGV = -30000.0


class Res:
    def __init__(self, name, t=None):
        self.name = name
        self.t = t
        self.w = None
        self.r = []
        self.sem = None
        self.cnt = 0


class Sched:
    ENG = ("tensor", "vector", "scalar", "gpsimd", "sync")

    def __init__(self, nc, es):
        self.nc = nc
        self.es = es
        self.thunks = {e: [] for e in self.ENG}
        self.esem = {e: es.enter_context(nc.semaphore("s_" + e)) for e in self.ENG}
        self.ecnt = {e: 0 for e in self.ENG}
        self.known = {e: {} for e in self.ENG}
        self.semobj = {id(s): s for s in self.esem.values()}
        self.pool = []
        self.live = []
        self.nsem = 0
        self.nblocks = 0

    def _need(self, need, ev):
        if ev is None:
            return
        k, v = ev
        if need.get(k, 0) < v:
            need[k] = v

    def _deps(self, eng, reads, writes):
        need = {}
        for r in reads:
            self._need(need, r.w)
        for w in writes:
            self._need(need, w.w)
            for ev in w.r:
                self._need(need, ev)
        waits = []
        kn = self.known[eng]
        own = id(self.esem[eng])
        for k, v in need.items():
            if eng == "tensor" and k == own:
                continue
            if kn.get(k, 0) < v:
                kn[k] = v
                waits.append((self.semobj[k], v))
        return waits

    def _commit(self, ev, reads, writes):
        for r in reads:
            r.r.append(ev)
            if len(r.r) > 24:
                m = {}
                for k, v in r.r:
                    if m.get(k, 0) < v:
                        m[k] = v
                r.r = list(m.items())
        for w in writes:
            w.w = ev
            w.r = []

    def op(self, eng, fn, reads=(), writes=(), inc=True):
        waits = self._deps(eng, reads, writes)
        sem = self.esem[eng]
        if inc:
            self.ecnt[eng] += 1
            ev = (id(sem), self.ecnt[eng])
        else:
            ev = (id(sem), self.ecnt[eng] + 1)
        self._commit(ev, reads, writes)

        def thunk(e, waits=waits, fn=fn, inc=inc, sem=sem):
            for s, v in waits:
                e.wait_ge(s, v)
            ins = fn(e)
            if inc:
                ins.then_inc(sem, 1)
        self.thunks[eng].append(thunk)
        return ev

    def dma(self, eng, fn, sb, reads=(), writes=()):
        if sb.sem is None:
            if self.pool:
                sb.sem, sb.cnt = self.pool.pop()
            else:
                sb.sem = self.es.enter_context(self.nc.semaphore("d%d" % self.nsem))
                self.nsem += 1
                sb.cnt = 0
                self.semobj[id(sb.sem)] = sb.sem
            self.live.append(sb)
        waits = self._deps(eng, reads, writes)
        sb.cnt += 16
        ev = (id(sb.sem), sb.cnt)
        self._commit(ev, reads, writes)
        sem = sb.sem

        def thunk(e, waits=waits, fn=fn, sem=sem):
            for s, v in waits:
                e.wait_ge(s, v)
            fn(e).then_inc(sem, 16)
        self.thunks[eng].append(thunk)
        return ev

    def flush(self):
        nc = self.nc
        targets = [(self.esem[x], self.ecnt[x]) for x in self.ENG if self.ecnt[x] > 0]
        targets += [(r.sem, r.cnt) for r in self.live]
        for e in self.ENG:
            def thunk(eng, targets=targets):
                for s, v in targets:
                    eng.wait_ge(s, v)
            self.thunks[e].append(thunk)
            kn = self.known[e]
            for s, v in targets:
                kn[id(s)] = v
        th = self.thunks
        with nc.Block() as block:
            @block.sync
            def _(e):
                for t in th["sync"]:
                    t(e)

            @block.tensor
            def _(e):
                for t in th["tensor"]:
                    t(e)

            @block.vector
            def _(e):
                for t in th["vector"]:
                    t(e)

            @block.scalar
            def _(e):
                for t in th["scalar"]:
                    t(e)

            @block.gpsimd
            def _(e):
                for t in th["gpsimd"]:
                    t(e)
        self.thunks = {e: [] for e in self.ENG}
        self.nblocks += 1
        for r in self.live:
            self.pool.append((r.sem, r.cnt))
            r.sem = None
        self.live = []


def build_nc(stop_after=None, n_exp=NE + 1, dbg=False, n_decl=NE + 1, sparse=True):
    nc = bass.Bass("TRN2", target_bir_lowering=False)
    din = lambda n, sh, dt=F32: nc.dram_tensor(n, sh, dt, kind="ExternalInput").ap()
    dbgs = dbg if dbg else ()
    dsc = lambda n, sh, dt: nc.dram_tensor(n, sh, dt, kind=("ExternalOutput" if n in dbgs else "Internal")).ap()
    x_b = din("x_b", [S_LEN, D])
    x_own = din("x_own", [NOWN, D])
    cT = din("cT", [128, 8])
    flag_in = din("flag", [128, 2])
    w_ada = din("w_ada", [D, 6 * D])
    b_ada = din("b_ada", [1, 6 * D])
    w_in = din("w_in", [D, PC])
    conv_wT = din("conv_wT", [D, 4])
    conv_bT = din("conv_bT", [128, 8])
    gate_b_rep = din("gate_b_rep", [128, 512])
    ml_norm_g = din("ml_norm_g", [1, 512])
    w_out = din("w_out", [D, D])
    ln1_g = din("ln1_g", [1, D]); ln1_b = din("ln1_b", [1, D])
    w_router = din("w_router", [D, NE])
    router_bias = din("router_bias", [1, NE])
    if sparse:
        moe_w1 = din("moe_w1", [n_decl, 128, 8, 256])
        moe_w3 = din("moe_w3", [n_decl, 128, 8, 256])
        moe_w2 = din("moe_w2", [n_decl, 128, 2, D])
    else:
        moe_w1 = din("moe_w1", [n_decl, D, 256])
        moe_w3 = din("moe_w3", [n_decl, D, 256])
        moe_w2 = din("moe_w2", [n_decl, 256, D])
    ln2_g = din("ln2_g", [1, D]); ln2_b = din("ln2_b", [1, D])
    out = nc.dram_tensor("out", [NOWN, D], F32, kind="ExternalOutput").ap()

    mod_s = dsc("mod_s", [1, 6 * D], F32)
    sbqT = dsc("sbqT", [512, S_LEN], BF16); sbkT = dsc("sbkT", [512, S_LEN], BF16)
    sbv = dsc("sbv", [S_LEN, 512], BF16)
    mlqT = dsc("mlqT", [512, S_LEN], BF16); mlkT = dsc("mlkT", [512, S_LEN], BF16)
    mlv = dsc("mlv", [S_LEN, 512], BF16)
    mlo = dsc("mlo", [S_LEN, 512], F32)
    mlif = dsc("mlif", [S_LEN, 8], F32)
    sbo = dsc("sbo", [512, NOWN], BF16)
    mlout = dsc("mlout", [S_LEN, 512], BF16)
    x1s = dsc("x1s", [NOWN, D], F32)
    u2Ts = dsc("u2Ts", [128, 8, NOWN], BF16)
    gates_s = dsc("gates_s", [128, 32, NE + 1], F32)
    u2s = dsc("u2s", [NOWN, D], BF16)
    Xs = dsc("Xs", [NBLK * 128, D], BF16)
    Ys = dsc("Ys", [NBLK * 128, D], F32)

    with ExitStack() as eg:
        S = Sched(nc, eg)

        def mk(es, name, shape, dt, psum=False):
            t = es.enter_context(nc.psum_tensor(name, shape, dt) if psum else nc.sbuf_tensor(name, shape, dt))
            return Res(name, t)

        V = lambda fn, r=(), w=(), **k: S.op("vector", fn, r, w, **k)
        A = lambda fn, r=(), w=(), **k: S.op("scalar", fn, r, w, **k)
        G = lambda fn, r=(), w=(), **k: S.op("gpsimd", fn, r, w, **k)
        T = lambda fn, r=(), w=(), **k: S.op("tensor", fn, r, w, **k)
        DM = lambda fn, sb, r=(), w=(), eng="sync": S.dma(eng, fn, sb, r, w)

        identb = mk(eg, "identb", [128, 128], BF16)
        identf = mk(eg, "identf", [128, 128], F32)
        onesf = mk(eg, "onesf", [128, 128], F32)
        onesb = mk(eg, "onesb", [128, 128], BF16)
        epsc = mk(eg, "epsc", [128, 1], F32)
        onec = mk(eg, "onec", [128, 1], F32)
        flag = mk(eg, "flagt", [128, 2], F32)
        G(lambda e: e.memset(onesf.t[:], 1.0), w=[onesf])
        G(lambda e: e.memset(identf.t[:], 1.0), w=[identf])
        G(lambda e: e.affine_select(out=identf.t[:], in_=identf.t[:], pattern=[[-1, 128]], base=0,
                                     channel_multiplier=1, compare_op=ALU.is_equal, fill=0.0), r=[identf], w=[identf])
        V(lambda e: e.tensor_copy(out=identb.t[:], in_=identf.t[:]), r=[identf], w=[identb])
        V(lambda e: e.tensor_copy(out=onesb.t[:], in_=onesf.t[:]), r=[onesf], w=[onesb])
        V(lambda e: e.memset(epsc.t[:], EPS), w=[epsc])
        V(lambda e: e.memset(onec.t[:], 1.0), w=[onec])
        DM(lambda e: e.dma_start(out=flag.t[:], in_=flag_in[:, :]), flag, w=[flag])
        Mall = mk(eg, "Mall", [128, 32, NE], BF16)

        def layer_norm_stats(src, mv_out, scr6, n):
            nch = (n + 511) // 512
            for ci in range(nch):
                lo = ci * 512
                hi = min(n, lo + 512)
                V(lambda e, ci=ci, lo=lo, hi=hi: e.bn_stats(out=scr6.t[:, ci * 6:(ci + 1) * 6], in_=src.t[:, lo:hi]),
                  r=[src], w=[scr6])
            V(lambda e: e.bn_aggr(out=mv_out.t[:, 0:2], in_=scr6.t[:, 0:6 * nch]), r=[scr6], w=[mv_out])
            A(lambda e: e.activation(out=mv_out.t[:, 2:3], in_=mv_out.t[:, 1:2], func=AF.Ln, bias=epsc.t[:, 0:1], scale=1.0),
              r=[mv_out, epsc], w=[mv_out])
            A(lambda e: e.activation(out=mv_out.t[:, 1:2], in_=mv_out.t[:, 2:3], func=AF.Exp, scale=-0.5),
              r=[mv_out], w=[mv_out])

        with ExitStack() as es:
            ct = mk(es, "ct", [128, 8], F32)
            st = mk(es, "st", [128, 8], F32)
            rep = mk(es, "rep", [128, 8, 128], F32)
            wa = [mk(es, "wa%d" % i, [128, 8, 512], F32) for i in range(2)]
            bad = mk(es, "bad", [1, 6 * D], F32)
            modr = mk(es, "modr", [1, 6 * D], F32)
            pm = [mk(es, "pm%d" % i, [128, 512], F32, psum=True) for i in range(2)]
            DM(lambda e: e.dma_start(out=ct.t[:], in_=cT[:, :]), ct, w=[ct])
            DM(lambda e: e.dma_start(out=bad.t[:], in_=b_ada[:, :]), bad, w=[bad])
            A(lambda e: e.activation(out=st.t[:], in_=ct.t[:], func=AF.Silu), r=[ct], w=[st])
            for kt in range(8):
                V(lambda e, kt=kt: e.tensor_copy(out=rep.t[:, kt, :], in_=st.t[:, kt:kt + 1].to_broadcast([128, 128])),
                  r=[st], w=[rep])
            wav = w_ada.rearrange("(kt k) c -> k kt c", k=128)
            for ch in range(12):
                wb = wa[ch % 2]
                for kh in range(2):
                    DM(lambda e, wb=wb, ch=ch, kh=kh: e.dma_start(out=wb.t[:, kh * 4:(kh + 1) * 4, :],
                                                                  in_=wav[:, kh * 4:(kh + 1) * 4, ch * 512:(ch + 1) * 512]),
                       wb, w=[wb], eng=("sync" if kh == 0 else "gpsimd"))
                p = pm[ch % 2]
                for kt in range(8):
                    T(lambda e, p=p, wb=wb, kt=kt: e.matmul(p.t[:], lhsT=rep.t[:, kt, :], rhs=wb.t[:, kt, :],
                                                            start=(kt == 0), stop=(kt == 7)),
                      r=[rep, wb], w=[p], inc=(kt == 7))
                V(lambda e, p=p, ch=ch: e.tensor_tensor(out=modr.t[0:1, ch * 512:(ch + 1) * 512], in0=p.t[0:1, :],
                                                        in1=bad.t[0:1, ch * 512:(ch + 1) * 512], op=ALU.add),
                  r=[p, bad], w=[modr])
            for cidx in (1, 4):
                V(lambda e, cidx=cidx: e.tensor_scalar_add(out=modr.t[0:1, cidx * D:(cidx + 1) * D],
                                                           in0=modr.t[0:1, cidx * D:(cidx + 1) * D], scalar1=1.0),
                  r=[modr], w=[modr])
            DM(lambda e: e.dma_start(out=mod_s[:, :], in_=modr.t[:]), modr, r=[modr])
            S.flush()
        if stop_after == 0:
            return nc

        def load_bc(es, name, src_ap, n, eng="sync"):
            t = mk(es, name, [128, n], F32)
            DM(lambda e: e.dma_start(out=t.t[:], in_=src_ap.partition_broadcast(128)), t, w=[t], eng=eng)
            return t

        with ExitStack() as es:
            winb = mk(es, "winb", [128, 8, PC], BF16)
            sc1p = load_bc(es, "sc1p", mod_s[0:1, D:2 * D], D)
            sh1 = load_bc(es, "sh1", mod_s[0:1, 0:D], D, eng="gpsimd")
            cw = mk(es, "cw", [128, 8, 4], F32)
            cb = mk(es, "cb", [128, 8], F32)
            DM(lambda e: e.dma_start(out=cw.t[:], in_=conv_wT.rearrange("(c f) k -> f c k", f=128)), cw, w=[cw])
            DM(lambda e: e.dma_start(out=cb.t[:], in_=conv_bT[:, :]), cb, w=[cb])
            with ExitStack() as es2:
                wst = [mk(es2, "wst%d" % i, [128, PC], F32) for i in range(2)]
                wiv = w_in.rearrange("(kt k) c -> k kt c", k=128)
                for kt in range(8):
                    w = wst[kt % 2]
                    DM(lambda e, w=w, kt=kt: e.dma_start(out=w.t[:], in_=wiv[:, kt, :]), w, w=[w],
                       eng=("sync" if kt % 2 == 0 else "gpsimd"))
                    if kt % 2 == 0:
                        V(lambda e, w=w, kt=kt: e.tensor_copy(out=winb.t[:, kt, :], in_=w.t[:]), r=[w], w=[winb])
                    else:
                        G(lambda e, w=w, kt=kt: e.tensor_copy(out=winb.t[:, kt, :], in_=w.t[:]), r=[w], w=[winb])
                S.flush()
            xt = [mk(es, "xt%d" % i, [128, D], F32) for i in range(4)]
            ub = [mk(es, "ub%d" % i, [128, D], BF16) for i in range(4)]
            uT = [mk(es, "uT%d" % i, [128, 8, 512], BF16) for i in range(2)]
            scr4 = [mk(es, "scr6_%d" % i, [128, 12], F32) for i in range(4)]
            mv = [mk(es, "mv%d" % i, [128, 4], F32) for i in range(4)]
            ptp = [mk(es, "ptp%d" % i, [128, 8, 128], BF16, psum=True) for i in range(2)]
            ptk = [mk(es, "ptk%d" % i, [128, 512], F32, psum=True) for i in range(3)]
            pfm = [mk(es, "pfm%d" % i, [128, 512], F32, psum=True) for i in range(2)]
            pif = mk(es, "pif", [128, 8], F32, psum=True)
            evb = [mk(es, "evb%d" % i, [128, 512], BF16) for i in range(4)]
            evo = [mk(es, "evo%d" % i, [128, 512], F32) for i in range(4)]
            evi = [mk(es, "evi%d" % i, [128, 8], F32) for i in range(4)]
            fo = [mk(es, "fo%d" % i, [128, 512], BF16) for i in range(3)]
            cin = [mk(es, "cin%d" % i, [128, 515], F32) for i in range(8)]
            cacc = [mk(es, "cacc%d" % i, [128, 512], F32) for i in range(2)]
            csl = [mk(es, "csl%d" % i, [128, 512], F32) for i in range(2)]
            for i in range(8):
                G(lambda e, i=i: e.memset(cin[i].t[:, 0:3], 0.0), w=[cin[i]])
            nev = 0
            nfo = 0
            for stl in range(S_LEN // 512):
                u = uT[stl % 2]
                for j in range(4):
                    ti = stl * 4 + j
                    x = xt[j]
                    DM(lambda e, x=x, ti=ti: e.dma_start(out=x.t[:], in_=x_b[ti * 128:(ti + 1) * 128, :]), x, w=[x],
                       eng=("sync" if j % 2 == 0 else "gpsimd"))
                for j in range(4):
                    x = xt[j]; m = mv[j]; sc6 = scr4[j]
                    for ci in range(2):
                        V(lambda e, x=x, sc6=sc6, ci=ci: e.bn_stats(out=sc6.t[:, ci * 6:(ci + 1) * 6], in_=x.t[:, ci * 512:(ci + 1) * 512]),
                          r=[x], w=[sc6])
                    V(lambda e, m=m, sc6=sc6: e.bn_aggr(out=m.t[:, 0:2], in_=sc6.t[:, 0:12]), r=[sc6], w=[m])
                for j in range(4):
                    m = mv[j]
                    A(lambda e, m=m: e.activation(out=m.t[:, 2:3], in_=m.t[:, 1:2], func=AF.Ln, bias=epsc.t[:, 0:1], scale=1.0),
                      r=[m, epsc], w=[m])
                for j in range(4):
                    m = mv[j]
                    A(lambda e, m=m: e.activation(out=m.t[:, 1:2], in_=m.t[:, 2:3], func=AF.Exp, scale=-0.5), r=[m], w=[m])
                for j in range(4):
                    x = xt[j]; m = mv[j]
                    V(lambda e, x=x, m=m: e.tensor_scalar(out=x.t[:], in0=x.t[:], scalar1=m.t[:, 0:1], scalar2=m.t[:, 1:2],
                                                          op0=ALU.subtract, op1=ALU.mult), r=[x, m], w=[x])
                for j in range(4):
                    x = xt[j]
                    G(lambda e, x=x: e.tensor_tensor(out=x.t[:], in0=x.t[:], in1=sc1p.t[:], op=ALU.mult), r=[x, sc1p], w=[x])
                for j in range(4):
                    x = xt[j]; ubf = ub[j]
                    V(lambda e, x=x, ubf=ubf: e.tensor_tensor(out=ubf.t[:], in0=x.t[:], in1=sh1.t[:], op=ALU.add),
                      r=[x, sh1], w=[ubf])
                for j in range(4):
                    ti = stl * 4 + j
                    ubf = ub[j]
                    pt = ptp[ti % 2]
                    for kt in range(8):
                        T(lambda e, pt=pt, ubf=ubf, kt=kt: e.transpose(out=pt.t[:, kt, :], in_=ubf.t[:, kt * 128:(kt + 1) * 128],
                                                                      identity=identb.t[:]),
                          r=[ubf, identb], w=[pt], inc=(kt == 7))
                    A(lambda e, pt=pt, u=u, j=j: e.activation(out=u.t[:, :, j * 128:(j + 1) * 128], in_=pt.t[:], func=AF.Copy),
                      r=[pt], w=[u])
                for j in range(4):
                    ti = stl * 4 + j
                    for ci, (c0, dst) in enumerate(((1024, sbv), (2560, mlv), (3072, mlo))):
                        p = ptk[ci]
                        for kt in range(8):
                            T(lambda e, p=p, u=u, kt=kt, j=j, c0=c0: e.matmul(p.t[:], lhsT=u.t[:, kt, j * 128:(j + 1) * 128],
                                                                              rhs=winb.t[:, kt, c0:c0 + 512],
                                                                              start=(kt == 0), stop=(kt == 7)),
                              r=[u, winb], w=[p], inc=(kt == 7))
                        if ci < 2:
                            ev = evb[nev % 4]; nev += 1
                            A(lambda e, p=p, ev=ev: e.activation(out=ev.t[:], in_=p.t[:], func=AF.Copy), r=[p], w=[ev])
                        else:
                            ev = evo[j]
                            A(lambda e, p=p, ev=ev: e.activation(out=ev.t[:], in_=p.t[:], func=AF.Sigmoid), r=[p], w=[ev])
                        DM(lambda e, ev=ev, dst=dst, ti=ti: e.dma_start(out=dst[ti * 128:(ti + 1) * 128, :], in_=ev.t[:]),
                           ev, r=[ev], eng="gpsimd")
                    for kt in range(8):
                        T(lambda e, u=u, kt=kt, j=j: e.matmul(pif.t[:], lhsT=u.t[:, kt, j * 128:(j + 1) * 128],
                                                              rhs=winb.t[:, kt, 3584:3592], start=(kt == 0), stop=(kt == 7)),
                          r=[u, winb], w=[pif], inc=(kt == 7))
                    ev = evi[j]
                    V(lambda e, ev=ev: e.tensor_copy(out=ev.t[:], in_=pif.t[:]), r=[pif], w=[ev])
                    DM(lambda e, ev=ev, ti=ti: e.dma_start(out=mlif[ti * 128:(ti + 1) * 128, :], in_=ev.t[:]), ev, r=[ev],
                       eng="gpsimd")
                for ct_ in range(16):
                    c0 = (ct_ * 128) if ct_ < 8 else (1536 + (ct_ - 8) * 128)
                    p = pfm[ct_ % 2]
                    for kt in range(8):
                        T(lambda e, p=p, u=u, kt=kt, c0=c0: e.matmul(p.t[:], lhsT=winb.t[:, kt, c0:c0 + 128], rhs=u.t[:, kt, :],
                                                                     start=(kt == 0), stop=(kt == 7)),
                          r=[u, winb], w=[p], inc=(kt == 7))
                    f = fo[nfo % 3]; nfo += 1
                    if ct_ < 8:
                        dst = sbqT if ct_ < 4 else sbkT
                        A(lambda e, p=p, f=f: e.activation(out=f.t[:], in_=p.t[:], func=AF.Copy), r=[p], w=[f])
                    else:
                        c8 = ct_ - 8
                        dst = mlqT if c8 < 4 else mlkT
                        ci_ = cin[c8]; ac = cacc[c8 % 2]; sl = csl[c8 % 2]
                        V(lambda e, p=p, ci_=ci_: e.tensor_copy(out=ci_.t[:, 3:515], in_=p.t[:]), r=[p], w=[ci_])
                        V(lambda e, ci_=ci_, ac=ac, c8=c8: e.tensor_scalar(out=ac.t[:], in0=ci_.t[:, 3:515],
                                                                           scalar1=cw.t[:, c8, 3:4], scalar2=cb.t[:, c8:c8 + 1],
                                                                           op0=ALU.mult, op1=ALU.add), r=[ci_, cw, cb], w=[ac])
                        for k in range(3):
                            V(lambda e, ci_=ci_, ac=ac, c8=c8, k=k: e.scalar_tensor_tensor(
                                out=ac.t[:], in0=ci_.t[:, k:k + 512], scalar=cw.t[:, c8, k:k + 1], in1=ac.t[:],
                                op0=ALU.mult, op1=ALU.add), r=[ci_, cw, ac], w=[ac])
                        G(lambda e, ci_=ci_: e.tensor_copy(out=ci_.t[:, 0:3], in_=ci_.t[:, 512:515]), r=[ci_], w=[ci_])
                        if c8 < 4:
                            A(lambda e, ac=ac, f=f: e.activation(out=f.t[:], in_=ac.t[:], func=AF.Silu), r=[ac], w=[f])
                        else:
                            A(lambda e, ac=ac, sl=sl: e.activation(out=sl.t[:], in_=ac.t[:], func=AF.Silu), r=[ac], w=[sl])
                            G(lambda e, sl=sl, f=f: e.tensor_scalar(out=f.t[:], in0=sl.t[:], scalar1=128.0 ** -0.5, scalar2=None,
                                                                    op0=ALU.mult), r=[sl], w=[f])
                    r0 = (ct_ % 4) * 128
                    DM(lambda e, f=f, dst=dst, r0=r0, stl=stl: e.dma_start(out=dst[r0:r0 + 128, stl * 512:(stl + 1) * 512],
                                                                           in_=f.t[:]), f, r=[f], eng="scalar")
            S.flush()
        if stop_after == 1:
            return nc

        with ExitStack() as es:
            tri = mk(es, "tri", [128, 128], BF16)
            trif = mk(es, "trif", [128, 128], F32)
            G(lambda e: e.memset(trif.t[:], 1.0), w=[trif])
            G(lambda e: e.affine_select(out=trif.t[:], in_=trif.t[:], pattern=[[-1, 128]], base=0, channel_multiplier=1,
                                         compare_op=ALU.is_ge, fill=0.0), r=[trif], w=[trif])
            V(lambda e: e.tensor_copy(out=tri.t[:], in_=trif.t[:]), r=[trif], w=[tri])
            msk = []
            for j in range(4):
                mj = mk(es, "msk%d" % j, [128, 512], F32)
                G(lambda e, mj=mj: e.memset(mj.t[:], 1.0), w=[mj])
                G(lambda e, mj=mj, j=j: e.affine_select(out=mj.t[:], in_=mj.t[:], pattern=[[1, 512]], base=-128 * j - 1,
                                                        channel_multiplier=-1, compare_op=ALU.is_ge, fill=0.0), r=[mj], w=[mj])
                msk.append(mj)
            meff = []
            for pp_ in range(8):
                me = mk(es, "meff%d" % pp_, [128, 512], BF16)
                if pp_ < 4:
                    V(lambda e, me=me, pp_=pp_: e.tensor_scalar(out=me.t[:], in0=msk[pp_].t[:], scalar1=flag.t[:, 1:2],
                                                                scalar2=flag.t[:, 0:1], op0=ALU.mult, op1=ALU.add),
                      r=[msk[pp_], flag], w=[me])
                else:
                    V(lambda e, me=me, pp_=pp_: e.tensor_scalar(out=me.t[:], in0=msk[pp_ - 4].t[:], scalar1=flag.t[:, 0:1],
                                                                scalar2=None, op0=ALU.mult), r=[msk[pp_ - 4], flag], w=[me])
                meff.append(me)
            kT2 = [mk(es, "kT2_%d" % i, [128, S_LEN], BF16) for i in range(2)]
            qT2 = [mk(es, "qT2_%d" % i, [128, S_LEN], BF16) for i in range(2)]
            v2 = [mk(es, "v2_%d" % i, [128, 64, 128], BF16) for i in range(2)]
            q_own = mk(es, "q_own", [128, NOWN], BF16)
            NS = 2
            E32 = [[mk(es, "E32_%d_%d" % (st_, i), [128, 512], F32) for i in range(3)] for st_ in range(NS)]
            G32 = [[mk(es, "G32_%d_%d" % (st_, i), [128, 512], F32) for i in range(2)] for st_ in range(NS)]
            Lb = [[mk(es, "Lb%d_%d" % (st_, i), [128, 512], BF16) for i in range(3)] for st_ in range(NS)]
            Ab = [[mk(es, "Ab%d_%d" % (st_, i), [128, 512], BF16) for i in range(3)] for st_ in range(NS)]
            Acc = [mk(es, "Acc%d" % st_, [128, 512], F32) for st_ in range(NS)]
            Accb = [[mk(es, "Accb%d_%d" % (st_, i), [128, 512], BF16) for i in range(3)] for st_ in range(NS)]
            ob = [[mk(es, "ob%d_%d" % (st_, i), [64, 512], BF16) for i in range(2)] for st_ in range(NS)]
            pZ = [mk(es, "pZ%d" % st_, [128, 512], F32, psum=True) for st_ in range(NS)]
            pC = [mk(es, "pC%d" % st_, [128, 512], F32, psum=True) for st_ in range(NS)]
            pO = [[mk(es, "pO%d_%d" % (st_, i), [64, 512], F32, psum=True) for i in range(1)] for st_ in range(NS)]
            pA = [mk(es, "pA%d" % st_, [128, 512], F32, psum=True) for st_ in range(NS)]
            sbv_v = sbv.rearrange("(b p) d -> p b d", p=128)
            n = 0
            nq = 0
            for hp in range(int(os.environ.get('KB_HEADS', '8')) // 2):
                kT = kT2[hp % 2]; qT = qT2[hp % 2]; v = v2[hp % 2]
                DM(lambda e, kT=kT, hp=hp: e.dma_start(out=kT.t[:], in_=sbkT[hp * 128:(hp + 1) * 128, :]), kT, w=[kT])
                DM(lambda e, qT=qT, hp=hp: e.dma_start(out=qT.t[:], in_=sbqT[hp * 128:(hp + 1) * 128, :]), qT, w=[qT], eng="gpsimd")
                for q4 in range(4):
                    DM(lambda e, v=v, hp=hp, q4=q4: e.dma_start(out=v.t[:, q4 * 16:(q4 + 1) * 16, :],
                                                                in_=sbv_v[:, q4 * 16:(q4 + 1) * 16, hp * 128:(hp + 1) * 128]),
                       v, w=[v])
                qv = qT.t[:].rearrange("p (j two t) -> p j two t", two=2, t=512)
                qo = q_own.t[:].rearrange("p (j t) -> p j t", t=512)
                G(lambda e, qv=qv, qT=qT: e.tensor_scalar(out=qv[:, :, 1, :], in0=qv[:, :, 1, :], scalar1=flag.t[:, 0:1], scalar2=None,
                                                          op0=ALU.mult), r=[qT, flag], w=[qT])
                V(lambda e, qv=qv, qo=qo, qT=qT: e.scalar_tensor_tensor(out=qo, in0=qv[:, :, 0, :], scalar=flag.t[:, 1:2],
                                                                       in1=qv[:, :, 1, :], op0=ALU.mult, op1=ALU.add),
                  r=[qT, flag], w=[q_own])
                for qt in range(int(os.environ.get('KB_QT', str(NOWN // 512)))):
                    blocks = list(reversed(range(8 * qt + 8)))
                    nkb = len(blocks)
                    pos = [pO[st_][0] for st_ in range(NS)]
                    obs = [ob[st_][nq % 2] for st_ in range(NS)]
                    nq += 1

                    def stage1(st_, idx, kb, n_, qt=qt, kT=kT):
                        dj = kb - 8 * qt
                        pz = pZ[st_]; Et = E32[st_][n_ % 3]; Lt = Lb[st_][n_ % 3]
                        p0 = st_ * 64
                        T(lambda e: e.matmul(pz.t[:], lhsT=kT.t[p0:p0 + 64, kb * 128:(kb + 1) * 128],
                                             rhs=q_own.t[p0:p0 + 64, qt * 512:(qt + 1) * 512], start=True, stop=True),
                          r=[kT, q_own], w=[pz])
                        A(lambda e: e.activation(out=Et.t[:], in_=pz.t[:], func=AF.Exp, scale=0.125), r=[pz], w=[Et])
                        if dj >= 0:
                            V(lambda e: e.tensor_tensor(out=Et.t[:], in0=Et.t[:], in1=meff[dj].t[:], op=ALU.mult),
                              r=[Et, meff[dj]], w=[Et])
                        return (idx, kb, Et, Lt, n_)

                    def stage1b(st_, item, nkb=nkb):
                        (idx, kb, Et, Lt, n_) = item
                        ac = Acc[st_]
                        A(lambda e: e.activation(out=Lt.t[:], in_=Et.t[:], func=AF.Ln, bias=onec.t[:, 0:1], scale=1.0),
                          r=[Et, onec], w=[Lt])

                    def stage1c(st_, item):
                        (idx, kb, Et, Lt, n_) = item
                        pa = pA[st_]
                        T(lambda e: e.matmul(pa.t[:], lhsT=identb.t[:], rhs=Lt.t[:], start=(idx == 0), stop=True),
                          r=[identb, Lt], w=[pa])
                        ab = Accb[st_][idx % 3]
                        V(lambda e: e.tensor_copy(out=ab.t[:], in_=pa.t[:]), r=[pa], w=[ab])

                    def stage2(st_, item):
                        (idx, kb, Et, Lt, n_) = item
                        pc = pC[st_]; g = G32[st_][n_ % 2]; a = Ab[st_][n_ % 3]
                        first = (idx == 0)
                        T(lambda e: e.matmul(pc.t[:], lhsT=tri.t[:], rhs=Lt.t[:], start=True, stop=first),
                          r=[tri, Lt], w=[pc], inc=first)
                        if not first:
                            ab = Accb[st_][(idx - 1) % 3]
                            T(lambda e: e.matmul(pc.t[:], lhsT=onesb.t[:], rhs=ab.t[:], start=False, stop=True),
                              r=[onesb, ab], w=[pc])
                        A(lambda e: e.activation(out=g.t[:], in_=pc.t[:], func=AF.Exp, scale=-1.0), r=[pc], w=[g])
                        V(lambda e: e.tensor_tensor(out=a.t[:], in0=Et.t[:], in1=g.t[:], op=ALU.mult), r=[Et, g], w=[a])
                        return (idx, kb, a)

                    def stage3(st_, item, v=v, nkb=nkb, pos=pos):
                        (idx, kb, a) = item
                        po = pos[st_]
                        last = (idx == nkb - 1)
                        T(lambda e: e.matmul(po.t[:], lhsT=v.t[:, kb, st_ * 64:(st_ + 1) * 64], rhs=a.t[:],
                                             start=(idx == 0), stop=last), r=[v, a], w=[po], inc=True)

                    s2q = [[] for _ in range(NS)]
                    s3q = [[] for _ in range(NS)]
                    pend1c = [None] * NS
                    for idx, blk in enumerate(blocks):
                        its = [stage1(st_, idx, blk, n) for st_ in range(NS)]
                        for st_ in range(NS):
                            if pend1c[st_] is not None:
                                stage1c(st_, pend1c[st_])
                                pend1c[st_] = None
                        for st_ in range(NS):
                            if s3q[st_]:
                                stage3(st_, s3q[st_].pop(0))
                        for st_ in range(NS):
                            if s2q[st_]:
                                s3q[st_].append(stage2(st_, s2q[st_].pop(0)))
                        for st_ in range(NS):
                            stage1b(st_, its[st_])
                            s2q[st_].append(its[st_])
                            if idx < nkb - 1:
                                pend1c[st_] = its[st_]
                        n += 1
                    while any(s2q) or any(s3q):
                        for st_ in range(NS):
                            if s3q[st_]:
                                stage3(st_, s3q[st_].pop(0))
                        for st_ in range(NS):
                            if s2q[st_]:
                                s3q[st_].append(stage2(st_, s2q[st_].pop(0)))
                    for st_ in range(NS):
                        po = pos[st_]; o = obs[st_]; h = hp * 2 + st_
                        V(lambda e, po=po, o=o: e.tensor_copy(out=o.t[:], in_=po.t[:]), r=[po], w=[o])
                        DM(lambda e, o=o, h=h, qt=qt: e.dma_start(out=sbo[h * 64:(h + 1) * 64, qt * 512:(qt + 1) * 512], in_=o.t[:]),
                           o, r=[o], eng="gpsimd")
                if hp % 2 == 1:
                    S.flush()
            S.flush()
        if stop_after == 2:
            return nc

        with ExitStack() as es:
            NT = S_LEN // 128
            triLE = mk(es, "triLE", [128, 128], F32)
            negm = mk(es, "negm", [128, 128], F32)
            G(lambda e: e.memset(triLE.t[:], 1.0), w=[triLE])
            G(lambda e: e.affine_select(out=triLE.t[:], in_=triLE.t[:], pattern=[[1, 128]], base=0, channel_multiplier=-1,
                                         compare_op=ALU.is_ge, fill=0.0), r=[triLE], w=[triLE])
            G(lambda e: e.memset(negm.t[:], 0.0), w=[negm])
            G(lambda e: e.affine_select(out=negm.t[:], in_=negm.t[:], pattern=[[1, 128]], base=0, channel_multiplier=-1,
                                         compare_op=ALU.is_ge, fill=NEGV), r=[negm], w=[negm])
            gbr = mk(es, "gbr", [128, 512], F32)
            DM(lambda e: e.dma_start(out=gbr.t[:], in_=gate_b_rep[:, :]), gbr, w=[gbr])
            gnb = load_bc(es, "gnb", ml_norm_g[0:1, :], 512, eng="gpsimd")
            gi = mk(es, "gi", [128, NT, 8], F32)
            for q4 in range(4):
                DM(lambda e, q4=q4: e.dma_start(out=gi.t[:, q4 * 16:(q4 + 1) * 16, :],
                                                in_=mlif.rearrange("(c p) g -> p c g", p=128)[:, q4 * 16:(q4 + 1) * 16, :]),
                   gi, w=[gi])
            V(lambda e: e.tensor_tensor(out=gi.t[:], in0=gi.t[:], in1=gbr.t[:].rearrange("p (c g) -> p c g", g=8), op=ALU.add),
              r=[gi, gbr], w=[gi])
            if os.environ.get('KC_STOP') == '1':
                S.flush(); return nc
            lf = mk(es, "lf", [128, NT, 4], F32)
            ipre = mk(es, "ipre", [128, NT, 4], F32)
            V(lambda e: e.tensor_copy(out=ipre.t[:], in_=gi.t[:, :, 0:4]), r=[gi], w=[ipre])
            A(lambda e: e.activation(out=lf.t[:], in_=gi.t[:, :, 4:8], func=AF.Exp, scale=-1.0), r=[gi], w=[lf])
            A(lambda e: e.activation(out=lf.t[:], in_=lf.t[:], func=AF.Ln, bias=onec.t[:, 0:1], scale=1.0), r=[lf, onec], w=[lf])
            V(lambda e: e.tensor_scalar(out=lf.t[:], in0=lf.t[:], scalar1=-1.0, scalar2=None, op0=ALU.mult), r=[lf], w=[lf])
            if os.environ.get('KC_STOP') == '2':
                S.flush(); return nc
            esg = ExitStack()
            pg = [mk(esg, "pg%d" % i, [128, NT * 4], F32, psum=True) for i in range(2)]
            lf2 = lf.t[:].rearrange("p c h -> p (c h)")
            T(lambda e: e.matmul(pg[0].t[:], lhsT=triLE.t[:], rhs=lf2, start=True, stop=True), r=[triLE, lf], w=[pg[0]])
            T(lambda e: e.matmul(pg[1].t[:], lhsT=onesf.t[:], rhs=lf2, start=True, stop=True), r=[onesf, lf], w=[pg[1]])
            av = mk(es, "av", [128, NT * 4], F32)
            civ = mk(es, "civ", [128, NT * 4], F32)
            wkv = mk(es, "wkv", [128, NT * 4], F32)
            dec = mk(es, "dec", [128, NT * 4], F32)
            ip2 = ipre.t[:].rearrange("p c h -> p (c h)")
            if os.environ.get('KC_STOP') == '3':
                S.flush(); return nc
            bsb = mk(es, "bsb", [128, NT * 4], F32)
            blsb = mk(es, "blsb", [128, NT * 4], F32)
            A(lambda e: e.activation(out=bsb.t[:], in_=pg[0].t[:], func=AF.Copy), r=[pg[0]], w=[bsb])
            A(lambda e: e.activation(out=blsb.t[:], in_=pg[1].t[:], func=AF.Copy), r=[pg[1]], w=[blsb])
            A(lambda e: e.activation(out=av.t[:], in_=bsb.t[:], func=AF.Exp), r=[bsb], w=[av])
            A(lambda e: e.activation(out=dec.t[:], in_=blsb.t[:], func=AF.Exp), r=[blsb], w=[dec])
            V(lambda e: e.tensor_tensor(out=civ.t[:], in0=ip2, in1=bsb.t[:], op=ALU.subtract), r=[ipre, bsb], w=[civ])
            V(lambda e: e.tensor_tensor(out=wkv.t[:], in0=civ.t[:], in1=blsb.t[:], op=ALU.add), r=[civ, blsb], w=[wkv])
            A(lambda e: e.activation(out=wkv.t[:], in_=wkv.t[:], func=AF.Exp), r=[wkv], w=[wkv])
            S.flush()
            esg.close()
            if os.environ.get('KC_STOP') == '4':
                return nc

            qTt = [mk(es, "qTt%d" % i, [128, 4, 512], BF16) for i in range(2)]
            kTt = [mk(es, "kTt%d" % i, [128, 4, 512], BF16) for i in range(2)]
            vp = [mk(es, "vp%d" % i, [128, 4, 130], BF16) for i in range(2)]
            sgo = [mk(es, "sgo%d" % i, [128, 512], F32) for i in range(2)]
            mot = [mk(es, "mot%d" % i, [128, 512], BF16) for i in range(2)]
            for i in range(2):
                G(lambda e, i=i: e.memset(vp[i].t[:], 1.0), w=[vp[i]])
            trl = [mk(es, "trl%d" % i, [128, 128], F32) for i in range(2)]
            Dt = [mk(es, "Dt%d" % i, [128, 128], F32) for i in range(2)]
            Wt = [mk(es, "Wt%d" % i, [128, 128], BF16) for i in range(4)]
            ktok = [mk(es, "ktok%d" % i, [128, 128], BF16) for i in range(4)]
            vw = [mk(es, "vw%d" % i, [128, 130], BF16) for i in range(4)]
            Hs = [mk(es, "Hs%d" % i, [128, 130], F32) for i in range(2)]
            num = [mk(es, "num%d" % i, [128, 130], F32) for i in range(2)]
            rr = [mk(es, "rr%d" % i, [128, 2], F32) for i in range(2)]
            hsc = [mk(es, "hsc%d" % i, [128, 128], F32) for i in range(2)]
            hmv = [mk(es, "hmv%d" % i, [128, 4], F32) for i in range(2)]
            hscr = mk(es, "hscr", [128, 6], F32)
            S32 = [mk(es, "S32_%d" % i, [128, 130], F32) for i in range(4)]
            Sbf = [mk(es, "Sbf%d" % i, [128, 130], BF16) for i in range(4)]
            pST = [mk(es, "pST%d" % i, [128, 128], F32, psum=True) for i in range(2)]
            pB = [mk(es, "pB%d" % i, [128, 128], F32, psum=True) for i in range(2)]
            pK = mk(es, "pK", [128, 128], BF16, psum=True)
            pHa = mk(es, "pHa", [128, 130], F32, psum=True)
            pHb = mk(es, "pHb", [128, 130], F32, psum=True)
            pU = mk(es, "pU", [128, 130], F32, psum=True)
            mlqv = mlqT.rearrange("(h k) t -> k h t", k=128)
            mlkv = mlkT.rearrange("(h k) t -> k h t", k=128)
            u_ = 0
            for c in range(int(os.environ.get('KC_NT', str(NT)))):
                g4 = c // 4
                qq = qTt[g4 % 2]; kk = kTt[g4 % 2]
                if c % 4 == 0:
                    DM(lambda e, qq=qq, g4=g4: e.dma_start(out=qq.t[:], in_=mlqv[:, :, g4 * 512:(g4 + 1) * 512]), qq, w=[qq])
                    DM(lambda e, kk=kk, g4=g4: e.dma_start(out=kk.t[:], in_=mlkv[:, :, g4 * 512:(g4 + 1) * 512]), kk, w=[kk])
                vv = vp[c % 2]; so = sgo[c % 2]; mo = mot[c % 2]
                DM(lambda e, vv=vv, c=c: e.dma_start(out=vv.t[:, :, 0:128],
                                                     in_=mlv[c * 128:(c + 1) * 128, :].rearrange("p (h d) -> p h d", d=128)),
                   vv, w=[vv], eng="gpsimd")
                DM(lambda e, so=so, c=c: e.dma_start(out=so.t[:], in_=mlo[c * 128:(c + 1) * 128, :]), so, w=[so], eng="gpsimd")
                t0 = (c % 4) * 128
                for hh in range(8):
                    h = hh % 4
                    back = hh >= 4
                    b2 = h % 2
                    col = c * 4 + h
                    kslc = lambda kk=kk, h=h, t0=t0: kk.t[:, h, t0:t0 + 128]
                    qslc = lambda qq=qq, h=h, t0=t0: qq.t[:, h, t0:t0 + 128]
                    ps = pST[b2]; pb = pB[b2]; tl = trl[b2]; dt_ = Dt[b2]; wt = Wt[h]; kt_ = ktok[h]; vw_ = vw[h]
                    hs = Hs[b2]; nm = num[b2]; r_ = rr[b2]; hc = hsc[b2]; hm = hmv[b2]
                    if not back:
                        phase_c_front(T, V, A, G, ps, pb, tl, dt_, wt, kt_, vw_, kslc, qslc, kk, qq, vv, h, col, triLE, lf, lf2, onesf,
                                      identf, identb, negm, civ, wkv, pK)
                        continue
                    T(lambda e, wt=wt, vv=vv, h=h: e.matmul(pHa.t[:, 0:129], lhsT=wt.t[:], rhs=vv.t[:, h, 0:129],
                                                            start=True, stop=True), r=[wt, vv], w=[pHa])
                    if c > 0:
                        T(lambda e, qslc=qslc, h=h: e.matmul(pHb.t[:, 0:129], lhsT=qslc(), rhs=Sbf[h].t[:, 0:129],
                                                             start=True, stop=True), r=[qq, Sbf[h]], w=[pHb])
                        A(lambda e, hs=hs, col=col: e.activation(out=hs.t[:, 0:129], in_=pHb.t[:, 0:129], func=AF.Identity,
                                                                 scale=av.t[:, col:col + 1]), r=[pHb, av], w=[hs])
                        V(lambda e, nm=nm, hs=hs: e.tensor_tensor(out=nm.t[:, 0:129], in0=pHa.t[:, 0:129], in1=hs.t[:, 0:129],
                                                                  op=ALU.add), r=[hs, pHa], w=[nm])
                    else:
                        V(lambda e, nm=nm: e.tensor_copy(out=nm.t[:, 0:129], in_=pHa.t[:, 0:129]), r=[pHa], w=[nm])
                    T(lambda e, kt_=kt_, vw_=vw_: e.matmul(pU.t[:, 0:129], lhsT=kt_.t[:], rhs=vw_.t[:, 0:129],
                                                           start=True, stop=True), r=[kt_, vw_], w=[pU])
                    V(lambda e, nm=nm, r_=r_: e.tensor_scalar(out=r_.t[:, 0:1], in0=nm.t[:, 128:129], scalar1=-1.0,
                                                              scalar2=nm.t[:, 128:129], op0=ALU.mult, op1=ALU.max),
                      r=[nm], w=[r_])
                    V(lambda e, r_=r_: e.tensor_scalar_max(out=r_.t[:, 0:1], in0=r_.t[:, 0:1], scalar1=1.0), r=[r_], w=[r_])
                    V(lambda e, r_=r_: e.reciprocal(out=r_.t[:, 1:2], in_=r_.t[:, 0:1]), r=[r_], w=[r_])
                    V(lambda e, hc=hc, nm=nm, r_=r_: e.tensor_scalar(out=hc.t[:], in0=nm.t[:, 0:128], scalar1=r_.t[:, 1:2],
                                                                     scalar2=None, op0=ALU.mult), r=[nm, r_], w=[hc])
                    layer_norm_stats(hc, hm, hscr, 128)
                    V(lambda e, hc=hc, hm=hm: e.tensor_scalar(out=hc.t[:], in0=hc.t[:], scalar1=hm.t[:, 0:1], scalar2=hm.t[:, 1:2],
                                                              op0=ALU.subtract, op1=ALU.mult), r=[hc, hm], w=[hc])
                    G(lambda e, hc=hc, h=h: e.tensor_tensor(out=hc.t[:], in0=hc.t[:], in1=gnb.t[:, h * 128:(h + 1) * 128],
                                                            op=ALU.mult), r=[hc, gnb], w=[hc])
                    G(lambda e, hc=hc, h=h, so=so, mo=mo: e.tensor_tensor(out=mo.t[:, h * 128:(h + 1) * 128], in0=hc.t[:],
                                                                          in1=so.t[:, h * 128:(h + 1) * 128], op=ALU.mult),
                      r=[hc, so], w=[mo])
                    if c == 0:
                        V(lambda e, h=h: e.tensor_copy(out=S32[h].t[:, 0:129], in_=pU.t[:, 0:129]), r=[pU], w=[S32[h]])
                    else:
                        V(lambda e, h=h, col=col: e.scalar_tensor_tensor(out=S32[h].t[:, 0:129], in0=S32[h].t[:, 0:129],
                                                                         scalar=dec.t[:, col:col + 1], in1=pU.t[:, 0:129],
                                                                         op0=ALU.mult, op1=ALU.add),
                          r=[S32[h], dec, pU], w=[S32[h]])
                    A(lambda e, h=h: e.activation(out=Sbf[h].t[:, 0:129], in_=S32[h].t[:, 0:129], func=AF.Copy),
                      r=[S32[h]], w=[Sbf[h]])
                DM(lambda e, mo=mo, c=c: e.dma_start(out=mlout[c * 128:(c + 1) * 128, :], in_=mo.t[:]), mo, r=[mo], eng="scalar")
            S.flush()
        if stop_after == 3:
            return nc

        with ExitStack() as es:
            woutb = mk(es, "woutb", [128, 8, D], BF16)
            wrt = mk(es, "wrt", [128, 8, NE], F32)
            DM(lambda e: e.dma_start(out=wrt.t[:], in_=w_router.rearrange("(kt k) c -> k kt c", k=128)), wrt, w=[wrt])
            with ExitStack() as es2:
                wst = [mk(es2, "wost%d" % i, [128, D], F32) for i in range(2)]
                wov = w_out.rearrange("(kt k) c -> k kt c", k=128)
                for kt in range(8):
                    w = wst[kt % 2]
                    DM(lambda e, w=w, kt=kt: e.dma_start(out=w.t[:], in_=wov[:, kt, :]), w, w=[w])
                    V(lambda e, w=w, kt=kt: e.tensor_copy(out=woutb.t[:, kt, :], in_=w.t[:]), r=[w], w=[woutb])
                S.flush()
            g1b = load_bc(es, "g1b", mod_s[0:1, 2 * D:3 * D], D)
            sh2 = load_bc(es, "sh2", mod_s[0:1, 3 * D:4 * D], D, eng="gpsimd")
            sc2p = load_bc(es, "sc2p", mod_s[0:1, 4 * D:5 * D], D)
            l1g = load_bc(es, "l1g", ln1_g[0:1, :], D, eng="gpsimd")
            l1b = load_bc(es, "l1b", ln1_b[0:1, :], D)
            rbb = load_bc(es, "rbb", router_bias[0:1, :], NE, eng="gpsimd")
            sA = [mk(es, "sA%d" % i, [128, 4, 128], BF16) for i in range(2)]
            sB = [mk(es, "sB%d" % i, [128, 4, 128], BF16) for i in range(2)]
            mA = [mk(es, "mA%d" % i, [128, 512], BF16) for i in range(2)]
            mB = [mk(es, "mB%d" % i, [128, 512], BF16) for i in range(2)]
            ssel = [mk(es, "ssel%d" % i, [128, 4, 128], BF16) for i in range(2)]
            msel = [mk(es, "msel%d" % i, [128, 512], BF16) for i in range(2)]
            mlT = [mk(es, "mlT%d" % i, [128, 4, 128], BF16) for i in range(2)]
            xo = [mk(es, "xo%d" % i, [128, D], F32) for i in range(2)]
            tt = [mk(es, "tt%d" % i, [128, D], F32) for i in range(2)]
            x1 = [mk(es, "x1_%d" % i, [128, D], F32) for i in range(2)]
            u2 = [mk(es, "u2_%d" % i, [128, D], F32) for i in range(2)]
            u2b = [mk(es, "u2b%d" % i, [128, D], BF16) for i in range(2)]
            u2T = [mk(es, "u2T%d" % i, [128, 8, 128], BF16) for i in range(2)]
            u2Tf = [mk(es, "u2Tf%d" % i, [128, 8, 128], F32) for i in range(2)]
            scr = mk(es, "scrD", [128, 12], F32)
            mvd = [mk(es, "mvd%d" % i, [128, 4], F32) for i in range(4)]
            scs = [mk(es, "scs%d" % i, [128, NE], F32) for i in range(2)]
            sel = [mk(es, "sel%d" % i, [128, NE], F32) for i in range(2)]
            selm = [mk(es, "selm%d" % i, [128, NE], F32) for i in range(2)]
            m8 = [mk(es, "m8_%d" % i, [128, 8, 8], F32) for i in range(2)]
            grp = [mk(es, "grp%d" % i, [128, 8], F32) for i in range(2)]
            gs8 = [mk(es, "gs8_%d" % i, [128, 8], F32) for i in range(2)]
            gmk = [mk(es, "gmk%d" % i, [128, 8], F32) for i in range(2)]
            t8 = [mk(es, "t8_%d" % i, [128, 8], F32) for i in range(2)]
            dn = [mk(es, "dn%d" % i, [128, 2], F32) for i in range(2)]
            gt = [mk(es, "gt%d" % i, [128, NE + 1], F32) for i in range(2)]
            for i in range(2):
                G(lambda e, i=i: e.memset(gt[i].t[:, NE:NE + 1], 1.0), w=[gt[i]])
            pM = [mk(es, "pM%d" % i, [128, 512], F32, psum=True) for i in range(2)]
            pT2 = mk(es, "pT2", [128, 8, 128], BF16, psum=True)
            pTf = [mk(es, "pTf%d" % i, [128, 4, 128], F32, psum=True) for i in range(2)]
            pT = mk(es, "pTd", [128, 4, 128], BF16, psum=True)
            pR = mk(es, "pR", [128, NE], F32, psum=True)
            sbov = sbo.rearrange("(ft f) t -> f ft t", f=128)
            for i in range(NOWN // 128):
                b2 = i % 2
                a_ = sA[b2]; b_ = sB[b2]; ma = mA[b2]; mb = mB[b2]; ss = ssel[b2]; ms = msel[b2]; mt = mlT[b2]
                x = xo[b2]; t_ = tt[b2]; x1_ = x1[b2]; u_2 = u2[b2]; ub_ = u2b[b2]; uT_ = u2T[b2]; uTf = u2Tf[b2]
                DM(lambda e, a_=a_, i=i: e.dma_start(out=a_.t[:], in_=sbov[:, :, i * 128:(i + 1) * 128]), a_, w=[a_])
                rA = (8 * (i // 4) + (i % 4)) * 128
                rB = rA + 512
                DM(lambda e, ma=ma, rA=rA: e.dma_start(out=ma.t[:], in_=mlout[rA:rA + 128, :]), ma, w=[ma], eng="gpsimd")
                DM(lambda e, mb=mb, rB=rB: e.dma_start(out=mb.t[:], in_=mlout[rB:rB + 128, :]), mb, w=[mb], eng="gpsimd")
                DM(lambda e, x=x, i=i: e.dma_start(out=x.t[:], in_=x_own[i * 128:(i + 1) * 128, :]), x, w=[x])
                a2 = a_.t[:].rearrange("p a b -> p (a b)"); b2_ = b_.t[:].rearrange("p a b -> p (a b)")
                s2 = ss.t[:].rearrange("p a b -> p (a b)")
                ss = a_
                G(lambda e, mb=mb: e.tensor_scalar(out=mb.t[:], in0=mb.t[:], scalar1=flag.t[:, 0:1], scalar2=None, op0=ALU.mult),
                  r=[mb, flag], w=[mb])
                V(lambda e, ms=ms, ma=ma, mb=mb: e.scalar_tensor_tensor(out=ms.t[:], in0=ma.t[:], scalar=flag.t[:, 1:2], in1=mb.t[:],
                                                                       op0=ALU.mult, op1=ALU.add), r=[ma, mb, flag], w=[ms])
                for k in range(4):
                    T(lambda e, ms=ms, k=k: e.transpose(out=pT.t[:, k, :], in_=ms.t[:, k * 128:(k + 1) * 128], identity=identb.t[:]),
                      r=[ms, identb], w=[pT], inc=(k == 3))
                A(lambda e, mt=mt: e.activation(out=mt.t[:], in_=pT.t[:], func=AF.Copy), r=[pT], w=[mt])
                for ch in range(2):
                    p = pM[ch]
                    for ft in range(8):
                        src = ss if ft < 4 else mt
                        T(lambda e, p=p, src=src, ft=ft, ch=ch: e.matmul(p.t[:], lhsT=src.t[:, ft % 4, :],
                                                                         rhs=woutb.t[:, ft, ch * 512:(ch + 1) * 512],
                                                                         start=(ft == 0), stop=(ft == 7)),
                          r=[src, woutb], w=[p], inc=(ft == 7))
                    V(lambda e, p=p, t_=t_, ch=ch: e.tensor_tensor(out=t_.t[:, ch * 512:(ch + 1) * 512], in0=p.t[:],
                                                                   in1=g1b.t[:, ch * 512:(ch + 1) * 512], op=ALU.mult),
                      r=[p, g1b], w=[t_])
                V(lambda e, t_=t_, x=x: e.scalar_tensor_tensor(out=t_.t[:], in0=x.t[:], scalar=ALPHA, in1=t_.t[:],
                                                               op0=ALU.mult, op1=ALU.add), r=[x, t_], w=[t_])
                m1 = mvd[(2 * i) % 4]; m2 = mvd[(2 * i + 1) % 4]
                layer_norm_stats(t_, m1, scr, D)
                V(lambda e, t_=t_, m1=m1: e.tensor_scalar(out=t_.t[:], in0=t_.t[:], scalar1=m1.t[:, 0:1], scalar2=m1.t[:, 1:2],
                                                          op0=ALU.subtract, op1=ALU.mult), r=[t_, m1], w=[t_])
                G(lambda e, t_=t_: e.tensor_tensor(out=t_.t[:], in0=t_.t[:], in1=l1g.t[:], op=ALU.mult), r=[t_, l1g], w=[t_])
                V(lambda e, t_=t_, x1_=x1_: e.tensor_tensor(out=x1_.t[:], in0=t_.t[:], in1=l1b.t[:], op=ALU.add),
                  r=[t_, l1b], w=[x1_])
                DM(lambda e, x1_=x1_, i=i: e.dma_start(out=x1s[i * 128:(i + 1) * 128, :], in_=x1_.t[:]), x1_, r=[x1_], eng="scalar")
                layer_norm_stats(x1_, m2, scr, D)
                V(lambda e, u_2=u_2, x1_=x1_, m2=m2: e.tensor_scalar(out=u_2.t[:], in0=x1_.t[:], scalar1=m2.t[:, 0:1],
                                                                     scalar2=m2.t[:, 1:2], op0=ALU.subtract, op1=ALU.mult),
                  r=[x1_, m2], w=[u_2])
                G(lambda e, u_2=u_2: e.tensor_tensor(out=u_2.t[:], in0=u_2.t[:], in1=sc2p.t[:], op=ALU.mult), r=[u_2, sc2p], w=[u_2])
                V(lambda e, u_2=u_2: e.tensor_tensor(out=u_2.t[:], in0=u_2.t[:], in1=sh2.t[:], op=ALU.add), r=[u_2, sh2], w=[u_2])
                G(lambda e, u_2=u_2, ub_=ub_: e.tensor_copy(out=ub_.t[:], in_=u_2.t[:]), r=[u_2], w=[ub_])
                DM(lambda e, ub_=ub_, i=i: e.dma_start(out=u2s[i * 128:(i + 1) * 128, :], in_=ub_.t[:]), ub_, r=[ub_], eng="scalar")
                for kt in range(8):
                    T(lambda e, ub_=ub_, kt=kt: e.transpose(out=pT2.t[:, kt, :], in_=ub_.t[:, kt * 128:(kt + 1) * 128],
                                                            identity=identb.t[:]), r=[ub_, identb], w=[pT2], inc=(kt == 7))
                A(lambda e, uT_=uT_: e.activation(out=uT_.t[:], in_=pT2.t[:], func=AF.Copy), r=[pT2], w=[uT_])
                DM(lambda e, uT_=uT_, i=i: e.dma_start(out=u2Ts[:, :, i * 128:(i + 1) * 128], in_=uT_.t[:]), uT_, r=[uT_],
                   eng="gpsimd")
                for hf in range(2):
                    pf = pTf[hf]
                    for k in range(4):
                        kt = hf * 4 + k
                        T(lambda e, pf=pf, u_2=u_2, k=k, kt=kt: e.transpose(out=pf.t[:, k, :], in_=u_2.t[:, kt * 128:(kt + 1) * 128],
                                                                           identity=identf.t[:]),
                          r=[u_2, identf], w=[pf], inc=(k == 3))
                    A(lambda e, pf=pf, uTf=uTf, hf=hf: e.activation(out=uTf.t[:, hf * 4:(hf + 1) * 4, :], in_=pf.t[:], func=AF.Copy),
                      r=[pf], w=[uTf])
                for kt in range(8):
                    T(lambda e, uTf=uTf, kt=kt: e.matmul(pR.t[:], lhsT=uTf.t[:, kt, :], rhs=wrt.t[:, kt, :],
                                                         start=(kt == 0), stop=(kt == 7)), r=[uTf, wrt], w=[pR], inc=(kt == 7))
                sc_ = scs[b2]; sl_ = sel[b2]; sm_ = selm[b2]; m8_ = m8[b2]; gr_ = grp[b2]; g8_ = gs8[b2]; gm_ = gmk[b2]
                t8_ = t8[b2]; dn_ = dn[b2]; gt_ = gt[b2]
                A(lambda e, sc_=sc_: e.activation(out=sc_.t[:], in_=pR.t[:], func=AF.Sigmoid), r=[pR], w=[sc_])
                V(lambda e, sc_=sc_, sl_=sl_: e.tensor_tensor(out=sl_.t[:], in0=sc_.t[:], in1=rbb.t[:], op=ALU.add),
                  r=[sc_, rbb], w=[sl_])
                for g in range(8):
                    V(lambda e, sl_=sl_, m8_=m8_, g=g: e.max(out=m8_.t[:, g, :], in_=sl_.t[:, g * 32:(g + 1) * 32]), r=[sl_], w=[m8_])
                V(lambda e, m8_=m8_, gr_=gr_: e.tensor_tensor(out=gr_.t[:], in0=m8_.t[:, :, 0], in1=m8_.t[:, :, 1], op=ALU.add),
                  r=[m8_], w=[gr_])
                V(lambda e, gr_=gr_, g8_=g8_: e.max(out=g8_.t[:], in_=gr_.t[:]), r=[gr_], w=[g8_])
                V(lambda e, gr_=gr_, g8_=g8_, gm_=gm_: e.tensor_scalar(out=gm_.t[:], in0=gr_.t[:], scalar1=g8_.t[:, 3:4], scalar2=None,
                                                                       op0=ALU.is_ge), r=[gr_, g8_], w=[gm_])
                for g in range(8):
                    V(lambda e, sl_=sl_, sm_=sm_, gm_=gm_, g=g: e.tensor_scalar(out=sm_.t[:, g * 32:(g + 1) * 32],
                                                                               in0=sl_.t[:, g * 32:(g + 1) * 32], scalar1=2.0,
                                                                               scalar2=gm_.t[:, g:g + 1], op0=ALU.add, op1=ALU.mult),
                      r=[sl_, gm_], w=[sm_])
                V(lambda e, sm_=sm_, t8_=t8_: e.max(out=t8_.t[:], in_=sm_.t[:]), r=[sm_], w=[t8_])
                V(lambda e, sm_=sm_, t8_=t8_: e.tensor_scalar(out=sm_.t[:], in0=sm_.t[:], scalar1=t8_.t[:, 7:8], scalar2=None,
                                                              op0=ALU.is_ge), r=[sm_, t8_], w=[sm_])
                G(lambda e, sm_=sm_, i=i: e.tensor_copy(out=Mall.t[:, i, :], in_=sm_.t[:]), r=[sm_], w=[Mall])
                V(lambda e, sm_=sm_, sc_=sc_: e.tensor_tensor(out=sm_.t[:], in0=sm_.t[:], in1=sc_.t[:], op=ALU.mult),
                  r=[sm_, sc_], w=[sm_])
                V(lambda e, sm_=sm_, dn_=dn_: e.reduce_sum(out=dn_.t[:, 0:1], in_=sm_.t[:], axis=mybir.AxisListType.X),
                  r=[sm_], w=[dn_])
                V(lambda e, dn_=dn_: e.reciprocal(out=dn_.t[:, 1:2], in_=dn_.t[:, 0:1]), r=[dn_], w=[dn_])
                V(lambda e, sm_=sm_, dn_=dn_, gt_=gt_: e.tensor_scalar(out=gt_.t[:, 0:NE], in0=sm_.t[:], scalar1=dn_.t[:, 1:2],
                                                                       scalar2=2.5, op0=ALU.mult, op1=ALU.mult),
                  r=[sm_, dn_], w=[gt_])
                DM(lambda e, gt_=gt_, i=i: e.dma_start(out=gates_s[:, i, :], in_=gt_.t[:]), gt_, r=[gt_], eng="scalar")
            S.flush()
        if stop_after == 4:
            return nc

        if sparse:
            phase_e_sparse(nc, S, mk, V, A, G, T, DM, load_bc, layer_norm_stats, Mall, identb, identf, onesf, onesb,
                           dict(mod_s=mod_s, ln2_g=ln2_g, ln2_b=ln2_b, gates_s=gates_s, u2s=u2s, u2Ts=u2Ts, x1s=x1s, Xs=Xs, Ys=Ys,
                                moe_w1=moe_w1, moe_w3=moe_w3, moe_w2=moe_w2, out=out), n_decl)
            return nc
        with ExitStack() as es:
            NP_TOK = 1024
            g2b = load_bc(es, "g2b", mod_s[0:1, 5 * D:6 * D], D)
            l2g = load_bc(es, "l2g", ln2_g[0:1, :], D, eng="gpsimd")
            l2b = load_bc(es, "l2b", ln2_b[0:1, :], D)
            uTp = mk(es, "uTp", [128, 8, NP_TOK], BF16)
            gtp = mk(es, "gtp", [128, 8, NE + 1], F32)
            yacc = mk(es, "yacc", [128, 8, D], F32)
            st1 = mk(es, "st1", [128, 8, 256], F32)
            st3 = mk(es, "st3", [128, 8, 256], F32)
            st2 = mk(es, "st2", [128, 2, D], F32)
            w1b = [mk(es, "w1b%d" % i, [128, 8, 256], BF16) for i in range(2)]
            w3b = [mk(es, "w3b%d" % i, [128, 8, 256], BF16) for i in range(2)]
            w2b = [mk(es, "w2b%d" % i, [128, 2, D], BF16) for i in range(2)]
            s1 = [mk(es, "s1_%d" % i, [128, 512], F32) for i in range(4)]
            hT = [mk(es, "hT%d" % i, [128, 512], BF16) for i in range(4)]
            ph1 = [mk(es, "ph1_%d" % i, [128, 512], F32, psum=True) for i in range(2)]
            ph3 = [mk(es, "ph3_%d" % i, [128, 512], F32, psum=True) for i in range(2)]
            py = [[mk(es, "py%d_%d" % (i, j), [128, 512], F32, psum=True) for j in range(2)] for i in range(2)]
            xl = [mk(es, "xl%d" % i, [128, D], F32) for i in range(2)]
            ot = [mk(es, "ot%d" % i, [128, D], F32) for i in range(2)]
            mve = [mk(es, "mve%d" % i, [128, 4], F32) for i in range(2)]
            scr = mk(es, "scrE", [128, 12], F32)
            w1v = moe_w1.rearrange("e (kt k) f -> e k kt f", k=128)
            w3v = moe_w3.rearrange("e (kt k) f -> e k kt f", k=128)
            w2v = moe_w2.rearrange("e (ft f) d -> e f ft d", f=128)
            nh = 0
            nyy = 0
            for ps_ in range(NOWN // NP_TOK):
                tok0 = ps_ * NP_TOK
                DM(lambda e, tok0=tok0: e.dma_start(out=uTp.t[:], in_=u2Ts[:, :, tok0:tok0 + NP_TOK]), uTp, w=[uTp])
                DM(lambda e, ps_=ps_: e.dma_start(out=gtp.t[:], in_=gates_s[:, ps_ * 8:(ps_ + 1) * 8, :]), gtp, w=[gtp], eng="gpsimd")
                for ei in range(n_exp):
                    ex = ei if ei < n_exp - 1 else NE
                    exw = ei if ei < n_exp - 1 else n_decl - 1
                    eb = ei % 2
                    a1 = w1b[eb]; a3 = w3b[eb]; a2 = w2b[eb]
                    DM(lambda e, exw=exw: e.dma_start(out=st1.t[:], in_=w1v[exw]), st1, w=[st1])
                    DM(lambda e, exw=exw: e.dma_start(out=st3.t[:], in_=w3v[exw]), st3, w=[st3], eng="gpsimd")
                    DM(lambda e, exw=exw: e.dma_start(out=st2.t[:], in_=w2v[exw]), st2, w=[st2])
                    G(lambda e, a1=a1: e.tensor_copy(out=a1.t[:], in_=st1.t[:]), r=[st1], w=[a1])
                    G(lambda e, a3=a3: e.tensor_copy(out=a3.t[:], in_=st3.t[:]), r=[st3], w=[a3])
                    G(lambda e, a2=a2: e.tensor_copy(out=a2.t[:], in_=st2.t[:]), r=[st2], w=[a2])
                    for tb in range(NP_TOK // 512):
                        hts = []
                        for ft in range(2):
                            p1 = ph1[ft]; p3 = ph3[ft]
                            for kt in range(8):
                                T(lambda e, p1=p1, a1=a1, kt=kt, ft=ft, tb=tb: e.matmul(
                                    p1.t[:], lhsT=a1.t[:, kt, ft * 128:(ft + 1) * 128], rhs=uTp.t[:, kt, tb * 512:(tb + 1) * 512],
                                    start=(kt == 0), stop=(kt == 7)), r=[a1, uTp], w=[p1], inc=(kt == 7))
                            for kt in range(8):
                                T(lambda e, p3=p3, a3=a3, kt=kt, ft=ft, tb=tb: e.matmul(
                                    p3.t[:], lhsT=a3.t[:, kt, ft * 128:(ft + 1) * 128], rhs=uTp.t[:, kt, tb * 512:(tb + 1) * 512],
                                    start=(kt == 0), stop=(kt == 7)), r=[a3, uTp], w=[p3], inc=(kt == 7))
                            s_ = s1[nh % 4]; h_ = hT[nh % 4]; nh += 1
                            A(lambda e, p1=p1, s_=s_: e.activation(out=s_.t[:], in_=p1.t[:], func=AF.Silu), r=[p1], w=[s_])
                            V(lambda e, s_=s_, p3=p3, h_=h_: e.tensor_tensor(out=h_.t[:], in0=p3.t[:], in1=s_.t[:], op=ALU.mult),
                              r=[s_, p3], w=[h_])
                            hts.append(h_)
                        for tq in range(4):
                            tl = tb * 4 + tq
                            pp = py[nyy % 2]; nyy += 1
                            for dc in range(2):
                                for ft in range(2):
                                    T(lambda e, pp=pp, dc=dc, ft=ft, tq=tq, hts=hts, a2=a2: e.matmul(
                                        pp[dc].t[:], lhsT=hts[ft].t[:, tq * 128:(tq + 1) * 128],
                                        rhs=a2.t[:, ft, dc * 512:(dc + 1) * 512], start=(ft == 0), stop=(ft == 1)),
                                      r=[hts[ft], a2], w=[pp[dc]], inc=(ft == 1))
                                if ei == 0:
                                    V(lambda e, pp=pp, dc=dc, tl=tl, ex=ex: e.tensor_scalar(
                                        out=yacc.t[:, tl, dc * 512:(dc + 1) * 512], in0=pp[dc].t[:],
                                        scalar1=gtp.t[:, tl, ex:ex + 1], scalar2=None, op0=ALU.mult),
                                      r=[pp[dc], gtp], w=[yacc])
                                else:
                                    V(lambda e, pp=pp, dc=dc, tl=tl, ex=ex: e.scalar_tensor_tensor(
                                        out=yacc.t[:, tl, dc * 512:(dc + 1) * 512], in0=pp[dc].t[:],
                                        scalar=gtp.t[:, tl, ex:ex + 1], in1=yacc.t[:, tl, dc * 512:(dc + 1) * 512],
                                        op0=ALU.mult, op1=ALU.add), r=[pp[dc], gtp, yacc], w=[yacc])
                    if ei % 32 == 31:
                        S.flush()
                for tl in range(8):
                    r0 = tok0 + tl * 128
                    xl_ = xl[tl % 2]; o_ = ot[tl % 2]; m_ = mve[tl % 2]
                    DM(lambda e, xl_=xl_, r0=r0: e.dma_start(out=xl_.t[:], in_=x1s[r0:r0 + 128, :]), xl_, w=[xl_])
                    G(lambda e, tl=tl: e.tensor_tensor(out=yacc.t[:, tl, :], in0=yacc.t[:, tl, :], in1=g2b.t[:], op=ALU.mult),
                      r=[yacc, g2b], w=[yacc])
                    V(lambda e, tl=tl, xl_=xl_: e.scalar_tensor_tensor(out=xl_.t[:], in0=xl_.t[:], scalar=ALPHA, in1=yacc.t[:, tl, :],
                                                                       op0=ALU.mult, op1=ALU.add), r=[xl_, yacc], w=[xl_])
                    layer_norm_stats(xl_, m_, scr, D)
                    V(lambda e, xl_=xl_, m_=m_: e.tensor_scalar(out=xl_.t[:], in0=xl_.t[:], scalar1=m_.t[:, 0:1], scalar2=m_.t[:, 1:2],
                                                                op0=ALU.subtract, op1=ALU.mult), r=[xl_, m_], w=[xl_])
                    G(lambda e, xl_=xl_: e.tensor_tensor(out=xl_.t[:], in0=xl_.t[:], in1=l2g.t[:], op=ALU.mult), r=[xl_, l2g], w=[xl_])
                    V(lambda e, xl_=xl_, o_=o_: e.tensor_tensor(out=o_.t[:], in0=xl_.t[:], in1=l2b.t[:], op=ALU.add),
                      r=[xl_, l2b], w=[o_])
                    DM(lambda e, o_=o_, r0=r0: e.dma_start(out=out[r0:r0 + 128, :], in_=o_.t[:]), o_, r=[o_])
            S.flush()
    return nc


def phase_c_front(T, V, A, G, ps, pb, tl, dt_, wt, kt_, vw_, kslc, qslc, kk, qq, vv, h, col, triLE, lf, lf2, onesf, identf, identb,
                  negm, civ, wkv, pK):
    T(lambda e, ps=ps, kslc=kslc, qslc=qslc: e.matmul(ps.t[:], lhsT=kslc(), rhs=qslc(), start=True, stop=True),
      r=[kk, qq], w=[ps])
    V(lambda e, tl=tl, col=col: e.tensor_scalar(out=tl.t[:], in0=triLE.t[:], scalar1=lf2[:, col:col + 1],
                                                scalar2=None, op0=ALU.mult), r=[triLE, lf], w=[tl])
    T(lambda e, pb=pb, tl=tl: e.matmul(pb.t[:], lhsT=onesf.t[:], rhs=tl.t[:], start=True, stop=False),
      r=[onesf, tl], w=[pb], inc=False)
    T(lambda e, pb=pb: e.matmul(pb.t[:], lhsT=identf.t[:], rhs=negm.t[:], start=False, stop=True),
      r=[identf, negm], w=[pb])
    A(lambda e, pb=pb, dt_=dt_, col=col: e.activation(out=dt_.t[:], in_=pb.t[:], func=AF.Exp,
                                                     bias=civ.t[:, col:col + 1], scale=1.0),
      r=[pb, civ], w=[dt_])
    V(lambda e, wt=wt, ps=ps, dt_=dt_: e.tensor_tensor(out=wt.t[:], in0=ps.t[:], in1=dt_.t[:], op=ALU.mult),
      r=[ps, dt_], w=[wt])
    T(lambda e, kslc=kslc: e.transpose(out=pK.t[:], in_=kslc(), identity=identb.t[:]), r=[kk, identb], w=[pK])
    A(lambda e, kt_=kt_: e.activation(out=kt_.t[:], in_=pK.t[:], func=AF.Copy), r=[pK], w=[kt_])
    G(lambda e, vw_=vw_, vv=vv, h=h, col=col: e.tensor_scalar(out=vw_.t[:, 0:129], in0=vv.t[:, h, 0:129],
                                                             scalar1=wkv.t[:, col:col + 1], scalar2=None,
                                                             op0=ALU.mult), r=[vv, wkv], w=[vw_])


def phase_e_sparse(nc, S, mk, V, A, G, T, DM, load_bc, layer_norm_stats, Mall, identb, identf, onesf, onesb, dr, n_decl):
    mod_s = dr["mod_s"]; gates_s = dr["gates_s"]; u2s = dr["u2s"]; u2Ts = dr["u2Ts"]; x1s = dr["x1s"]
    Xs = dr["Xs"]; Ys = dr["Ys"]; out = dr["out"]
    w1r = dr["moe_w1"].rearrange("e k kt f -> (e k) (kt f)")
    w3r = dr["moe_w3"].rearrange("e k kt f -> (e k) (kt f)")
    w2r = dr["moe_w2"].rearrange("e f ft d -> (e f) (ft d)")
    IOA = bass.IndirectOffsetOnAxis
    NT = NOWN // 128
    _breg = []

    def breg(e):
        if not _breg or _breg[0][0] != S.nblocks:
            _breg[:] = [(S.nblocks, e.to_reg(NE * 128 - 1))]
        return _breg[0][1]
    with ExitStack() as es:
        stri = mk(es, "stri", [128, 128], BF16)
        strif = mk(es, "strif", [128, 128], F32)
        G(lambda e: e.memset(strif.t[:], 1.0), w=[strif])
        G(lambda e: e.affine_select(out=strif.t[:], in_=strif.t[:], pattern=[[1, 128]], base=-1, channel_multiplier=-1,
                                     compare_op=ALU.is_ge, fill=0.0), r=[strif], w=[strif])
        V(lambda e: e.tensor_copy(out=stri.t[:], in_=strif.t[:]), r=[strif], w=[stri])
        ut = [mk(es, "ut%d" % h, [128, NE], F32) for h in range(2)]
        for h in range(2):
            G(lambda e, h=h: e.memset(ut[h].t[:], 1.0), w=[ut[h]])
            G(lambda e, h=h: e.affine_select(out=ut[h].t[:], in_=ut[h].t[:], pattern=[[1, NE]], base=-1 - 128 * h,
                                             channel_multiplier=-1, compare_op=ALU.is_ge, fill=0.0), r=[ut[h]], w=[ut[h]])
        iota_e = mk(es, "iota_e", [128, NE], F32)
        G(lambda e: e.iota(iota_e.t[:], pattern=[[1, NE]], base=0, channel_multiplier=0, allow_small_or_imprecise_dtypes=True),
          w=[iota_e])
        iota_p = mk(es, "iota_p", [128, 1], F32)
        G(lambda e: e.iota(iota_p.t[:], pattern=[[0, 1]], base=0, channel_multiplier=1, allow_small_or_imprecise_dtypes=True),
          w=[iota_p])
        bglob = mk(es, "bglob", [128, 4], F32)
        G(lambda e: e.iota(bglob.t[:], pattern=[[128, 4]], base=0, channel_multiplier=1, allow_small_or_imprecise_dtypes=True),
          w=[bglob])
        Pall = mk(es, "Pall", [128, NT, NE], F32)
        cnt = mk(es, "cnt", [128, NE], F32)
        esc = ExitStack()
        pP = [mk(esc, "pP%d" % i, [128, NE], F32, psum=True) for i in range(2)]
        for i in range(NT):
            p = pP[i % 2]
            for j in range(i):
                T(lambda e, p=p, j=j: e.matmul(p.t[:], lhsT=onesb.t[:], rhs=Mall.t[:, j, :], start=(j == 0), stop=False),
                  r=[onesb, Mall], w=[p], inc=False)
            T(lambda e, p=p, i=i: e.matmul(p.t[:], lhsT=stri.t[:], rhs=Mall.t[:, i, :], start=(i == 0), stop=True),
              r=[stri, Mall], w=[p])
            A(lambda e, p=p, i=i: e.activation(out=Pall.t[:, i, :], in_=p.t[:], func=AF.Copy), r=[p], w=[Pall])
        p = pP[0]
        for j in range(NT):
            T(lambda e, p=p, j=j: e.matmul(p.t[:], lhsT=onesb.t[:], rhs=Mall.t[:, j, :], start=(j == 0), stop=(j == NT - 1)),
              r=[onesb, Mall], w=[p], inc=(j == NT - 1))
        V(lambda e, p=p: e.tensor_scalar_add(out=cnt.t[:], in0=p.t[:], scalar1=127.0), r=[p], w=[cnt])
        cnti = mk(es, "cnti", [128, NE], I32)
        nblk = mk(es, "nblk", [128, NE], F32)
        V(lambda e: e.tensor_copy(out=cnti.t[:], in_=cnt.t[:]), r=[cnt], w=[cnti])
        V(lambda e: e.tensor_scalar(out=cnti.t[:], in0=cnti.t[:], scalar1=7, scalar2=None, op0=ALU.arith_shift_right),
          r=[cnti], w=[cnti])
        V(lambda e: e.tensor_copy(out=nblk.t[:], in_=cnti.t[:]), r=[cnti], w=[nblk])
        pTn = mk(esc, "pTn", [128, 2, 128], F32, psum=True)
        for h in range(2):
            T(lambda e, h=h: e.transpose(out=pTn.t[:, h, :], in_=nblk.t[:, h * 128:(h + 1) * 128], identity=identf.t[:]),
              r=[nblk, identf], w=[pTn], inc=(h == 1))
        ncol = mk(es, "ncol", [128, 2], F32)
        V(lambda e: e.tensor_copy(out=ncol.t[:], in_=pTn.t[:, :, 0]), r=[pTn], w=[ncol])
        utn = [mk(es, "utn%d" % h, [128, NE], F32) for h in range(2)]
        for h in range(2):
            V(lambda e, h=h: e.tensor_scalar(out=utn[h].t[:], in0=ut[h].t[:], scalar1=ncol.t[:, h:h + 1], scalar2=None, op0=ALU.mult),
              r=[ut[h], ncol], w=[utn[h]])
        pBo = mk(esc, "pBo", [128, NE], F32, psum=True)
        for h in range(2):
            T(lambda e, h=h: e.matmul(pBo.t[:], lhsT=onesf.t[:], rhs=utn[h].t[:], start=(h == 0), stop=(h == 1)),
              r=[onesf, utn[h]], w=[pBo], inc=(h == 1))
        boff128 = mk(es, "boff128", [128, NE], F32)
        bend = mk(es, "bend", [128, NE], F32)
        V(lambda e: e.tensor_scalar(out=boff128.t[:], in0=pBo.t[:], scalar1=128.0, scalar2=1.0, op0=ALU.mult, op1=ALU.add),
          r=[pBo], w=[boff128])
        V(lambda e: e.tensor_tensor(out=bend.t[:], in0=pBo.t[:], in1=nblk.t[:], op=ALU.add), r=[pBo, nblk], w=[bend])
        becol = mk(es, "becol", [128, 4], F32)
        junk = mk(es, "junkE", [128, NE], F32)
        for bt in range(4):
            V(lambda e, bt=bt: e.tensor_scalar(out=junk.t[:], in0=bend.t[:], scalar1=bglob.t[:, bt:bt + 1], scalar2=None,
                                               op0=ALU.is_le), r=[bend, bglob], w=[junk])
            V(lambda e, bt=bt: e.reduce_sum(out=becol.t[:, bt:bt + 1], in_=junk.t[:], axis=mybir.AxisListType.X), r=[junk], w=[becol])
        dg = mk(es, "dg", [128, 4, 128], F32)
        for bt in range(4):
            V(lambda e, bt=bt: e.tensor_scalar(out=dg.t[:, bt, :], in0=identf.t[:], scalar1=becol.t[:, bt:bt + 1], scalar2=None,
                                               op0=ALU.mult), r=[identf, becol], w=[dg])
        pBe = mk(esc, "pBe", [128, 4, 128], F32, psum=True)
        for bt in range(4):
            T(lambda e, bt=bt: e.matmul(pBe.t[:, bt, :], lhsT=onesf.t[:], rhs=dg.t[:, bt, :], start=True, stop=True),
              r=[onesf, dg], w=[pBe], inc=(bt == 3))
        widf = mk(es, "widf", [128, NBLK], F32)
        widx = mk(es, "widx", [128, NBLK], U32)
        V(lambda e: e.tensor_scalar(out=widf.t[:], in0=pBe.t[:].rearrange("p a b -> p (a b)"), scalar1=128.0,
                                    scalar2=iota_p.t[:, 0:1], op0=ALU.mult, op1=ALU.add), r=[pBe, iota_p], w=[widf])
        bef = mk(es, "bef", [128, NBLK], F32)
        same = mk(es, "same", [128, NBLK], F32)
        V(lambda e: e.tensor_copy(out=bef.t[:], in_=pBe.t[:].rearrange("p a b -> p (a b)")), r=[pBe], w=[bef])
        V(lambda e: e.memset(same.t[:, 0:1], 0.0), w=[same])
        V(lambda e: e.tensor_tensor(out=same.t[:, 1:NBLK], in0=bef.t[:, 1:NBLK], in1=bef.t[:, 0:NBLK - 1], op=ALU.is_equal),
          r=[bef], w=[same])
        V(lambda e: e.scalar_tensor_tensor(out=widf.t[:], in0=same.t[:], scalar=1.0e6, in1=widf.t[:], op0=ALU.mult, op1=ALU.add),
          r=[same, widf], w=[widf])
        V(lambda e: e.tensor_copy(out=widx.t[:], in_=widf.t[:]), r=[widf], w=[widx])
        destU = mk(es, "destU", [128, NT, 8], U32)
        gk = mk(es, "gk", [128, NT, 8], F32)
        Dm = [mk(es, "Dm%d" % i, [128, NE], F32) for i in range(2)]
        dv = [mk(es, "dv%d" % i, [128, 8], F32) for i in range(2)]
        ixu = [mk(es, "ixu%d" % i, [128, 8], U32) for i in range(2)]
        ixf = [mk(es, "ixf%d" % i, [128, 8], F32) for i in range(2)]
        gtl = [mk(es, "gtl%d" % i, [128, NE + 1], F32) for i in range(2)]
        ubt = [mk(es, "ubt%d" % i, [128, D], BF16) for i in range(2)]
        for i in range(NT):
            dm = Dm[i % 2]; dv_ = dv[i % 2]; xu = ixu[i % 2]; xf = ixf[i % 2]; gl = gtl[i % 2]; ub = ubt[i % 2]
            DM(lambda e, gl=gl, i=i: e.dma_start(out=gl.t[:], in_=gates_s[:, i, :]), gl, w=[gl])
            DM(lambda e, ub=ub, i=i: e.dma_start(out=ub.t[:], in_=u2s[i * 128:(i + 1) * 128, :]), ub, w=[ub])
            V(lambda e, dm=dm, i=i: e.tensor_tensor(out=dm.t[:], in0=Pall.t[:, i, :], in1=boff128.t[:], op=ALU.add),
              r=[Pall, boff128], w=[dm])
            V(lambda e, dm=dm, i=i: e.tensor_tensor(out=dm.t[:], in0=dm.t[:], in1=Mall.t[:, i, :], op=ALU.mult), r=[dm, Mall], w=[dm])
            V(lambda e, dm=dm, dv_=dv_: e.max(out=dv_.t[:], in_=dm.t[:]), r=[dm], w=[dv_])
            V(lambda e, dm=dm, dv_=dv_, xu=xu: e.max_index(out=xu.t[:], in_max=dv_.t[:], in_values=dm.t[:]), r=[dm, dv_], w=[xu])
            V(lambda e, xu=xu, xf=xf: e.tensor_copy(out=xf.t[:], in_=xu.t[:]), r=[xu], w=[xf])
            V(lambda e, dv_=dv_: e.tensor_scalar_add(out=dv_.t[:], in0=dv_.t[:], scalar1=-1.0), r=[dv_], w=[dv_])
            V(lambda e, dv_=dv_, i=i: e.tensor_copy(out=destU.t[:, i, :], in_=dv_.t[:]), r=[dv_], w=[destU])
            for k in range(8):
                V(lambda e, dm=dm, xf=xf, gl=gl, i=i, k=k: e.scalar_tensor_tensor(
                    out=dm.t[:], in0=iota_e.t[:], scalar=xf.t[:, k:k + 1], in1=gl.t[:, 0:NE], op0=ALU.is_equal, op1=ALU.mult,
                    accum_out=gk.t[:, i, k:k + 1]), r=[iota_e, xf, gl, dm], w=[dm, gk])
            for k in range(8):
                DM(lambda e, ub=ub, i=i, k=k: e.indirect_dma_start(out=Xs[:, :], out_offset=IOA(destU.t[:, i, k:k + 1], 0),
                                                                  in_=ub.t[:], in_offset=None),
                   ub, r=[ub, destU], eng="gpsimd")
        S.flush()
        esc.close()
        es_all = es
        es = ExitStack()
        xs = [mk(es, "xs%d" % i, [128, D], BF16) for i in range(2)]
        xT = [mk(es, "xTb%d" % i, [128, 8, 128], BF16) for i in range(2)]
        w1g = [mk(es, "w1g%d" % i, [128, 8, 256], F32) for i in range(1)]
        w3g = [mk(es, "w3g%d" % i, [128, 8, 256], F32) for i in range(1)]
        w2g = [mk(es, "w2g%d" % i, [128, 2, D], F32) for i in range(1)]
        w1b = [mk(es, "w1bs%d" % i, [128, 8, 256], BF16) for i in range(2)]
        w3b = [mk(es, "w3bs%d" % i, [128, 8, 256], BF16) for i in range(2)]
        w2b = [mk(es, "w2bs%d" % i, [128, 2, D], BF16) for i in range(3)]
        s1 = [mk(es, "s1s%d" % i, [128, 2, 128], F32) for i in range(2)]
        hT = [mk(es, "hTs%d" % i, [128, 2, 128], BF16) for i in range(2)]
        yb = [mk(es, "yb%d" % i, [128, D], F32) for i in range(2)]
        pX = mk(es, "pX", [128, 8, 128], BF16, psum=True)
        ph1 = [mk(es, "ph1s%d" % i, [128, 2, 128], F32, psum=True) for i in range(1)]
        ph3 = [mk(es, "ph3s%d" % i, [128, 2, 128], F32, psum=True) for i in range(1)]
        pY = [[mk(es, "pYs%d_%d" % (i, j), [128, 512], F32, psum=True) for j in range(2)] for i in range(2)]
        def blk_bufs(b):
            bb = b % 2
            return dict(x_=xs[bb], xt_=xT[bb], g1_=w1g[0], g3_=w3g[0], g2_=w2g[0], a1=w1b[bb], a3=w3b[bb], a2=w2b[b % 3],
                        s_=s1[bb], h_=hT[bb], y_=yb[bb], p1=ph1[0], p3=ph3[0], pp=pY[bb])

        def stA(b):
            d_ = blk_bufs(b)
            x_, xt_, g1_, g3_, g2_, a1, a3, a2 = (d_[k] for k in ("x_", "xt_", "g1_", "g3_", "g2_", "a1", "a3", "a2"))
            DM(lambda e: e.dma_start(out=x_.t[:], in_=Xs[b * 128:(b + 1) * 128, :]), x_, w=[x_])
            DM(lambda e: e.indirect_dma_start(out=g1_.t[:].rearrange("p a b -> p (a b)"), out_offset=None,
                                              in_=w1r, in_offset=IOA(widx.t[:, b:b + 1], 0),
                                              bounds_check=breg(e), oob_is_err=False), g1_, r=[widx], w=[g1_], eng="gpsimd")
            DM(lambda e: e.indirect_dma_start(out=g3_.t[:].rearrange("p a b -> p (a b)"), out_offset=None,
                                              in_=w3r, in_offset=IOA(widx.t[:, b:b + 1], 0),
                                              bounds_check=breg(e), oob_is_err=False), g3_, r=[widx], w=[g3_], eng="gpsimd")
            DM(lambda e: e.indirect_dma_start(out=g2_.t[:].rearrange("p a b -> p (a b)"), out_offset=None,
                                              in_=w2r, in_offset=IOA(widx.t[:, b:b + 1], 0),
                                              bounds_check=breg(e), oob_is_err=False), g2_, r=[widx], w=[g2_], eng="gpsimd")
            A(lambda e: e.activation(out=a1.t[:], in_=g1_.t[:], func=AF.Copy), r=[g1_], w=[a1])
            V(lambda e: e.tensor_copy(out=a3.t[:], in_=g3_.t[:]), r=[g3_], w=[a3])
            A(lambda e: e.activation(out=a2.t[:], in_=g2_.t[:], func=AF.Copy), r=[g2_], w=[a2])
            for kt in range(8):
                T(lambda e, kt=kt: e.transpose(out=pX.t[:, kt, :], in_=x_.t[:, kt * 128:(kt + 1) * 128], identity=identb.t[:]),
                  r=[x_, identb], w=[pX], inc=(kt == 7))
            V(lambda e: e.tensor_copy(out=xt_.t[:], in_=pX.t[:]), r=[pX], w=[xt_])

        def stB(b):
            d_ = blk_bufs(b)
            xt_, a1, a3, s_, h_, p1, p3 = (d_[k] for k in ("xt_", "a1", "a3", "s_", "h_", "p1", "p3"))
            for ft in range(2):
                for kt in range(8):
                    T(lambda e, kt=kt, ft=ft: e.matmul(p1.t[:, ft, :], lhsT=a1.t[:, kt, ft * 128:(ft + 1) * 128],
                                                       rhs=xt_.t[:, kt, :], start=(kt == 0), stop=(kt == 7)),
                      r=[a1, xt_], w=[p1], inc=(kt == 7 and ft == 1))
            for ft in range(2):
                for kt in range(8):
                    T(lambda e, kt=kt, ft=ft: e.matmul(p3.t[:, ft, :], lhsT=a3.t[:, kt, ft * 128:(ft + 1) * 128],
                                                       rhs=xt_.t[:, kt, :], start=(kt == 0), stop=(kt == 7)),
                      r=[a3, xt_], w=[p3], inc=(kt == 7 and ft == 1))
            A(lambda e: e.activation(out=s_.t[:], in_=p1.t[:], func=AF.Silu), r=[p1], w=[s_])
            V(lambda e: e.tensor_tensor(out=h_.t[:], in0=p3.t[:], in1=s_.t[:], op=ALU.mult), r=[p3, s_], w=[h_])

        def stC(b):
            d_ = blk_bufs(b)
            h_, a2, y_, pp = (d_[k] for k in ("h_", "a2", "y_", "pp"))
            for dc in range(2):
                for ft in range(2):
                    T(lambda e, dc=dc, ft=ft: e.matmul(pp[dc].t[:], lhsT=h_.t[:, ft, :], rhs=a2.t[:, ft, dc * 512:(dc + 1) * 512],
                                                       start=(ft == 0), stop=(ft == 1)),
                      r=[h_, a2], w=[pp[dc]], inc=(ft == 1))
            A(lambda e: e.activation(out=y_.t[:, 0:512], in_=pp[0].t[:], func=AF.Copy), r=[pp[0]], w=[y_])
            V(lambda e: e.tensor_copy(out=y_.t[:, 512:1024], in_=pp[1].t[:]), r=[pp[1]], w=[y_])
            DM(lambda e: e.dma_start(out=Ys[b * 128:(b + 1) * 128, :], in_=y_.t[:]), y_, r=[y_], eng="scalar")

        stA(0)
        for b in range(NBLK):
            if b + 1 < NBLK:
                stA(b + 1)
            stB(b)
            if b >= 1:
                stC(b - 1)
            if b % 128 == 127:
                S.flush()
        stC(NBLK - 1)
        S.flush()
        es.close()
        es = es_all
        g2b = load_bc(es, "g2b", mod_s[0:1, 5 * D:6 * D], D)
        l2g = load_bc(es, "l2g", dr["ln2_g"][0:1, :], D, eng="gpsimd")
        l2b = load_bc(es, "l2b", dr["ln2_b"][0:1, :], D)
        sw1 = mk(es, "sw1", [128, 8, 256], BF16); sw3 = mk(es, "sw3", [128, 8, 256], BF16); sw2 = mk(es, "sw2", [128, 2, D], BF16)
        with ExitStack() as es3:
            f1 = mk(es3, "sf1", [128, 8, 256], F32); f3 = mk(es3, "sf3", [128, 8, 256], F32); f2 = mk(es3, "sf2", [128, 2, D], F32)
            DM(lambda e: e.dma_start(out=f1.t[:], in_=dr["moe_w1"][n_decl - 1]), f1, w=[f1])
            DM(lambda e: e.dma_start(out=f3.t[:], in_=dr["moe_w3"][n_decl - 1]), f3, w=[f3])
            DM(lambda e: e.dma_start(out=f2.t[:], in_=dr["moe_w2"][n_decl - 1]), f2, w=[f2])
            V(lambda e: e.tensor_copy(out=sw1.t[:], in_=f1.t[:]), r=[f1], w=[sw1])
            V(lambda e: e.tensor_copy(out=sw3.t[:], in_=f3.t[:]), r=[f3], w=[sw3])
            V(lambda e: e.tensor_copy(out=sw2.t[:], in_=f2.t[:]), r=[f2], w=[sw2])
            S.flush()
        uTl = [mk(es, "uTl%d" % i, [128, 8, 128], BF16) for i in range(2)]
        s1c = [mk(es, "s1c%d" % i, [128, 2, 128], F32) for i in range(2)]
        hTc = [mk(es, "hTc%d" % i, [128, 2, 128], BF16) for i in range(2)]
        acc = [mk(es, "acc%d" % i, [128, D], F32) for i in range(2)]
        yk = [mk(es, "yk%d" % i, [128, D], F32) for i in range(3)]
        xl = [mk(es, "xlc%d" % i, [128, D], F32) for i in range(2)]
        ot = [mk(es, "otc%d" % i, [128, D], F32) for i in range(2)]
        mve = [mk(es, "mvc%d" % i, [128, 4], F32) for i in range(2)]
        scr = mk(es, "scrC", [128, 12], F32)
        qh1 = mk(es, "qh1", [128, 2, 128], F32, psum=True)
        qh3 = mk(es, "qh3", [128, 2, 128], F32, psum=True)
        qY = [[mk(es, "qY%d_%d" % (i, j), [128, 512], F32, psum=True) for j in range(2)] for i in range(2)]
        nyk = 0
        for i in range(NT):
            ib = i % 2
            u_ = uTl[ib]; s_ = s1c[ib]; h_ = hTc[ib]; ac = acc[ib]; xl_ = xl[ib]; o_ = ot[ib]; m_ = mve[ib]; pp = qY[ib]
            DM(lambda e, u_=u_, i=i: e.dma_start(out=u_.t[:], in_=u2Ts[:, :, i * 128:(i + 1) * 128]), u_, w=[u_])
            DM(lambda e, xl_=xl_, i=i: e.dma_start(out=xl_.t[:], in_=x1s[i * 128:(i + 1) * 128, :]), xl_, w=[xl_])
            for ft in range(2):
                for kt in range(8):
                    T(lambda e, u_=u_, kt=kt, ft=ft: e.matmul(qh1.t[:, ft, :], lhsT=sw1.t[:, kt, ft * 128:(ft + 1) * 128],
                                                              rhs=u_.t[:, kt, :], start=(kt == 0), stop=(kt == 7)),
                      r=[sw1, u_], w=[qh1], inc=(kt == 7 and ft == 1))
            for ft in range(2):
                for kt in range(8):
                    T(lambda e, u_=u_, kt=kt, ft=ft: e.matmul(qh3.t[:, ft, :], lhsT=sw3.t[:, kt, ft * 128:(ft + 1) * 128],
                                                              rhs=u_.t[:, kt, :], start=(kt == 0), stop=(kt == 7)),
                      r=[sw3, u_], w=[qh3], inc=(kt == 7 and ft == 1))
            A(lambda e, s_=s_: e.activation(out=s_.t[:], in_=qh1.t[:], func=AF.Silu), r=[qh1], w=[s_])
            V(lambda e, s_=s_, h_=h_: e.tensor_tensor(out=h_.t[:], in0=qh3.t[:], in1=s_.t[:], op=ALU.mult), r=[qh3, s_], w=[h_])
            for dc in range(2):
                for ft in range(2):
                    T(lambda e, pp=pp, dc=dc, ft=ft, h_=h_: e.matmul(pp[dc].t[:], lhsT=h_.t[:, ft, :],
                                                                    rhs=sw2.t[:, ft, dc * 512:(dc + 1) * 512],
                                                                    start=(ft == 0), stop=(ft == 1)),
                      r=[h_, sw2], w=[pp[dc]], inc=(ft == 1))
            A(lambda e, pp=pp, ac=ac: e.activation(out=ac.t[:, 0:512], in_=pp[0].t[:], func=AF.Copy), r=[pp[0]], w=[ac])
            A(lambda e, pp=pp, ac=ac: e.activation(out=ac.t[:, 512:1024], in_=pp[1].t[:], func=AF.Copy), r=[pp[1]], w=[ac])
            for k in range(8):
                y_ = yk[nyk % 3]; nyk += 1
                DM(lambda e, y_=y_, i=i, k=k: e.indirect_dma_start(out=y_.t[:], out_offset=None, in_=Ys[:, :],
                                                                  in_offset=IOA(destU.t[:, i, k:k + 1], 0)),
                   y_, r=[destU], w=[y_], eng="gpsimd")
                V(lambda e, y_=y_, ac=ac, i=i, k=k: e.scalar_tensor_tensor(out=ac.t[:], in0=y_.t[:], scalar=gk.t[:, i, k:k + 1],
                                                                           in1=ac.t[:], op0=ALU.mult, op1=ALU.add),
                  r=[y_, gk, ac], w=[ac])
            V(lambda e, ac=ac: e.tensor_tensor(out=ac.t[:], in0=ac.t[:], in1=g2b.t[:], op=ALU.mult), r=[ac, g2b], w=[ac])
            V(lambda e, xl_=xl_, ac=ac: e.scalar_tensor_tensor(out=xl_.t[:], in0=xl_.t[:], scalar=ALPHA, in1=ac.t[:],
                                                               op0=ALU.mult, op1=ALU.add), r=[xl_, ac], w=[xl_])
            layer_norm_stats(xl_, m_, scr, D)
            V(lambda e, xl_=xl_, m_=m_: e.tensor_scalar(out=xl_.t[:], in0=xl_.t[:], scalar1=m_.t[:, 0:1], scalar2=m_.t[:, 1:2],
                                                        op0=ALU.subtract, op1=ALU.mult), r=[xl_, m_], w=[xl_])
            V(lambda e, xl_=xl_: e.tensor_tensor(out=xl_.t[:], in0=xl_.t[:], in1=l2g.t[:], op=ALU.mult), r=[xl_, l2g], w=[xl_])
            V(lambda e, xl_=xl_, o_=o_: e.tensor_tensor(out=o_.t[:], in0=xl_.t[:], in1=l2b.t[:], op=ALU.add), r=[xl_, l2b], w=[o_])
            DM(lambda e, o_=o_, i=i: e.dma_start(out=out[i * 128:(i + 1) * 128, :], in_=o_.t[:]), o_, r=[o_], eng="scalar")
        S.flush()
    return


def make_in_maps(inputs, sparse=True):
    f = lambda a: np.ascontiguousarray(np.asarray(a, dtype=np.float32))
    x = f(inputs["x"]); c = f(inputs["c"])
    w1 = np.concatenate([f(inputs["moe_w1"])[0], f(inputs["sh_w1"])], axis=0)
    w3 = np.concatenate([f(inputs["moe_w3"])[0], f(inputs["sh_w3"])], axis=0)
    w2 = np.concatenate([f(inputs["moe_w2"])[0], f(inputs["sh_w2"])], axis=0)
    if sparse:
        ne = w1.shape[0]
        w1 = np.ascontiguousarray(w1.reshape(ne, 8, 128, 256).transpose(0, 2, 1, 3))
        w3 = np.ascontiguousarray(w3.reshape(ne, 8, 128, 256).transpose(0, 2, 1, 3))
        w2 = np.ascontiguousarray(w2.reshape(ne, 2, 128, D).transpose(0, 2, 1, 3))
    shared = {
        "w_ada": f(inputs["w_ada"])[0], "b_ada": f(inputs["b_ada"]), "w_in": f(inputs["w_in"])[0],
        "conv_wT": np.ascontiguousarray(f(inputs["ml_conv_w"])[0].T),
        "conv_bT": np.ascontiguousarray(f(inputs["ml_conv_b"])[0].reshape(8, 128).T),
        "gate_b_rep": np.ascontiguousarray(np.broadcast_to(np.tile(f(inputs["ml_gate_b"])[0], 64)[None, :], (128, 512))),
        "ml_norm_g": f(inputs["ml_norm_g"]), "w_out": f(inputs["w_out"])[0],
        "ln1_g": f(inputs["ln1_g"]), "ln1_b": f(inputs["ln1_b"]),
        "w_router": f(inputs["w_router"])[0], "router_bias": f(inputs["router_bias"]),
        "moe_w1": w1, "moe_w3": w3, "moe_w2": w2,
        "ln2_g": f(inputs["ln2_g"]), "ln2_b": f(inputs["ln2_b"]),
    }
    maps = []
    for core in range(8):
        b, half = core // 2, core % 2
        m = dict(shared)
        m["x_b"] = x[b]
        m["x_own"] = np.ascontiguousarray(x[b].reshape(8, 2, 512, D)[:, half].reshape(NOWN, D))
        m["cT"] = np.ascontiguousarray(c[b].reshape(8, 128).T)
        fl = np.zeros((128, 2), np.float32); fl[:, 0] = half; fl[:, 1] = 1 - half
        m["flag"] = fl
        maps.append(m)
    return maps


_NC = None


def kernel(**inputs):
    global _NC
    if _NC is None:
        _NC = build_nc()
    maps = make_in_maps(inputs)
    res = run_bass_kernel_spmd(_NC, maps, core_ids=list(range(8)))
    outp = np.empty((4, S_LEN, D), np.float32)
    for core in range(8):
        b, half = core // 2, core % 2
        outp[b].reshape(8, 2, 512, D)[:, half] = np.asarray(res.results[core]["out"]).reshape(8, 512, D)
    return outp
```
